# Optimizing a Trainium2 kernel written in Bass

```python
import math
import jax, jax.numpy as jnp
from jax import lax
import numpy as np

D_MODEL = 1024
BATCH = 8
SEQ = 4096
DEPTH = 2

MEM_LEN = 256
BLOCK_Q = 128
H_A = 8
DH_A = 64
DC_A = 128
H_IDX = 8
DH_IDX = 64
TOPK_MAX = 256
H_SB = 8
DH_SB = 64
C_CONV = 512
CONV_W = 3
W_A = H_A * DH_A
W_SB = H_SB * DH_SB
W_BRANCH = 512
N_BRANCH = 3
H_X = 4
DH_X = 128
D_FF = 2816
N_BUCKETS = 32
MAX_DISTANCE = 128
DN_ALPHA = (2 * DEPTH) ** 0.25
DN_BETA = (8 * DEPTH) ** -0.25
LN_EPS = 1e-5
N_LN = 4
SPLITS = (W_A, DC_A, H_IDX * DH_IDX, DH_IDX, H_IDX, W_SB, W_SB, W_SB,
          C_CONV, C_CONV, C_CONV, D_MODEL, D_MODEL, D_MODEL)
P_TOTAL = sum(SPLITS)

kernel_name = "hybrid_dsa_stickbreak_shortconv_gated"


def layer_norm(x, g, b):
    xf = x.astype(jnp.float32)
    mu = jnp.mean(xf, axis=-1, keepdims=True)
    var = jnp.mean(jnp.square(xf - mu), axis=-1, keepdims=True)
    y = (xf - mu) * lax.rsqrt(var + LN_EPS) * g.astype(jnp.float32) + b.astype(jnp.float32)
    return y.astype(x.dtype)


def rms_norm(x, g):
    xf = x.astype(jnp.float32)
    y = xf * lax.rsqrt(jnp.mean(jnp.square(xf), axis=-1, keepdims=True) + LN_EPS) * g.astype(jnp.float32)
    return y.astype(x.dtype)


def swiglu_ffn(x, w_in, w_out):
    a, b = jnp.split(x @ w_in, 2, axis=-1)
    return (jax.nn.silu(a) * b) @ w_out


def t5_bucket(n):
    max_exact = N_BUCKETS // 2
    n = jnp.maximum(n, 0)
    nf = jnp.maximum(n, 1).astype(jnp.float32)
    large = max_exact + (jnp.log(nf / max_exact) / math.log(MAX_DISTANCE / max_exact)
                         * (N_BUCKETS - max_exact)).astype(jnp.int32)
    large = jnp.minimum(large, N_BUCKETS - 1)
    return jnp.where(n < max_exact, n, large)


def dsa_branch(q, c_kv, iq, ik, iw, w_uk, w_uv, rel_bias):
    bsz, seq = q.shape[0], q.shape[1]
    k_top = min(TOPK_MAX, seq // 4)
    nblk = seq // BLOCK_Q
    q_lat = jnp.einsum('bshd,hdc->bshc', q, w_uk) * (DH_A ** -0.5)
    idx_scale = (H_IDX ** -0.5) * (DH_IDX ** -0.5)

    def to_blocks(a):
        return jnp.swapaxes(a.reshape((bsz, nblk, BLOCK_Q) + a.shape[2:]), 0, 1)

    starts = jnp.arange(nblk, dtype=jnp.int32) * BLOCK_Q
    key_pos = jnp.arange(seq, dtype=jnp.int32)

    def block(args):
        ql, iqb, iwb, start = args
        t = start + jnp.arange(BLOCK_Q, dtype=jnp.int32)
        causal = key_pos[None, :] <= t[:, None]
        dots = jnp.einsum('bqhd,bsd->bhqs', iqb, ik).astype(jnp.float32)
        score = jnp.einsum('bqh,bhqs->bqs', iwb.astype(jnp.float32) * idx_scale, jax.nn.relu(dots))
        score = jnp.where(causal[None], score, -jnp.inf)
        _, idx = lax.top_k(score, k_top)
        valid = idx <= t[None, :, None]
        kv_sel = jax.vmap(lambda c, i: c[i])(c_kv, idx)
        logits = jnp.einsum('bqhc,bqkc->bhqk', ql, kv_sel).astype(jnp.float32)
        bias = rel_bias[t5_bucket(t[None, :, None] - idx)]
        logits = logits + jnp.transpose(bias, (0, 3, 1, 2)).astype(jnp.float32)
        logits = jnp.where(valid[:, None], logits, -1e30)
        p = jax.nn.softmax(logits, axis=-1).astype(kv_sel.dtype)
        return jnp.einsum('bhqk,bqkc->bqhc', p, kv_sel)

    o_lat = lax.map(block, (to_blocks(q_lat), to_blocks(iq), to_blocks(iw), starts))
    o_lat = jnp.swapaxes(o_lat, 0, 1).reshape(bsz, seq, H_A, DC_A)
    o = jnp.einsum('bshc,hcd->bshd', o_lat, w_uv)
    return o.reshape(bsz, seq, W_A)


def stick_breaking_branch(q, k, v):
    bsz, seq = q.shape[0], q.shape[1]
    scale = DH_SB ** -0.5
    outs = []
    for i in range(seq // BLOCK_Q):
        q0 = i * BLOCK_Q
        end = q0 + BLOCK_Q
        z = jnp.einsum('bqhd,bshd->bhqs', q[:, q0:end], k[:, :end]).astype(jnp.float32) * scale
        t = q0 + jnp.arange(BLOCK_Q)
        s = jnp.arange(end)
        strict = (s[None, :] < t[:, None])[None, None]
        log_not = jnp.where(strict, jax.nn.log_sigmoid(-z), 0.0)
        later = lax.cumsum(log_not, axis=3, reverse=True) - log_not
        a = jnp.where(strict, jnp.exp(jax.nn.log_sigmoid(z) + later), 0.0)
        outs.append(jnp.einsum('bhqs,bshd->bqhd', a.astype(v.dtype), v[:, :end]))
    return jnp.concatenate(outs, axis=1).reshape(bsz, seq, W_SB)


def short_conv_branch(b_gate_in, c_gate_in, h, conv_w):
    z = c_gate_in * h
    y = lax.conv_general_dilated(z, conv_w[:, None, :].astype(z.dtype), window_strides=(1,),
                                 padding=((CONV_W - 1, 0),),
                                 dimension_numbers=('NWC', 'WIO', 'NWC'),
                                 feature_group_count=C_CONV)
    return b_gate_in * y


def mixer_sublayer(x, w_in, b_gate, kv_g, w_uk, w_uv, conv_w, w_branch, w_out, rel_bias):
    bsz, seq = x.shape[0], x.shape[1]
    offsets = [int(o) for o in np.cumsum(SPLITS)[:-1]]
    (qa, ckv, iq, ik, iw, qs, ks, vs, cb, cc, ch, ga, gb, gc) = jnp.split(x @ w_in, offsets, axis=-1)
    ckv = rms_norm(ckv, kv_g)
    y_a = dsa_branch(qa.reshape(bsz, seq, H_A, DH_A), ckv,
                     iq.reshape(bsz, seq, H_IDX, DH_IDX), ik, iw, w_uk, w_uv, rel_bias)
    hs = (bsz, seq, H_SB, DH_SB)
    y_b = stick_breaking_branch(qs.reshape(hs), ks.reshape(hs), vs.reshape(hs))
    y_c = short_conv_branch(cb, cc, ch, conv_w)
    merged = (jax.nn.sigmoid(ga + b_gate[0]) * (y_a @ w_branch[0])
              + jax.nn.sigmoid(gb + b_gate[1]) * (y_b @ w_branch[1])
              + jax.nn.sigmoid(gc + b_gate[2]) * (y_c @ w_branch[2]))
    return merged @ w_out


def memory_cross_attention(x, mem, wq, wkv, wo):
    bsz, seq = x.shape[0], x.shape[1]
    q = (x @ wq).reshape(bsz, seq, H_X, DH_X)
    k, v = jnp.split(mem @ wkv, 2, axis=-1)
    k = k.reshape(bsz, mem.shape[1], H_X, DH_X)
    v = v.reshape(bsz, mem.shape[1], H_X, DH_X)
    logits = jnp.einsum('bqhd,bmhd->bhqm', q, k).astype(jnp.float32) * (DH_X ** -0.5)
    p = jax.nn.softmax(logits, axis=-1).astype(v.dtype)
    o = jnp.einsum('bhqm,bmhd->bqhd', p, v).reshape(bsz, seq, H_X * DH_X)
    return o @ wo


def setup_inputs(seed: int = 0) -> dict:
    key = jax.random.key(seed)
    ks = jax.random.split(key, 20)
    n = jax.random.normal
    f32 = jnp.float32
    return {
        "x": n(ks[0], (BATCH, SEQ, D_MODEL), f32),
        "mem": n(ks[1], (BATCH, MEM_LEN, D_MODEL), f32),
        "ln_g": 1.0 + 0.01 * n(ks[2], (DEPTH, N_LN, D_MODEL), f32),
        "ln_b": 0.01 * n(ks[3], (DEPTH, N_LN, D_MODEL), f32),
        "ffn_w_in": n(ks[4], (DEPTH, 2, D_MODEL, 2 * D_FF), f32) * D_MODEL ** -0.5,
        "ffn_w_out": n(ks[5], (DEPTH, 2, D_FF, D_MODEL), f32) * (D_FF ** -0.5 * DN_BETA),
        "w_mix_in": n(ks[6], (DEPTH, D_MODEL, P_TOTAL), f32) * D_MODEL ** -0.5,
        "b_gate": 0.01 * n(ks[7], (DEPTH, N_BRANCH, D_MODEL), f32),
        "kv_norm_g": 1.0 + 0.01 * n(ks[8], (DEPTH, DC_A), f32),
        "w_uk": n(ks[9], (DEPTH, H_A, DH_A, DC_A), f32) * DH_A ** -0.5,
        "w_uv": n(ks[10], (DEPTH, H_A, DC_A, DH_A), f32) * DC_A ** -0.5,
        "conv_w": n(ks[11], (DEPTH, CONV_W, C_CONV), f32) * CONV_W ** -0.5,
        "w_branch": n(ks[12], (DEPTH, N_BRANCH, W_BRANCH, D_MODEL), f32) * W_BRANCH ** -0.5,
        "w_mix_out": n(ks[13], (DEPTH, D_MODEL, D_MODEL), f32) * (D_MODEL ** -0.5 * DN_BETA),
        "xa_wq": n(ks[14], (DEPTH, D_MODEL, H_X * DH_X), f32) * D_MODEL ** -0.5,
        "xa_wkv": n(ks[15], (DEPTH, D_MODEL, 2 * H_X * DH_X), f32) * D_MODEL ** -0.5,
        "xa_wo": n(ks[16], (DEPTH, H_X * DH_X, D_MODEL), f32) * ((H_X * DH_X) ** -0.5 * DN_BETA),
        "rel_bias": 0.1 * n(ks[17], (N_BUCKETS, H_A), f32),
    }


def reference(x, mem, ln_g, ln_b, ffn_w_in, ffn_w_out, w_mix_in, b_gate, kv_norm_g, w_uk, w_uv,
              conv_w, w_branch, w_mix_out, xa_wq, xa_wkv, xa_wo, rel_bias):
    for l in range(DEPTH):
        x = layer_norm(DN_ALPHA * x + 0.5 * swiglu_ffn(x, ffn_w_in[l, 0], ffn_w_out[l, 0]),
                       ln_g[l, 0], ln_b[l, 0])
        x = layer_norm(DN_ALPHA * x + mixer_sublayer(x, w_mix_in[l], b_gate[l], kv_norm_g[l], w_uk[l],
                                                     w_uv[l], conv_w[l], w_branch[l], w_mix_out[l],
                                                     rel_bias),
                       ln_g[l, 1], ln_b[l, 1])
        x = layer_norm(DN_ALPHA * x + memory_cross_attention(x, mem, xa_wq[l], xa_wkv[l], xa_wo[l]),
                       ln_g[l, 2], ln_b[l, 2])
        x = layer_norm(DN_ALPHA * x + 0.5 * swiglu_ffn(x, ffn_w_in[l, 1], ffn_w_out[l, 1]),
                       ln_g[l, 3], ln_b[l, 3])
    return x
```

```python
import numpy as np
from contextlib import ExitStack
import concourse.bass as bass
import concourse.mybir as mybir
from concourse.bass_utils import run_bass_kernel_spmd

F32 = mybir.dt.float32
BF16 = mybir.dt.bfloat16
AF = mybir.ActivationFunctionType
ALU = mybir.AluOpType
AX = mybir.AxisListType


class Res:
    __slots__ = ("name", "w", "r", "excl", "dram")

    def __init__(self, name, excl=False):
        self.name = name
        self.w = None
        self.r = {}
        self.excl = excl
        self.dram = False


class Prog:
    ENG = ("pe", "act", "dve", "pool", "sp")

    def __init__(self, nc, n_slots=12):
        self.nc = nc
        self.ops = {e: [] for e in self.ENG}
        self.cnt = {e: 0 for e in self.ENG}
        self.seen = {e: {} for e in self.ENG}
        self.n_slots = n_slots
        self.slot_uses = {q: [0] * n_slots for q in ("sp", "pool", "act")}
        self.slot_rr = {q: 0 for q in ("sp", "pool", "act")}
        self.pe_pending = False
        self.nops = 0

    def _deps(self, eng, reads, writes):
        deps = {}

        def add(kv):
            if kv is None:
                return
            k, v = kv
            if deps.get(k, 0) < v:
                deps[k] = v
        for r in reads:
            add(r.w)
            if r.excl:
                for k, v in r.r.items():
                    add((k, v))
        for w in writes:
            add(w.w)
            for k, v in w.r.items():
                add((k, v))
        out = []
        seen = self.seen[eng]
        for k, v in deps.items():
            if k == "pe" and eng == "pe":
                continue
            if seen.get(k, 0) >= v:
                continue
            seen[k] = v
            out.append((k, v))
        return out

    def _register(self, key, val, reads, writes):
        for r in reads:
            if r.excl:
                r.w = (key, val)
                r.r = {}
            else:
                if r.r.get(key, 0) < val:
                    r.r[key] = val
        for w in writes:
            w.w = (key, val)
            w.r = {}

    def op(self, eng, emit, reads=(), writes=(), inc=True):
        waits = self._deps(eng, reads, writes)
        if inc:
            self.cnt[eng] += 1
            val = self.cnt[eng]
            if eng == "pe":
                self.pe_pending = False
        else:
            assert eng == "pe"
            val = self.cnt[eng] + 1
            self.pe_pending = True
        self._register(eng, val, reads, writes)
        self.ops[eng].append((waits, emit, (eng, 1) if inc else None))
        self.nops += 1

    def dma(self, q, emit, reads=(), writes=()):
        j = self.slot_rr[q]
        self.slot_rr[q] = (j + 1) % self.n_slots
        n = self.slot_uses[q][j]
        key = ("dma", q, j)
        waits = self._deps(q, reads, writes)
        if n > 0 and self.seen[q].get(key, 0) < 16 * n:
            self.seen[q][key] = 16 * n
            waits.append((key, 16 * n))
        self.slot_uses[q][j] = n + 1
        val = 16 * (n + 1)
        self._register(key, val, reads, writes)
        self.ops[q].append((waits, emit, (key, 16)))
        self.nops += 1

    def barrier(self):
        assert not self.pe_pending
        targets = [(e, self.cnt[e]) for e in ("pe", "act", "dve", "pool") if self.cnt[e] > 0]
        for q in self.slot_uses:
            for j, n in enumerate(self.slot_uses[q]):
                if n > 0:
                    targets.append((("dma", q, j), 16 * n))
        for e in self.ENG:
            waits = []
            for k, v in targets:
                if k == "pe" and e == "pe":
                    continue
                if self.seen[e].get(k, 0) < v:
                    self.seen[e][k] = v
                    waits.append((k, v))
            self.ops[e].append((waits, None, None))

    def wait_all(self, eng, resources):
        waits = self._deps(eng, resources, ())
        self.ops[eng].append((waits, None, None))

    def emit(self):
        nc = self.nc
        keys = set(self.ENG)
        for q in self.slot_uses:
            for j in range(self.n_slots):
                if self.slot_uses[q][j] > 0:
                    keys.add(("dma", q, j))
        with ExitStack() as es:
            sems = {}
            for k in sorted(keys, key=str):
                nm = k if isinstance(k, str) else "d_%s_%d" % (k[1], k[2])
                sems[k] = es.enter_context(nc.semaphore("s_" + nm))
            block = es.enter_context(nc.Block())

            def run(e, name):
                for waits, emit, inc in self.ops[name]:
                    for k, v in waits:
                        e.wait_ge(sems[k], v)
                    if emit is None:
                        continue
                    ins = emit(e)
                    if inc is not None:
                        ins.then_inc(sems[inc[0]], inc[1])

            @block.tensor
            def _(e):
                run(e, "pe")

            @block.scalar
            def _(e):
                run(e, "act")

            @block.vector
            def _(e):
                run(e, "dve")

            @block.gpsimd
            def _(e):
                run(e, "pool")

            @block.sync
            def _(e):
                run(e, "sp")

D = 1024
DFF = 2816
PMIX = 7368
ALPHA = 4.0 ** 0.25
EPS = 1e-5
KTOP = 256
NIT = 16
NEG = -1.0e30


class Tl:
    def __init__(self, t, name, excl=False):
        self.t = t
        self.r = Res(name, excl)

    def __getitem__(self, k):
        return self.t[k]


def build(S=4096, L=2, dbg=False, stop_after=99):
    NT = S // 128
    nc = bass.Bass("TRN2", target_bir_lowering=False)
    P = Prog(nc)

    def din(name, shape, dt=F32):
        return nc.dram_tensor(name, list(shape), dt, kind="ExternalInput").ap()

    def dres(name):
        r = Res(name)
        r.dram = True
        return r

    def dscr(name, shape, dt):
        return nc.dram_tensor(name, list(shape), dt, kind=("ExternalOutput" if dbg else "Internal")).ap(), dres(name)

    xT = din("xT", [8, 128, S]); r_xT = dres("xT")
    memT = din("memT", [8, 128, 256])
    lng = din("lng", [L, 4, 128, 8]); lnb = din("lnb", [L, 4, 128, 8])
    w_in = din("ffn_w_in", [L, 2, D, 2 * DFF]); w_out = din("ffn_w_out", [L, 2, DFF, D])
    w_mix = din("w_mix_in", [L, D, PMIX])
    bgate = din("bgate", [L, 3, 128, 8])
    kvg = din("kvg", [L, 128])
    wuk = din("wuk", [L, 128, 4, 128])
    wuv = din("wuv", [L, 8, 128, 64])
    convw = din("convw", [L, 128, 4, 3])
    wbr = din("w_branch", [L, 3, 512, D])
    wmo = din("w_mix_out", [L, D, D])
    xwq = din("xa_wq", [L, D, 512]); xwkv = din("xa_wkv", [L, D, D]); xwo = din("xa_wo", [L, 512, D])
    biasT = din("biasT", [128, 8, 256])
    rb31 = din("rb31", [8])
    outT = nc.dram_tensor("outT", [8, 128, S], F32, kind="ExternalOutput").ap(); r_outT = dres("outT")
    r_const = dres("const_in")

    XA, r_XA = dscr("XA", [8, 128, S], F32)
    XB, r_XB = dscr("XB", [8, 128, S], F32)
    QL, r_QL = dscr("QL", [8, 128, S], BF16)
    IQT, r_IQT = dscr("IQT", [4, 128, S], BF16)
    IKT, r_IKT = dscr("IKT", [64, S], BF16)
    QST, r_QST = dscr("QST", [4, 128, S], BF16)
    KST, r_KST = dscr("KST", [4, 128, S], BF16)
    VS, r_VS = dscr("VS", [S, 512], BF16)
    CKV, r_CKV = dscr("CKV", [S, 128], BF16)
    CKVT, r_CKVT = dscr("CKVT", [128, S], BF16)
    IW, r_IW = dscr("IW", [S, 8], F32)
    YCT, r_YCT = dscr("YCT", [4, 128, S], BF16)
    YAT, r_YAT = dscr("YAT", [4, 128, S], BF16)
    YBT, r_YBT = dscr("YBT", [4, 128, S], BF16)

    def mm(out, lhsT, rhs, start, stop, reads, writes, inc=None):
        P.op("pe", lambda e: e.matmul(out, lhsT=lhsT, rhs=rhs, start=start, stop=stop),
             reads, writes, inc=(stop if inc is None else inc))

    def tr(out, in_, ident, reads, writes, inc=True):
        P.op("pe", lambda e: e.transpose(out=out, in_=in_, identity=ident), reads, writes, inc=inc)

    def act(out, in_, func, reads, writes, bias=None, scale=None, accum=None):
        kw = {}
        if bias is not None:
            kw["bias"] = bias
        if scale is not None:
            kw["scale"] = scale
        if accum is not None:
            kw["accum_out"] = accum
        P.op("act", lambda e: e.activation(out=out, in_=in_, func=func, **kw), reads, writes)

    def tt(eng, out, a, b, op, reads, writes):
        P.op(eng, lambda e: e.tensor_tensor(out=out, in0=a, in1=b, op=op), reads, writes)

    def ts(eng, out, a, s1, op0, reads, writes, s2=None, op1=None, accum=None):
        kw = {}
        if op1 is not None:
            kw["op1"] = op1
        if accum is not None:
            kw["accum_out"] = accum
        P.op(eng, lambda e: e.tensor_scalar(out=out, in0=a, scalar1=s1, scalar2=s2, op0=op0, **kw), reads, writes)

    def stt(out, in0, scalar, in1, op0, op1, reads, writes, accum=None):
        kw = {}
        if accum is not None:
            kw["accum_out"] = accum
        P.op("dve", lambda e: e.scalar_tensor_tensor(out=out, in0=in0, scalar=scalar, in1=in1, op0=op0, op1=op1, **kw),
             reads, writes)

    def cp(eng, out, in_, reads, writes):
        if eng == "act":
            P.op("act", lambda e: e.copy(out=out, in_=in_), reads, writes)
        else:
            P.op(eng, lambda e: e.tensor_copy(out=out, in_=in_), reads, writes)

    def red(out, in_, op, reads, writes):
        P.op("dve", lambda e: e.tensor_reduce(out=out, in_=in_, axis=AX.X, op=op), reads, writes)

    def recip(out, in_, reads, writes):
        P.op("dve", lambda e: e.reciprocal(out=out, in_=in_), reads, writes)

    def memset(eng, out, val, writes):
        P.op(eng, lambda e: e.memset(out, val), (), writes)

    def dma(q, out, in_, reads, writes, **kw):
        reads = [r for r in reads if not r.dram]
        writes = [r for r in writes if not r.dram]
        P.dma(q, lambda e: e.dma_start(out=out, in_=in_, **kw), reads, writes)

    def wload(dst, k_chunks, src2d, c0, c1, rows0=0):
        for k in range(k_chunks):
            dma("pool", dst.t[:, k, 0:c1 - c0], src2d[rows0 + k * 128: rows0 + (k + 1) * 128, c0:c1],
                [r_const], [dst.r], max_dma_last_dim=8192)

    top = ExitStack()

    uid = [0]

    def alloc(stack, name, shape, dt):
        uid[0] += 1
        name = "%s_%d" % (name, uid[0])
        return Tl(stack.enter_context(nc.sbuf_tensor(name, list(shape), dt)), name)

    PS = [Tl(top.enter_context(nc.psum_tensor("ps%d" % i, [128, 512], F32)), "ps%d" % i, True) for i in range(7)]
    PB = Tl(top.enter_context(nc.psum_tensor("pb", [128, 1024], BF16)), "pb", True)

    identf = alloc(top, "identf", [128, 128], F32)
    ident = alloc(top, "ident", [128, 128], BF16)
    onesM = alloc(top, "onesM", [128, 128], F32)
    onesB = alloc(top, "onesB", [128, 128], BF16)
    caus = alloc(top, "caus", [128, 128], F32)
    strict = alloc(top, "strict", [128, 128], F32)
    pow2 = alloc(top, "pow2", [128, NIT], F32)
    neglo = alloc(top, "neglo", [128, 1], F32)
    epsT = alloc(top, "epsT", [128, 1], F32)
    oneT = alloc(top, "oneT", [128, 1], F32)
    memset("pool", identf.t[:], 0.0, [identf.r])
    P.op("pool", lambda e: e.affine_select(out=identf.t[:], in_=identf.t[:], pattern=[[-1, 128]],
                                           compare_op=ALU.not_equal, fill=1.0, base=0, channel_multiplier=1),
         [identf.r], [identf.r])
    cp("dve", ident.t[:], identf.t[:], [identf.r], [ident.r])
    memset("pool", onesM.t[:], 1.0 / D, [onesM.r])
    memset("pool", onesB.t[:], 1.0, [onesB.r])
    memset("pool", caus.t[:], 0.0, [caus.r])
    P.op("pool", lambda e: e.affine_select(out=caus.t[:], in_=caus.t[:], pattern=[[-1, 128]],
                                           compare_op=ALU.is_ge, fill=NEG, base=0, channel_multiplier=1),
         [caus.r], [caus.r])
    memset("pool", strict.t[:], 1.0, [strict.r])
    P.op("pool", lambda e: e.affine_select(out=strict.t[:], in_=strict.t[:], pattern=[[-1, 128]],
                                           compare_op=ALU.is_gt, fill=0.0, base=0, channel_multiplier=1),
         [strict.r], [strict.r])
    for j in range(NIT):
        memset("pool", pow2.t[:, j:j + 1], 0.5 ** (j + 1), [pow2.r])
    memset("pool", neglo.t[:], -1.0e29, [neglo.r])
    memset("pool", epsT.t[:], EPS, [epsT.r])
    memset("pool", oneT.t[:], 1.0, [oneT.r])

    def ln_part1(Z, SQ, G, pa):
        for c in range(8):
            mm(pa.t[:, 0:G], onesM.t[:], Z.t[:, c, :], c == 0, c == 7, [onesM.r, Z.r], [pa.r])
        for c in range(8):
            tt("dve", Z.t[:, c, :], Z.t[:, c, :], pa.t[:, 0:G], ALU.subtract, [Z.r, pa.r], [Z.r])
        act(SQ.t[:], Z.t[:], AF.Square, [Z.r], [SQ.r])

    def ln_part2(Z, SQ, RS, G, gT, bT, dst, r_dst, g0, pb_):
        for c in range(8):
            mm(pb_.t[:, 0:G], onesM.t[:], SQ.t[:, c, :], c == 0, c == 7, [onesM.r, SQ.r], [pb_.r])
        act(RS.t[:], pb_.t[:, 0:G], AF.Sqrt, [pb_.r, epsT.r], [RS.r], bias=epsT.t[:, 0:1])
        recip(RS.t[:], RS.t[:], [RS.r], [RS.r])
        for c in range(8):
            tt("pool", Z.t[:, c, :], Z.t[:, c, :], RS.t[:], ALU.mult, [Z.r, RS.r], [Z.r])
        for c in range(8):
            act(SQ.t[:, c, :], Z.t[:, c, :], AF.Identity, [Z.r, gT.r, bT.r], [SQ.r],
                scale=gT.t[:, c:c + 1], bias=bT.t[:, c:c + 1])
        dma("sp", dst[:, :, g0:g0 + G].rearrange("c p t -> p c t"), SQ.t[:], [SQ.r], [r_dst])

    def layer_norm(Z, SQ, RS, G, gT, bT, dst, r_dst, g0, pa, pb_):
        ln_part1(Z, SQ, G, pa)
        ln_part2(Z, SQ, RS, G, gT, bT, dst, r_dst, g0, pb_)

    def ffn_phase(l, which, ln_i, src, r_src, dst, r_dst):
        G = 256
        NG = S // G
        with ExitStack() as st:
            Win = alloc(st, "Win", [128, 8, 2 * DFF], BF16)
            Wout = alloc(st, "Wout", [128, 22, D], BF16)
            gT = alloc(st, "gT", [128, 8], F32); bT = alloc(st, "bT", [128, 8], F32)
            X32 = [alloc(st, "X32%d" % b, [128, 8, G], F32) for b in range(2)]
            Xb = [alloc(st, "Xb%d" % b, [128, 8, G], BF16) for b in range(2)]
            H = alloc(st, "H", [128, 22, G], BF16)
            SA = [alloc(st, "SA%d" % i, [128, G], F32) for i in range(2)]
            Z = [alloc(st, "Z%d" % b, [128, 8, G], F32) for b in range(2)]
            SQ = alloc(st, "SQ", [128, 8, G], F32)
            RS = alloc(st, "RS", [128, G], F32)
            dma("sp", gT.t[:], lng[l, ln_i], [r_const], [gT.r])
            dma("sp", bT.t[:], lnb[l, ln_i], [r_const], [bT.r])
            wload(Win, 8, w_in[l, which], 0, 2 * DFF)
            wload(Wout, 22, w_out[l, which], 0, D)

            def load(g):
                b = g % 2
                g0 = g * G
                dma("sp", X32[b].t[:], src[:, :, g0:g0 + G].rearrange("c p t -> p c t"), [r_src], [X32[b].r])
                cp("pool", Xb[b].t[:], X32[b].t[:], [X32[b].r], [Xb[b].r])

            def inproj(g):
                xb = Xb[g % 2]
                for j in range(22):
                    pa = PS[(2 * j) % 4]; pb_ = PS[(2 * j + 1) % 4]
                    for k in range(8):
                        mm(pa.t[:, 0:G], Win.t[:, k, j * 128:(j + 1) * 128], xb.t[:, k, :], k == 0, k == 7,
                           [Win.r, xb.r], [pa.r])
                    for k in range(8):
                        mm(pb_.t[:, 0:G], Win.t[:, k, DFF + j * 128:DFF + (j + 1) * 128], xb.t[:, k, :], k == 0, k == 7,
                           [Win.r, xb.r], [pb_.r])
                    sa = SA[j % 2]
                    act(sa.t[:], pa.t[:, 0:G], AF.Silu, [pa.r], [sa.r])
                    stt(H.t[:, j, :], sa.t[:], 0.5, pb_.t[:, 0:G], ALU.mult, ALU.mult, [sa.r, pb_.r], [H.r])

            def outproj(g):
                x32, z = X32[g % 2], Z[g % 2]
                for c in range(8):
                    po = PS[4 + (c % 2)]
                    for j in range(22):
                        mm(po.t[:, 0:G], Wout.t[:, j, c * 128:(c + 1) * 128], H.t[:, j, :], j == 0, j == 21,
                           [Wout.r, H.r], [po.r])
                    stt(z.t[:, c, :], x32.t[:, c, :], ALPHA, po.t[:, 0:G], ALU.mult, ALU.add, [x32.r, po.r], [z.r])

            load(0)
            if NG > 1:
                load(1)
            inproj(0)
            outproj(0)
            for g in range(NG):
                if g + 2 < NG:
                    load(g + 2)
                if g + 1 < NG:
                    inproj(g + 1)
                ln_part1(Z[g % 2], SQ, G, PS[6])
                if g + 1 < NG:
                    outproj(g + 1)
                ln_part2(Z[g % 2], SQ, RS, G, gT, bT, dst, r_dst, g * G, PS[6])
            P.barrier()

    C_QA, C_CKV, C_IQ, C_IK, C_IW, C_QS, C_KS, C_VS, C_CB, C_CC, C_CH, C_G = \
        0, 512, 640, 1152, 1216, 1224, 1736, 2248, 2760, 3272, 3784, 4296

    def proj_phase(l, src, r_src):
        G = 512
        NW = C_G
        with ExitStack() as st:
            Wm = alloc(st, "Wm", [128, 8, NW], BF16)
            WUK = alloc(st, "WUK", [128, 4, 128], BF16)
            CW = alloc(st, "CW", [128, 4, 3], F32)
            KVG = alloc(st, "KVG", [128, 128], F32)
            X32 = alloc(st, "X32", [128, 8, G], F32)
            Xb = alloc(st, "Xb", [128, 8, G], BF16)
            QA = alloc(st, "QA", [128, 4, G], BF16)
            OB = [alloc(st, "OB%d" % i, [128, G], BF16) for i in range(3)]
            CCs = alloc(st, "CCs", [128, G], F32)
            ZC = [alloc(st, "ZC%d" % q, [128, G + 2], F32) for q in range(4)]
            YC = alloc(st, "YC", [128, G], F32)
            JK = alloc(st, "JK", [128, 128], F32)
            SS = alloc(st, "SS", [128, 2], F32)
            CKb = alloc(st, "CKb", [128, 128], BF16)
            CKTb = alloc(st, "CKTb", [128, 128], BF16)
            IWs = alloc(st, "IWs", [128, 8], F32)
            VSb = alloc(st, "VSb", [128, 512], BF16)
            wload(Wm, 8, w_mix[l], 0, NW)
            dma("pool", WUK.t[:], wuk[l], [r_const], [WUK.r])
            dma("sp", CW.t[:], convw[l], [r_const], [CW.r])
            dma("sp", KVG.t[:], kvg[l].partition_broadcast(128), [r_const], [KVG.r])
            for q in range(4):
                memset("pool", ZC[q].t[:, 0:2], 0.0, [ZC[q].r])
            rot = [0]

            def nextps():
                rot[0] = (rot[0] + 1) % 5
                return PS[rot[0]]
            obr = [0]

            def nextob():
                obr[0] = (obr[0] + 1) % 3
                return OB[obr[0]]

            def fm_chunk(col, M=128):
                ps = nextps()
                for k in range(8):
                    mm(ps.t[0:M, 0:G], Wm.t[:, k, col:col + M], Xb.t[:, k, :], k == 0, k == 7, [Wm.r, Xb.r], [ps.r])
                return ps

            for g in range(S // G):
                g0 = g * G
                dma("sp", X32.t[:], src[:, :, g0:g0 + G].rearrange("c p t -> p c t"), [r_src], [X32.r])
                cp("pool", Xb.t[:], X32.t[:], [X32.r], [Xb.r])
                for q in range(4):
                    ps = fm_chunk(C_QA + q * 128)
                    cp("act", QA.t[:, q, :], ps.t[:, 0:G], [ps.r], [QA.r])
                for h in range(8):
                    p0 = (h % 2) * 64
                    ps = nextps()
                    mm(ps.t[:, 0:G], WUK.t[p0:p0 + 64, h // 2, :], QA.t[p0:p0 + 64, h // 2, :], True, True,
                       [WUK.r, QA.r], [ps.r])
                    ob = nextob()
                    act(ob.t[:], ps.t[:, 0:G], AF.Copy, [ps.r], [ob.r], scale=0.125)
                    dma("sp", QL[h, :, g0:g0 + G], ob.t[:], [ob.r], [r_QL])
                for (col, dstT, r_d, sc) in ((C_IQ, IQT, r_IQT, 1.0), (C_QS, QST, r_QST, 0.125), (C_KS, KST, r_KST, 1.0)):
                    for q in range(4):
                        ps = fm_chunk(col + q * 128)
                        ob = nextob()
                        act(ob.t[:], ps.t[:, 0:G], AF.Copy, [ps.r], [ob.r], scale=sc)
                        dma("sp", dstT[q, :, g0:g0 + G], ob.t[:], [ob.r], [r_d])
                ps = fm_chunk(C_IK, 64)
                ob = nextob()
                cp("act", ob.t[0:64, :], ps.t[0:64, 0:G], [ps.r], [ob.r])
                dma("sp", IKT[:, g0:g0 + G], ob.t[0:64, :], [ob.r], [r_IKT])
                for q in range(4):
                    ps = fm_chunk(C_CC + q * 128)
                    cp("act", CCs.t[:], ps.t[:, 0:G], [ps.r], [CCs.r])
                    ps = fm_chunk(C_CH + q * 128)
                    zc = ZC[q]
                    tt("dve", zc.t[:, 2:G + 2], CCs.t[:], ps.t[:, 0:G], ALU.mult, [CCs.r, ps.r], [zc.r])
                    ts("dve", YC.t[:], zc.t[:, 2:G + 2], CW.t[:, q, 2:3], ALU.mult, [zc.r, CW.r], [YC.r])
                    stt(YC.t[:], zc.t[:, 1:G + 1], CW.t[:, q, 1:2], YC.t[:], ALU.mult, ALU.add, [zc.r, CW.r, YC.r], [YC.r])
                    stt(YC.t[:], zc.t[:, 0:G], CW.t[:, q, 0:1], YC.t[:], ALU.mult, ALU.add, [zc.r, CW.r, YC.r], [YC.r])
                    ps = fm_chunk(C_CB + q * 128)
                    ob = nextob()
                    tt("dve", ob.t[:], YC.t[:], ps.t[:, 0:G], ALU.mult, [YC.r, ps.r], [ob.r])
                    dma("sp", YCT[q, :, g0:g0 + G], ob.t[:], [ob.r], [r_YCT])
                    cp("pool", zc.t[:, 0:2], zc.t[:, G:G + 2], [zc.r], [zc.r])
                for t4 in range(G // 128):
                    t0 = g0 + t4 * 128
                    xs = slice(t4 * 128, (t4 + 1) * 128)
                    ps = nextps()
                    for k in range(8):
                        mm(ps.t[:, 0:128], Xb.t[:, k, xs], Wm.t[:, k, C_CKV:C_CKV + 128], k == 0, k == 7,
                           [Wm.r, Xb.r], [ps.r])
                    act(JK.t[:], ps.t[:, 0:128], AF.Square, [ps.r], [JK.r, SS.r], accum=SS.t[:, 0:1])
                    act(SS.t[:, 1:2], SS.t[:, 0:1], AF.Sqrt, [SS.r, epsT.r], [SS.r], scale=1.0 / 128, bias=epsT.t[:, 0:1])
                    recip(SS.t[:, 1:2], SS.t[:, 1:2], [SS.r], [SS.r])
                    stt(CKb.t[:], ps.t[:, 0:128], SS.t[:, 1:2], KVG.t[:], ALU.mult, ALU.mult, [ps.r, SS.r, KVG.r], [CKb.r])
                    dma("sp", CKV[t0:t0 + 128, :], CKb.t[:], [CKb.r], [r_CKV])
                    tr(PB.t[:, 0:128], CKb.t[:], ident.t[:], [CKb.r, ident.r], [PB.r])
                    cp("act", CKTb.t[:], PB.t[:, 0:128], [PB.r], [CKTb.r])
                    dma("sp", CKVT[:, t0:t0 + 128], CKTb.t[:], [CKTb.r], [r_CKVT])
                    ps = nextps()
                    for k in range(8):
                        mm(ps.t[:, 0:8], Xb.t[:, k, xs], Wm.t[:, k, C_IW:C_IW + 8], k == 0, k == 7, [Wm.r, Xb.r], [ps.r])
                    cp("act", IWs.t[:], ps.t[:, 0:8], [ps.r], [IWs.r])
                    dma("sp", IW[t0:t0 + 128, :], IWs.t[:], [IWs.r], [r_IW])
                    ps = nextps()
                    for k in range(8):
                        mm(ps.t[:, 0:512], Xb.t[:, k, xs], Wm.t[:, k, C_VS:C_VS + 512], k == 0, k == 7, [Wm.r, Xb.r], [ps.r])
                    cp("act", VSb.t[:], ps.t[:, 0:512], [ps.r], [VSb.r])
                    dma("sp", VS[t0:t0 + 128, :], VSb.t[:], [VSb.r], [r_VS])
            P.barrier()

    def chunks(n):
        return [(c0, min(512, n - c0)) for c0 in range(0, n, 512)]

    def dsa_phase(l):
        with ExitStack() as st:
            CKVX = alloc(st, "CKVX", [128, NT, 128], BF16)
            CKT = alloc(st, "CKT", [128, S], BF16)
            IK2 = alloc(st, "IK2", [128, S], BF16)
            BI = alloc(st, "BI", [128, 8, 256], F32)
            RB = alloc(st, "RB", [128, 8], F32)
            WUVP = alloc(st, "WUVP", [128, 8, 128], BF16)
            QLg = [alloc(st, "QLg%d" % b, [128, 8, 512], BF16) for b in range(2)]
            IQg = [alloc(st, "IQg%d" % b, [128, 4, 512], BF16) for b in range(2)]
            IWg = [alloc(st, "IWg%d" % b, [128, 4, 8], F32) for b in range(2)]
            WA = [alloc(st, "WA%d" % b, [128, 8], F32) for b in range(2)]
            SG = [alloc(st, "SG%d" % b, [128, 8], F32) for b in range(2)]
            SC = [alloc(st, "SC%d" % b, [128, S], F32) for b in range(2)]
            Mk = [alloc(st, "Mk%d" % b, [128, S], BF16) for b in range(2)]
            TH = [alloc(st, "TH%d" % b, [128, 8], F32) for b in range(2)]
            WALL = [alloc(st, "WALL%d" % b, [128, NIT], F32) for b in range(2)]
            PM = [alloc(st, "PM%d" % b, [128, S], BF16) for b in range(2)]
            PT = [alloc(st, "PT%d" % b, [128, NT, 128], BF16) for b in range(2)]
            TMP = [alloc(st, "TMP%d" % i, [128, 512], F32) for i in range(4)]
            JKB = alloc(st, "JKB", [128, S], BF16)
            RSM = alloc(st, "RSM", [128, 8, 8], F32)
            RSUM = alloc(st, "RSUM", [128, 8], F32)
            OLN = alloc(st, "OLN", [128, 8, 128], BF16)
            OLT = alloc(st, "OLT", [128, 8, 128], BF16)
            YAg = alloc(st, "YAg", [128, 4, 512], BF16)
            dma("sp", CKVX.t[:], CKV.rearrange("(j p) c -> p j c", p=128), [r_CKV], [CKVX.r])
            dma("sp", CKT.t[:], CKVT, [r_CKVT], [CKT.r])
            dma("sp", IK2.t[0:64, :], IKT, [r_IKT], [IK2.r])
            dma("sp", IK2.t[64:128, :], IKT, [r_IKT], [IK2.r])
            dma("sp", BI.t[:], biasT, [r_const], [BI.r])
            dma("sp", RB.t[:], rb31.partition_broadcast(128), [r_const], [RB.r])
            memset("pool", WUVP.t[:], 0.0, [WUVP.r])
            for h in range(8):
                p0 = (h % 2) * 64
                dma("pool", WUVP.t[:, h, p0:p0 + 64], wuv[l, h], [r_const], [WUVP.r])
            tmr = [0]

            def nexttmp():
                tmr[0] = (tmr[0] + 1) % 4
                return TMP[tmr[0]]
            psr = [0]

            def nextps():
                psr[0] = (psr[0] + 1) % 3
                return PS[psr[0]]

            def load_group(g):
                b = g % 2
                g0 = g * 512
                dma("sp", QLg[b].t[:], QL[:, :, g0:g0 + 512].rearrange("h c t -> c h t"), [r_QL], [QLg[b].r])
                dma("sp", IQg[b].t[:], IQT[:, :, g0:g0 + 512].rearrange("q p t -> p q t"), [r_IQT], [IQg[b].r])
                dma("sp", IWg[b].t[:], IW[g0:g0 + 512, :].rearrange("(a p) h -> p a h", p=128), [r_IW], [IWg[b].r])

            def index_scores(i):
                g, t4 = divmod(i, 4)
                gb = g % 2
                b = i % 2
                n = (i + 1) * 128
                tsl = slice(t4 * 128, (t4 + 1) * 128)
                sc, th, wall, wa, sg, mk = SC[b], TH[b], WALL[b], WA[b], SG[b], Mk[b]
                act(wa.t[:], IWg[gb].t[:, t4, :], AF.Abs, [IWg[gb].r], [wa.r])
                P.op("act", lambda e: e.sign(out=sg.t[:], in_=IWg[gb].t[:, t4, :]), [IWg[gb].r], [sg.r])
                for (c0, w) in chunks(n):
                    for h in range(8):
                        p0 = (h % 2) * 64
                        ps = nextps()
                        mm(ps.t[:, 0:w], IQg[gb].t[p0:p0 + 64, h // 2, tsl], IK2.t[p0:p0 + 64, c0:c0 + w], True, True,
                           [IQg[gb].r, IK2.r], [ps.r])
                        R = nexttmp()
                        act(R.t[:, 0:w], ps.t[:, 0:w], AF.Relu, [ps.r, wa.r], [R.r], scale=wa.t[:, h:h + 1])
                        if h == 0:
                            ts("dve", sc.t[:, c0:c0 + w], R.t[:, 0:w], sg.t[:, 0:1], ALU.mult, [R.r, sg.r], [sc.r])
                        else:
                            stt(sc.t[:, c0:c0 + w], R.t[:, 0:w], sg.t[:, h:h + 1], sc.t[:, c0:c0 + w], ALU.mult, ALU.add,
                                [R.r, sg.r, sc.r], [sc.r])
                tt("dve", sc.t[:, n - 128:n], sc.t[:, n - 128:n], caus.t[:], ALU.add, [sc.r, caus.r], [sc.r])
                steps = []
                if i >= 2:
                    def init():
                        red(th.t[:, 0:1], sc.t[:, 0:n - 128], ALU.min, [sc.r], [th.r])
                        red(th.t[:, 1:2], sc.t[:, 0:n], ALU.max, [sc.r], [th.r])
                        tt("dve", th.t[:, 2:3], th.t[:, 1:2], th.t[:, 0:1], ALU.subtract, [th.r], [th.r])
                        ts("dve", wall.t[:], pow2.t[:], th.t[:, 2:3], ALU.mult, [pow2.r, th.r], [wall.r])
                    steps.append(init)

                    def mkstep(j):
                        def step():
                            tt("dve", th.t[:, 3:4], th.t[:, 0:1], wall.t[:, j:j + 1], ALU.add, [th.r, wall.r], [th.r])
                            ts("dve", JKB.t[:, 0:n], sc.t[:, 0:n], th.t[:, 3:4], ALU.is_ge, [sc.r, th.r], [th.r],
                               op1=ALU.add, accum=th.t[:, 4:5])
                            ts("dve", th.t[:, 5:6], th.t[:, 4:5], KTOP - 0.5, ALU.is_ge, [th.r, wall.r], [th.r],
                               s2=wall.t[:, j:j + 1], op1=ALU.mult)
                            tt("dve", th.t[:, 0:1], th.t[:, 0:1], th.t[:, 5:6], ALU.add, [th.r], [th.r])
                        return step
                    for j in range(NIT):
                        steps.append(mkstep(j))
                    steps.append(lambda: ts("dve", mk.t[:, 0:n], sc.t[:, 0:n], th.t[:, 0:1], ALU.is_ge, [sc.r, th.r], [mk.r]))
                else:
                    steps.append(lambda: ts("dve", mk.t[:, 0:n], sc.t[:, 0:n], neglo.t[:, 0:1], ALU.is_ge, [sc.r, neglo.r], [mk.r]))
                return steps

            def heads(i, pending):
                g, t4 = divmod(i, 4)
                gb = g % 2
                n = (i + 1) * 128
                tsl = slice(t4 * 128, (t4 + 1) * 128)
                mk = Mk[i % 2]
                nb0 = max(0, n - 256)
                per_head = (len(pending) + 7) // 8
                for h in range(8):
                    pm, pt = PM[h % 2], PT[h % 2]
                    ch = chunks(n)
                    for ci, (c0, w) in enumerate(ch):
                        ps = nextps()
                        mm(ps.t[:, 0:w], QLg[gb].t[:, h, tsl], CKT.t[:, c0:c0 + w], True, True, [QLg[gb].r, CKT.r], [ps.r])
                        Pe = nexttmp()
                        fa, fb = c0, min(c0 + w, nb0)
                        na, nb_ = max(c0, nb0), c0 + w
                        if fb > fa:
                            act(Pe.t[:, fa - c0:fb - c0], ps.t[:, fa - c0:fb - c0], AF.Exp, [ps.r, RB.r], [Pe.r],
                                bias=RB.t[:, h:h + 1])
                        if nb_ > na:
                            bo = 256 - (n - na)
                            tt("dve", Pe.t[:, na - c0:nb_ - c0], ps.t[:, na - c0:nb_ - c0], BI.t[:, h, bo:bo + (nb_ - na)],
                               ALU.add, [ps.r, BI.r], [Pe.r])
                            act(Pe.t[:, na - c0:nb_ - c0], Pe.t[:, na - c0:nb_ - c0], AF.Exp, [Pe.r], [Pe.r])
                        stt(pm.t[:, c0:c0 + w], Pe.t[:, 0:w], 1.0, mk.t[:, c0:c0 + w], ALU.mult, ALU.mult,
                            [Pe.r, mk.r], [pm.r, RSM.r], accum=RSM.t[:, h, ci:ci + 1])
                    red(RSUM.t[:, h:h + 1], RSM.t[:, h, 0:len(ch)], ALU.add, [RSM.r], [RSUM.r])
                    recip(RSUM.t[:, h:h + 1], RSUM.t[:, h:h + 1], [RSUM.r], [RSUM.r])
                    for _ in range(per_head):
                        if pending:
                            pending.pop(0)()
                    for j0 in range(0, i + 1, 8):
                        nbk = min(8, i + 1 - j0)
                        for jj in range(nbk):
                            j = j0 + jj
                            tr(PB.t[:, jj * 128:(jj + 1) * 128], pm.t[:, j * 128:(j + 1) * 128], ident.t[:],
                               [pm.r, ident.r], [PB.r], inc=(jj == nbk - 1))
                        cp("act", pt.t[:, j0:j0 + nbk, :], PB.t[:, 0:nbk * 128].rearrange("p (j t) -> p j t", t=128),
                           [PB.r], [pt.r])
                    ol = PS[3 + (h % 2)]
                    for j in range(i + 1):
                        mm(ol.t[:, 0:128], pt.t[:, j, :], CKVX.t[:, j, :], j == 0, j == i, [pt.r, CKVX.r], [ol.r])
                    act(OLN.t[:, h, :], ol.t[:, 0:128], AF.Copy, [ol.r, RSUM.r], [OLN.r], scale=RSUM.t[:, h:h + 1])
                while pending:
                    pending.pop(0)()
                for h in range(8):
                    tr(PB.t[:, h * 128:(h + 1) * 128], OLN.t[:, h, :], ident.t[:], [OLN.r, ident.r], [PB.r], inc=(h == 7))
                cp("act", OLT.t[:], PB.t[:].rearrange("p (j t) -> p j t", t=128), [PB.r], [OLT.r])
                for q in range(4):
                    ps = PS[5]
                    mm(ps.t[:, 0:128], WUVP.t[:, 2 * q, :], OLT.t[:, 2 * q, :], True, False, [WUVP.r, OLT.r], [ps.r])
                    mm(ps.t[:, 0:128], WUVP.t[:, 2 * q + 1, :], OLT.t[:, 2 * q + 1, :], False, True, [WUVP.r, OLT.r], [ps.r])
                    cp("act", YAg.t[:, q, tsl], ps.t[:, 0:128], [ps.r], [YAg.r])
                if t4 == 3:
                    g0 = g * 512
                    dma("sp", YAT[:, :, g0:g0 + 512].rearrange("q p t -> p q t"), YAg.t[:], [YAg.r], [r_YAT])

            load_group(0)
            for s_ in index_scores(0):
                s_()
            for i in range(NT):
                pending = []
                if i + 1 < NT:
                    if (i + 1) % 4 == 0:
                        load_group((i + 1) // 4)
                    pending = index_scores(i + 1)
                heads(i, pending)
            P.barrier()

    def sb_phase(l):
        with ExitStack() as st:
            KS_ = alloc(st, "KS_", [128, 4, S], BF16)
            VSX = alloc(st, "VSX", [128, NT, 512], BF16)
            QSg = [alloc(st, "QSg%d" % b, [128, 4, 512], BF16) for b in range(2)]
            LB = [alloc(st, "LB%d" % b, [128, S + 1], F32) for b in range(2)]
            NLX = [alloc(st, "NLX%d" % b, [128, S], F32) for b in range(2)]
            AB = [alloc(st, "AB%d" % b, [128, S], BF16) for b in range(2)]
            PT = [alloc(st, "PT2%d" % b, [128, NT, 128], BF16) for b in range(2)]
            TMP = [alloc(st, "TMQ%d" % i, [128, 512], F32) for i in range(4)]
            TSM = [alloc(st, "TSM%d" % b, [128, 16], F32) for b in range(2)]
            TOT = [alloc(st, "TOT%d" % b, [128, 2], F32) for b in range(2)]
            YBs = alloc(st, "YBs", [128, 512], BF16)
            YBg = alloc(st, "YBg", [128, 4, 512], BF16)
            dma("sp", KS_.t[:], KST.rearrange("q p t -> p q t"), [r_KST], [KS_.r])
            dma("sp", VSX.t[:], VS.rearrange("(j p) c -> p j c", p=128), [r_VS], [VSX.r])
            for b in range(2):
                memset("pool", LB[b].t[:, 0:1], 0.0, [LB[b].r])
            tmr = [0]

            def nexttmp():
                tmr[0] = (tmr[0] + 1) % 4
                return TMP[tmr[0]]
            psr = [0]

            def nextps():
                psr[0] = (psr[0] + 1) % 4
                return PS[psr[0]]
            yb = PS[4]

            def pass1(i, h):
                g, t4 = divmod(i, 4)
                qs = QSg[g % 2]
                n = (i + 1) * 128
                tsl = slice(t4 * 128, (t4 + 1) * 128)
                p0 = (h % 2) * 64
                lb, tsm, tot = LB[h % 2], TSM[h % 2], TOT[h % 2]
                memset("dve", tsm.t[:], 0.0, [tsm.r])
                for ci, (c0, w) in enumerate(chunks(n)):
                    ps = nextps()
                    mm(ps.t[:, 0:w], qs.t[p0:p0 + 64, h // 2, tsl], KS_.t[p0:p0 + 64, h // 2, c0:c0 + w], True, True,
                       [qs.r, KS_.r], [ps.r])
                    E1 = nexttmp()
                    act(E1.t[:, 0:w], ps.t[:, 0:w], AF.Exp, [ps.r], [E1.r])
                    last = (c0 + w == n)
                    wf = w - 128 if last else w
                    if wf > 0:
                        act(lb.t[:, 1 + c0:1 + c0 + wf], E1.t[:, 0:wf], AF.Ln, [E1.r, oneT.r], [lb.r, tsm.r],
                            bias=oneT.t[:, 0:1], accum=tsm.t[:, ci:ci + 1])
                    if last:
                        act(lb.t[:, 1 + n - 128:1 + n], E1.t[:, w - 128:w], AF.Ln, [E1.r, oneT.r], [lb.r], bias=oneT.t[:, 0:1])
                        tt("dve", lb.t[:, 1 + n - 128:1 + n], lb.t[:, 1 + n - 128:1 + n], strict.t[:], ALU.mult,
                           [lb.r, strict.r], [lb.r])
                        red(tsm.t[:, 15:16], lb.t[:, 1 + n - 128:1 + n], ALU.add, [lb.r], [tsm.r])
                red(tot.t[:, 0:1], tsm.t[:], ALU.add, [tsm.r], [tot.r])
                ts("dve", tot.t[:, 1:2], tot.t[:, 0:1], -1.0, ALU.mult, [tot.r], [tot.r])

            def pass2(i, h):
                g, t4 = divmod(i, 4)
                qs = QSg[g % 2]
                n = (i + 1) * 128
                tsl = slice(t4 * 128, (t4 + 1) * 128)
                p0 = (h % 2) * 64
                lb, tot, nlx, ab, pt = LB[h % 2], TOT[h % 2], NLX[h % 2], AB[h % 2], PT[h % 2]
                P.op("dve", lambda e: e.tensor_tensor_scan(out=nlx.t[:, 0:n], data0=lb.t[:, 0:n], data1=lb.t[:, 0:n],
                                                           initial=tot.t[:, 1:2], op0=ALU.add, op1=ALU.min),
                     [lb.r, tot.r], [nlx.r])
                for ci, (c0, w) in enumerate(chunks(n)):
                    ps = nextps()
                    mm(ps.t[:, 0:w], qs.t[p0:p0 + 64, h // 2, tsl], KS_.t[p0:p0 + 64, h // 2, c0:c0 + w], True, True,
                       [qs.r, KS_.r], [ps.r])
                    EB = nexttmp()
                    tt("dve", EB.t[:, 0:w], ps.t[:, 0:w], nlx.t[:, c0:c0 + w], ALU.add, [ps.r, nlx.r], [EB.r])
                    act(ab.t[:, c0:c0 + w], EB.t[:, 0:w], AF.Exp, [EB.r], [ab.r])
                tt("dve", ab.t[:, n - 128:n], ab.t[:, n - 128:n], strict.t[:], ALU.mult, [ab.r, strict.r], [ab.r])
                for j0 in range(0, i + 1, 8):
                    nbk = min(8, i + 1 - j0)
                    for jj in range(nbk):
                        j = j0 + jj
                        tr(PB.t[:, jj * 128:(jj + 1) * 128], ab.t[:, j * 128:(j + 1) * 128], ident.t[:],
                           [ab.r, ident.r], [PB.r], inc=(jj == nbk - 1))
                    cp("act", pt.t[:, j0:j0 + nbk, :], PB.t[:, 0:nbk * 128].rearrange("p (j t) -> p j t", t=128),
                       [PB.r], [pt.r])
                for j in range(i + 1):
                    mm(yb.t[:, h * 64:(h + 1) * 64], pt.t[:, j, :], VSX.t[:, j, h * 64:(h + 1) * 64], j == 0, j == i,
                       [pt.r, VSX.r], [yb.r])

            def load_group(g):
                g0 = g * 512
                dma("sp", QSg[g % 2].t[:], QST[:, :, g0:g0 + 512].rearrange("q p t -> p q t"), [r_QST], [QSg[g % 2].r])

            load_group(0)
            pass1(0, 0)
            for i in range(NT):
                g, t4 = divmod(i, 4)
                tsl = slice(t4 * 128, (t4 + 1) * 128)
                for h in range(8):
                    if h < 7:
                        pass1(i, h + 1)
                    elif i + 1 < NT:
                        if (i + 1) % 4 == 0:
                            load_group((i + 1) // 4)
                        pass1(i + 1, 0)
                    pass2(i, h)
                cp("act", YBs.t[:], yb.t[:, 0:512], [yb.r], [YBs.r])
                for q in range(4):
                    tr(PB.t[:, q * 128:(q + 1) * 128], YBs.t[:, q * 128:(q + 1) * 128], ident.t[:], [YBs.r, ident.r], [PB.r], inc=(q == 3))
                cp("act", YBg.t[:, :, tsl], PB.t[:, 0:512].rearrange("p (q t) -> p q t", t=128), [PB.r], [YBg.r])
                if t4 == 3:
                    g0 = g * 512
                    dma("sp", YBT[:, :, g0:g0 + 512].rearrange("q p t -> p q t"), YBg.t[:], [YBg.r], [r_YBT])
            P.barrier()

    def merge_phase(l, src, r_src, dst, r_dst):
        G = 256
        NG = S // G
        with ExitStack() as st:
            WG = alloc(st, "WG", [128, 8, 3 * D], BF16)
            WBR = alloc(st, "WBR", [128, 12, D], BF16)
            WO = alloc(st, "WO", [128, 8, D], BF16)
            BG = alloc(st, "BG", [128, 3, 8], F32)
            gT = alloc(st, "gT", [128, 8], F32); bT = alloc(st, "bT", [128, 8], F32)
            X32 = [alloc(st, "X32%d" % b, [128, 8, G], F32) for b in range(2)]
            Xb = [alloc(st, "Xb%d" % b, [128, 8, G], BF16) for b in range(2)]
            Y = [[alloc(st, "Y%d_%d" % (i, b), [128, 4, G], BF16) for i in range(3)] for b in range(2)]
            SGt = [alloc(st, "SGt%d" % i, [128, G], F32) for i in range(2)]
            TM = [alloc(st, "TM%d" % i, [128, G], F32) for i in range(2)]
            MGf = alloc(st, "MGf", [128, 8, G], F32)
            MGb = alloc(st, "MGb", [128, 8, G], BF16)
            Z = [alloc(st, "Z%d" % b, [128, 8, G], F32) for b in range(2)]
            SQ = alloc(st, "SQ", [128, 8, G], F32)
            RS = alloc(st, "RS", [128, G], F32)
            dma("sp", gT.t[:], lng[l, 1], [r_const], [gT.r])
            dma("sp", bT.t[:], lnb[l, 1], [r_const], [bT.r])
            dma("sp", BG.t[:], bgate[l].rearrange("i p c -> p i c"), [r_const], [BG.r])
            wload(WG, 8, w_mix[l], C_G, PMIX)
            for i in range(3):
                for k in range(4):
                    dma("pool", WBR.t[:, i * 4 + k, :], wbr[l, i, k * 128:(k + 1) * 128, :], [r_const], [WBR.r], max_dma_last_dim=8192)
            wload(WO, 8, wmo[l], 0, D)
            srcs = ((YAT, r_YAT), (YBT, r_YBT), (YCT, r_YCT))

            def load(g):
                b = g % 2
                g0 = g * G
                dma("sp", X32[b].t[:], src[:, :, g0:g0 + G].rearrange("c p t -> p c t"), [r_src], [X32[b].r])
                cp("pool", Xb[b].t[:], X32[b].t[:], [X32[b].r], [Xb[b].r])
                for i in range(3):
                    dma("sp", Y[b][i].t[:], srcs[i][0][:, :, g0:g0 + G].rearrange("q p t -> p q t"), [srcs[i][1]], [Y[b][i].r])

            def stage1(g):
                xb, y = Xb[g % 2], Y[g % 2]
                n_ = 0
                for c in range(8):
                    for i in range(3):
                        pg = PS[n_ % 2]; pb_ = PS[2 + n_ % 2]; sg = SGt[n_ % 2]; tm = TM[n_ % 2]
                        n_ += 1
                        col = i * D + c * 128
                        for k in range(8):
                            mm(pg.t[:, 0:G], WG.t[:, k, col:col + 128], xb.t[:, k, :], k == 0, k == 7, [WG.r, xb.r], [pg.r])
                        act(sg.t[:], pg.t[:, 0:G], AF.Sigmoid, [pg.r, BG.r], [sg.r], bias=BG.t[:, i, c:c + 1])
                        for k in range(4):
                            mm(pb_.t[:, 0:G], WBR.t[:, i * 4 + k, c * 128:(c + 1) * 128], y[i].t[:, k, :], k == 0, k == 3,
                               [WBR.r, y[i].r], [pb_.r])
                        if i == 0:
                            tt("dve", MGf.t[:, c, :], sg.t[:], pb_.t[:, 0:G], ALU.mult, [sg.r, pb_.r], [MGf.r])
                        else:
                            tt("dve", tm.t[:], sg.t[:], pb_.t[:, 0:G], ALU.mult, [sg.r, pb_.r], [tm.r])
                            tt("pool", MGf.t[:, c, :], MGf.t[:, c, :], tm.t[:], ALU.add, [MGf.r, tm.r], [MGf.r])
                cp("pool", MGb.t[:], MGf.t[:], [MGf.r], [MGb.r])

            def stage2(g):
                x32, z = X32[g % 2], Z[g % 2]
                for c in range(8):
                    po = PS[4 + (c % 2)]
                    for k in range(8):
                        mm(po.t[:, 0:G], WO.t[:, k, c * 128:(c + 1) * 128], MGb.t[:, k, :], k == 0, k == 7, [WO.r, MGb.r], [po.r])
                    stt(z.t[:, c, :], x32.t[:, c, :], ALPHA, po.t[:, 0:G], ALU.mult, ALU.add, [x32.r, po.r], [z.r])

            load(0)
            if NG > 1:
                load(1)
            stage1(0)
            stage2(0)
            for g in range(NG):
                if g + 2 < NG:
                    load(g + 2)
                if g + 1 < NG:
                    stage1(g + 1)
                ln_part1(Z[g % 2], SQ, G, PS[6])
                if g + 1 < NG:
                    stage2(g + 1)
                ln_part2(Z[g % 2], SQ, RS, G, gT, bT, dst, r_dst, g * G, PS[6])
            P.barrier()

    def xattn_phase(l, src, r_src, dst, r_dst):
        G = 256
        with ExitStack() as st:
            WQ = alloc(st, "WQ", [128, 8, 512], BF16)
            WKV = alloc(st, "WKV", [128, 8, D], BF16)
            WO = alloc(st, "WOx", [128, 4, D], BF16)
            gT = alloc(st, "gT", [128, 8], F32); bT = alloc(st, "bT", [128, 8], F32)
            MT = alloc(st, "MT", [128, 8, 256], BF16)
            KT = alloc(st, "KT", [128, 4, 256], BF16)
            VX = alloc(st, "VX", [128, 2, 512], BF16)
            X32 = alloc(st, "X32", [128, 8, G], F32)
            Xb = alloc(st, "Xb", [128, 8, G], BF16)
            QT = alloc(st, "QT", [128, 4, G], BF16)
            PTm = [alloc(st, "PTm%d" % i, [128, G], BF16) for i in range(2)]
            RD = alloc(st, "RD", [128, G], F32)
            OT = alloc(st, "OT", [128, 4, G], BF16)
            Z = alloc(st, "Z", [128, 8, G], F32)
            SQ = alloc(st, "SQ", [128, 8, G], F32)
            RS = alloc(st, "RS", [128, G], F32)
            dma("sp", gT.t[:], lng[l, 2], [r_const], [gT.r])
            dma("sp", bT.t[:], lnb[l, 2], [r_const], [bT.r])
            wload(WQ, 8, xwq[l], 0, 512)
            wload(WKV, 8, xwkv[l], 0, D)
            wload(WO, 4, xwo[l], 0, D)
            dma("pool", MT.t[:], memT.rearrange("c p m -> p c m"), [r_const], [MT.r])
            for h in range(4):
                ps = PS[h % 2]
                for k in range(8):
                    mm(ps.t[:, 0:256], WKV.t[:, k, h * 128:(h + 1) * 128], MT.t[:, k, :], k == 0, k == 7, [WKV.r, MT.r], [ps.r])
                cp("act", KT.t[:, h, :], ps.t[:, 0:256], [ps.r], [KT.r])
            for mt in range(2):
                ps = PS[2 + mt]
                for k in range(8):
                    mm(ps.t[:, 0:512], MT.t[:, k, mt * 128:(mt + 1) * 128], WKV.t[:, k, 512:1024], k == 0, k == 7, [WKV.r, MT.r], [ps.r])
                cp("act", VX.t[:, mt, :], ps.t[:, 0:512], [ps.r], [VX.r])
            for g in range(S // G):
                g0 = g * G
                dma("sp", X32.t[:], src[:, :, g0:g0 + G].rearrange("c p t -> p c t"), [r_src], [X32.r])
                cp("pool", Xb.t[:], X32.t[:], [X32.r], [Xb.r])
                for h in range(4):
                    ps = PS[h % 2]
                    for k in range(8):
                        mm(ps.t[:, 0:G], WQ.t[:, k, h * 128:(h + 1) * 128], Xb.t[:, k, :], k == 0, k == 7, [WQ.r, Xb.r], [ps.r])
                    act(QT.t[:, h, :], ps.t[:, 0:G], AF.Copy, [ps.r], [QT.r], scale=128.0 ** -0.5)
                for h in range(4):
                    for mt in range(2):
                        ps = PS[mt]
                        mm(ps.t[:, 0:G], KT.t[:, h, mt * 128:(mt + 1) * 128], QT.t[:, h, :], True, True, [KT.r, QT.r], [ps.r])
                        act(PTm[mt].t[:], ps.t[:, 0:G], AF.Exp, [ps.r], [PTm[mt].r])
                    po = PS[2]; pd = PS[3]
                    for mt in range(2):
                        mm(po.t[:, 0:G], VX.t[:, mt, h * 128:(h + 1) * 128], PTm[mt].t[:], mt == 0, mt == 1, [VX.r, PTm[mt].r], [po.r])
                    for mt in range(2):
                        mm(pd.t[:, 0:G], onesB.t[:], PTm[mt].t[:], mt == 0, mt == 1, [onesB.r, PTm[mt].r], [pd.r])
                    recip(RD.t[:], pd.t[:, 0:G], [pd.r], [RD.r])
                    tt("dve", OT.t[:, h, :], po.t[:, 0:G], RD.t[:], ALU.mult, [po.r, RD.r], [OT.r])
                for c in range(8):
                    po = PS[4 + (c % 2)]
                    for k in range(4):
                        mm(po.t[:, 0:G], WO.t[:, k, c * 128:(c + 1) * 128], OT.t[:, k, :], k == 0, k == 3, [WO.r, OT.r], [po.r])
                    stt(Z.t[:, c, :], X32.t[:, c, :], ALPHA, po.t[:, 0:G], ALU.mult, ALU.add, [X32.r, po.r], [Z.r])
                layer_norm(Z, SQ, RS, G, gT, bT, dst, r_dst, g0, PS[6], PS[4])
            P.barrier()

    P.barrier()
    cur, r_cur = xT, r_xT
    nph = [0]

    def run(f, *a):
        if nph[0] < stop_after:
            f(*a)
        nph[0] += 1
    for l in range(L):
        last = (l == L - 1)
        run(ffn_phase, l, 0, 0, cur, r_cur, XB, r_XB)
        run(proj_phase, l, XB, r_XB)
        run(dsa_phase, l)
        run(sb_phase, l)
        run(merge_phase, l, XB, r_XB, XA, r_XA)
        run(xattn_phase, l, XA, r_XA, XB, r_XB)
        if last:
            run(ffn_phase, l, 1, 3, XB, r_XB, outT, r_outT)
        else:
            run(ffn_phase, l, 1, 3, XB, r_XB, XA, r_XA)
        cur, r_cur = XA, r_XA
    P.emit()
    top.close()
    return nc, P


def _bucket_table():
    import math
    n = np.arange(256)
    nf = np.maximum(n, 1).astype(np.float32)
    large = 16 + (np.log(nf / np.float32(16)) / np.float32(math.log(8.0)) * np.float32(16)).astype(np.int32)
    large = np.minimum(large, 31)
    return np.where(n < 16, n, large)


def prep_inputs(x, mem, ln_g, ln_b, ffn_w_in, ffn_w_out, w_mix_in, b_gate, kv_norm_g, w_uk, w_uv,
                conv_w, w_branch, w_mix_out, xa_wq, xa_wkv, xa_wo, rel_bias):
    f = lambda a: np.ascontiguousarray(np.asarray(a, dtype=np.float32))
    B, S, _ = x.shape
    L = ln_g.shape[0]
    bt = _bucket_table()
    tl = np.arange(128)[:, None]
    col = np.arange(256)[None, :]
    nrel = np.where(col < 128, tl - col + 128, tl - (col - 128))
    idx = bt[np.clip(nrel, 0, 255)]
    rel_bias = f(rel_bias)
    biasT = f(np.transpose(rel_bias[idx], (0, 2, 1)))
    shared = {
        "lng": f(np.transpose(f(ln_g).reshape(L, 4, 8, 128), (0, 1, 3, 2))),
        "lnb": f(np.transpose(f(ln_b).reshape(L, 4, 8, 128), (0, 1, 3, 2))),
        "ffn_w_in": f(ffn_w_in), "ffn_w_out": f(ffn_w_out), "w_mix_in": f(w_mix_in),
        "bgate": f(np.transpose(f(b_gate).reshape(L, 3, 8, 128), (0, 1, 3, 2))),
        "kvg": f(kv_norm_g),
        "wuk": f(np.transpose(f(w_uk).reshape(L, 4, 2, 64, 128), (0, 2, 3, 1, 4)).reshape(L, 128, 4, 128)),
        "wuv": f(w_uv),
        "convw": f(np.transpose(f(conv_w).reshape(L, 3, 4, 128), (0, 3, 2, 1))),
        "w_branch": f(w_branch), "w_mix_out": f(w_mix_out),
        "xa_wq": f(xa_wq), "xa_wkv": f(xa_wkv), "xa_wo": f(xa_wo),
        "biasT": biasT, "rb31": f(rel_bias[31]),
    }
    maps = []
    for b in range(B):
        m = dict(shared)
        m["xT"] = f(f(x[b]).T.reshape(8, 128, S))
        m["memT"] = f(f(mem[b]).T.reshape(8, 128, 256))
        maps.append(m)
    return maps


_NC_CACHE = {}


def kernel(**inputs):
    x = np.asarray(inputs["x"])
    B, S, _ = x.shape
    L = np.asarray(inputs["ln_g"]).shape[0]
    key = (S, L)
    if key not in _NC_CACHE:
        _NC_CACHE[key] = build(S, L)[0]
    nc = _NC_CACHE[key]
    maps = prep_inputs(**inputs)
    res = run_bass_kernel_spmd(nc, maps, core_ids=list(range(B)))
    out = np.empty((B, S, D), dtype=np.float32)
    for b in range(B):
        out[b] = np.asarray(res.results[b]["outT"]).reshape(D, S).T
    return out
```

```python
import numpy as np
from contextlib import ExitStack
import concourse.bass as bass
import concourse.mybir as mybir
from concourse.bass_utils import run_bass_kernel_spmd

F32 = mybir.dt.float32
BF16 = mybir.dt.bfloat16
AF = mybir.ActivationFunctionType
ALU = mybir.AluOpType
AX = mybir.AxisListType


class Res:
    __slots__ = ("name", "w", "r", "excl", "dram")

    def __init__(self, name, excl=False):
        self.name = name
        self.w = None
        self.r = {}
        self.excl = excl
        self.dram = False


class Prog:
    ENG = ("pe", "act", "dve", "pool", "sp")

    def __init__(self, nc, n_slots=12):
        self.nc = nc
        self.ops = {e: [] for e in self.ENG}
        self.cnt = {e: 0 for e in self.ENG}
        self.seen = {e: {} for e in self.ENG}
        self.n_slots = n_slots
        self.slot_uses = {q: [0] * n_slots for q in ("sp", "pool", "act")}
        self.slot_rr = {q: 0 for q in ("sp", "pool", "act")}
        self.pe_pending = False
        self.nops = 0

    def _deps(self, eng, reads, writes):
        deps = {}

        def add(kv):
            if kv is None:
                return
            k, v = kv
            if deps.get(k, 0) < v:
                deps[k] = v
        for r in reads:
            add(r.w)
            if r.excl:
                for k, v in r.r.items():
                    add((k, v))
        for w in writes:
            add(w.w)
            for k, v in w.r.items():
                add((k, v))
        out = []
        seen = self.seen[eng]
        for k, v in deps.items():
            if k == "pe" and eng == "pe":
                continue
            if seen.get(k, 0) >= v:
                continue
            seen[k] = v
            out.append((k, v))
        return out

    def _register(self, key, val, reads, writes):
        for r in reads:
            if r.excl:
                r.w = (key, val)
                r.r = {}
            else:
                if r.r.get(key, 0) < val:
                    r.r[key] = val
        for w in writes:
            w.w = (key, val)
            w.r = {}

    def op(self, eng, emit, reads=(), writes=(), inc=True):
        waits = self._deps(eng, reads, writes)
        if inc:
            self.cnt[eng] += 1
            val = self.cnt[eng]
            if eng == "pe":
                self.pe_pending = False
        else:
            assert eng == "pe"
            val = self.cnt[eng] + 1
            self.pe_pending = True
        self._register(eng, val, reads, writes)
        self.ops[eng].append((waits, emit, (eng, 1) if inc else None))
        self.nops += 1

    def dma(self, q, emit, reads=(), writes=()):
        j = self.slot_rr[q]
        self.slot_rr[q] = (j + 1) % self.n_slots
        n = self.slot_uses[q][j]
        key = ("dma", q, j)
        waits = self._deps(q, reads, writes)
        if n > 0 and self.seen[q].get(key, 0) < 16 * n:
            self.seen[q][key] = 16 * n
            waits.append((key, 16 * n))
        self.slot_uses[q][j] = n + 1
        val = 16 * (n + 1)
        self._register(key, val, reads, writes)
        self.ops[q].append((waits, emit, (key, 16)))
        self.nops += 1

    def barrier(self):
        assert not self.pe_pending
        targets = [(e, self.cnt[e]) for e in ("pe", "act", "dve", "pool") if self.cnt[e] > 0]
        for q in self.slot_uses:
            for j, n in enumerate(self.slot_uses[q]):
                if n > 0:
                    targets.append((("dma", q, j), 16 * n))
        for e in self.ENG:
            waits = []
            for k, v in targets:
                if k == "pe" and e == "pe":
                    continue
                if self.seen[e].get(k, 0) < v:
                    self.seen[e][k] = v
                    waits.append((k, v))
            self.ops[e].append((waits, None, None))

    def wait_all(self, eng, resources):
        waits = self._deps(eng, resources, ())
        self.ops[eng].append((waits, None, None))

    def emit(self):
        nc = self.nc
        keys = set(self.ENG)
        for q in self.slot_uses:
            for j in range(self.n_slots):
                if self.slot_uses[q][j] > 0:
                    keys.add(("dma", q, j))
        with ExitStack() as es:
            sems = {}
            for k in sorted(keys, key=str):
                nm = k if isinstance(k, str) else "d_%s_%d" % (k[1], k[2])
                sems[k] = es.enter_context(nc.semaphore("s_" + nm))
            block = es.enter_context(nc.Block())

            def run(e, name):
                for waits, emit, inc in self.ops[name]:
                    for k, v in waits:
                        e.wait_ge(sems[k], v)
                    if emit is None:
                        continue
                    ins = emit(e)
                    if inc is not None:
                        ins.then_inc(sems[inc[0]], inc[1])

            @block.tensor
            def _(e):
                run(e, "pe")

            @block.scalar
            def _(e):
                run(e, "act")

            @block.vector
            def _(e):
                run(e, "dve")

            @block.gpsimd
            def _(e):
                run(e, "pool")

            @block.sync
            def _(e):
                run(e, "sp")

D = 1024
DFF = 2816
PMIX = 7368
ALPHA = 4.0 ** 0.25
EPS = 1e-5
KTOP = 256
NIT = 16
NEG = -1.0e30


class Tl:
    def __init__(self, t, name, excl=False):
        self.t = t
        self.r = Res(name, excl)
        self.rl = [self.r]

    def __getitem__(self, k):
        return self.t[k]


def build(S=4096, L=2, dbg=False, stop_after=99):
    NT = S // 128
    nc = bass.Bass("TRN2", target_bir_lowering=False)
    P = Prog(nc)

    def din(name, shape, dt=F32):
        return nc.dram_tensor(name, list(shape), dt, kind="ExternalInput").ap()

    def dres(name):
        r = Res(name)
        r.dram = True
        return r

    def dscr(name, shape, dt):
        return nc.dram_tensor(name, list(shape), dt, kind=("ExternalOutput" if dbg else "Internal")).ap(), dres(name)

    xT = din("xT", [8, 128, S]); r_xT = dres("xT")
    memT = din("memT", [8, 128, 256])
    lng = din("lng", [L, 4, 128, 8]); lnb = din("lnb", [L, 4, 128, 8])
    w_in = din("ffn_w_in", [L, 2, D, 2 * DFF]); w_out = din("ffn_w_out", [L, 2, DFF, D])
    w_mix = din("w_mix_in", [L, D, PMIX])
    bgate = din("bgate", [L, 3, 128, 8])
    kvg = din("kvg", [L, 128])
    wuk = din("wuk", [L, 128, 4, 128])
    wuv = din("wuv", [L, 8, 128, 64])
    convw = din("convw", [L, 128, 4, 3])
    wbr = din("w_branch", [L, 3, 512, D])
    wmo = din("w_mix_out", [L, D, D])
    xwq = din("xa_wq", [L, D, 512]); xwkv = din("xa_wkv", [L, D, D]); xwo = din("xa_wo", [L, 512, D])
    biasT = din("biasT", [128, 8, 256])
    rb31 = din("rb31", [8])
    outT = nc.dram_tensor("outT", [8, 128, S], F32, kind="ExternalOutput").ap(); r_outT = dres("outT")
    r_const = dres("const_in")

    XA, r_XA = dscr("XA", [8, 128, S], F32)
    XB, r_XB = dscr("XB", [8, 128, S], F32)
    QL, r_QL = dscr("QL", [8, 128, S], BF16)
    IQT, r_IQT = dscr("IQT", [4, 128, S], BF16)
    IKT, r_IKT = dscr("IKT", [64, S], BF16)
    QST, r_QST = dscr("QST", [4, 128, S], BF16)
    KST, r_KST = dscr("KST", [4, 128, S], BF16)
    VS, r_VS = dscr("VS", [S, 512], BF16)
    CKV, r_CKV = dscr("CKV", [S, 128], BF16)
    CKVT, r_CKVT = dscr("CKVT", [128, S], BF16)
    IW, r_IW = dscr("IW", [S, 8], F32)
    YCT, r_YCT = dscr("YCT", [4, 128, S], BF16)
    YAT, r_YAT = dscr("YAT", [4, 128, S], BF16)
    YBT, r_YBT = dscr("YBT", [4, 128, S], BF16)

    def mm(out, lhsT, rhs, start, stop, reads, writes, inc=None):
        P.op("pe", lambda e: e.matmul(out, lhsT=lhsT, rhs=rhs, start=start, stop=stop),
             reads, writes, inc=(stop if inc is None else inc))

    def tr(out, in_, ident, reads, writes, inc=True):
        P.op("pe", lambda e: e.transpose(out=out, in_=in_, identity=ident), reads, writes, inc=inc)

    def act(out, in_, func, reads, writes, bias=None, scale=None, accum=None):
        kw = {}
        if bias is not None:
            kw["bias"] = bias
        if scale is not None:
            kw["scale"] = scale
        if accum is not None:
            kw["accum_out"] = accum
        P.op("act", lambda e: e.activation(out=out, in_=in_, func=func, **kw), reads, writes)

    def tt(eng, out, a, b, op, reads, writes):
        P.op(eng, lambda e: e.tensor_tensor(out=out, in0=a, in1=b, op=op), reads, writes)

    def ts(eng, out, a, s1, op0, reads, writes, s2=None, op1=None, accum=None):
        kw = {}
        if op1 is not None:
            kw["op1"] = op1
        if accum is not None:
            kw["accum_out"] = accum
        P.op(eng, lambda e: e.tensor_scalar(out=out, in0=a, scalar1=s1, scalar2=s2, op0=op0, **kw), reads, writes)

    def stt(out, in0, scalar, in1, op0, op1, reads, writes, accum=None):
        kw = {}
        if accum is not None:
            kw["accum_out"] = accum
        P.op("dve", lambda e: e.scalar_tensor_tensor(out=out, in0=in0, scalar=scalar, in1=in1, op0=op0, op1=op1, **kw),
             reads, writes)

    def cp(eng, out, in_, reads, writes):
        if eng == "act":
            P.op("act", lambda e: e.copy(out=out, in_=in_), reads, writes)
        else:
            P.op(eng, lambda e: e.tensor_copy(out=out, in_=in_), reads, writes)

    def red(out, in_, op, reads, writes):
        P.op("dve", lambda e: e.tensor_reduce(out=out, in_=in_, axis=AX.X, op=op), reads, writes)

    def recip(out, in_, reads, writes):
        P.op("dve", lambda e: e.reciprocal(out=out, in_=in_), reads, writes)

    def memset(eng, out, val, writes):
        P.op(eng, lambda e: e.memset(out, val), (), writes)

    def dma(q, out, in_, reads, writes, **kw):
        reads = [r for r in reads if not r.dram]
        writes = [r for r in writes if not r.dram]
        P.dma(q, lambda e: e.dma_start(out=out, in_=in_, **kw), reads, writes)

    def wload(dst, k_chunks, src2d, c0, c1, rows0=0):
        dst.rl = [Res("%s_k%d" % (dst.r.name, k)) for k in range(k_chunks)]
        for k in range(k_chunks):
            dma("pool", dst.t[:, k, 0:c1 - c0], src2d[rows0 + k * 128: rows0 + (k + 1) * 128, c0:c1],
                [r_const] + ([dst.rl[k - 1]] if k > 0 else []), [dst.rl[k]], max_dma_last_dim=8192)

    top = ExitStack()

    uid = [0]

    def alloc(stack, name, shape, dt):
        uid[0] += 1
        name = "%s_%d" % (name, uid[0])
        return Tl(stack.enter_context(nc.sbuf_tensor(name, list(shape), dt)), name)

    PS = [Tl(top.enter_context(nc.psum_tensor("ps%d" % i, [128, 512], F32)), "ps%d" % i, True) for i in range(7)]
    PB = Tl(top.enter_context(nc.psum_tensor("pb", [128, 1024], BF16)), "pb", True)

    identf = alloc(top, "identf", [128, 128], F32)
    ident = alloc(top, "ident", [128, 128], BF16)
    onesM = alloc(top, "onesM", [128, 128], F32)
    onesB = alloc(top, "onesB", [128, 128], BF16)
    caus = alloc(top, "caus", [128, 128], F32)
    strict = alloc(top, "strict", [128, 128], F32)
    pow2 = alloc(top, "pow2", [128, NIT], F32)
    neglo = alloc(top, "neglo", [128, 1], F32)
    epsT = alloc(top, "epsT", [128, 1], F32)
    oneT = alloc(top, "oneT", [128, 1], F32)
    memset("pool", identf.t[:], 0.0, [identf.r])
    P.op("pool", lambda e: e.affine_select(out=identf.t[:], in_=identf.t[:], pattern=[[-1, 128]],
                                           compare_op=ALU.not_equal, fill=1.0, base=0, channel_multiplier=1),
         [identf.r], [identf.r])
    cp("dve", ident.t[:], identf.t[:], [identf.r], [ident.r])
    memset("pool", onesM.t[:], 1.0 / D, [onesM.r])
    memset("pool", onesB.t[:], 1.0, [onesB.r])
    memset("pool", caus.t[:], 0.0, [caus.r])
    P.op("pool", lambda e: e.affine_select(out=caus.t[:], in_=caus.t[:], pattern=[[-1, 128]],
                                           compare_op=ALU.is_ge, fill=NEG, base=0, channel_multiplier=1),
         [caus.r], [caus.r])
    memset("pool", strict.t[:], 1.0, [strict.r])
    P.op("pool", lambda e: e.affine_select(out=strict.t[:], in_=strict.t[:], pattern=[[-1, 128]],
                                           compare_op=ALU.is_gt, fill=0.0, base=0, channel_multiplier=1),
         [strict.r], [strict.r])
    for j in range(NIT):
        memset("pool", pow2.t[:, j:j + 1], 0.5 ** (j + 1), [pow2.r])
    memset("pool", neglo.t[:], -1.0e29, [neglo.r])
    memset("pool", epsT.t[:], EPS, [epsT.r])
    memset("pool", oneT.t[:], 1.0, [oneT.r])

    def ln_part1(Z, SQ, G, pa):
        for c in range(8):
            mm(pa.t[:, 0:G], onesM.t[:], Z.t[:, c, :], c == 0, c == 7, [onesM.r, Z.r], [pa.r])
        for c in range(8):
            tt("dve", Z.t[:, c, :], Z.t[:, c, :], pa.t[:, 0:G], ALU.subtract, [Z.r, pa.r], [Z.r])
        act(SQ.t[:], Z.t[:], AF.Square, [Z.r], [SQ.r])

    def ln_part2(Z, SQ, RS, G, gT, bT, dst, r_dst, g0, pb_):
        for c in range(8):
            mm(pb_.t[:, 0:G], onesM.t[:], SQ.t[:, c, :], c == 0, c == 7, [onesM.r, SQ.r], [pb_.r])
        act(RS.t[:], pb_.t[:, 0:G], AF.Sqrt, [pb_.r, epsT.r], [RS.r], bias=epsT.t[:, 0:1])
        recip(RS.t[:], RS.t[:], [RS.r], [RS.r])
        for c in range(8):
            tt("pool", Z.t[:, c, :], Z.t[:, c, :], RS.t[:], ALU.mult, [Z.r, RS.r], [Z.r])
        for c in range(8):
            act(SQ.t[:, c, :], Z.t[:, c, :], AF.Identity, [Z.r, gT.r, bT.r], [SQ.r],
                scale=gT.t[:, c:c + 1], bias=bT.t[:, c:c + 1])
        dma("sp", dst[:, :, g0:g0 + G].rearrange("c p t -> p c t"), SQ.t[:], [SQ.r], [r_dst])

    def layer_norm(Z, SQ, RS, G, gT, bT, dst, r_dst, g0, pa, pb_):
        ln_part1(Z, SQ, G, pa)
        ln_part2(Z, SQ, RS, G, gT, bT, dst, r_dst, g0, pb_)

    def ffn_phase(l, which, ln_i, src, r_src, dst, r_dst):
        G = 256
        NG = S // G
        with ExitStack() as st:
            Win = alloc(st, "Win", [128, 8, 2 * DFF], BF16)
            Wout = alloc(st, "Wout", [128, 22, D], BF16)
            gT = alloc(st, "gT", [128, 8], F32); bT = alloc(st, "bT", [128, 8], F32)
            X32 = [alloc(st, "X32%d" % b, [128, 8, G], F32) for b in range(2)]
            Xb = [alloc(st, "Xb%d" % b, [128, 8, G], BF16) for b in range(2)]
            H = alloc(st, "H", [128, 22, G], BF16)
            SA = [alloc(st, "SA%d" % i, [128, G], F32) for i in range(2)]
            Z = [alloc(st, "Z%d" % b, [128, 8, G], F32) for b in range(2)]
            SQ = alloc(st, "SQ", [128, 8, G], F32)
            RS = alloc(st, "RS", [128, G], F32)
            dma("sp", gT.t[:], lng[l, ln_i], [r_const], [gT.r])
            dma("sp", bT.t[:], lnb[l, ln_i], [r_const], [bT.r])
            wload(Win, 8, w_in[l, which], 0, 2 * DFF)
            wload(Wout, 22, w_out[l, which], 0, D)

            def load(g):
                b = g % 2
                g0 = g * G
                dma("sp", X32[b].t[:], src[:, :, g0:g0 + G].rearrange("c p t -> p c t"), [r_src], [X32[b].r])
                cp("pool", Xb[b].t[:], X32[b].t[:], [X32[b].r], [Xb[b].r])

            def inproj(g):
                xb = Xb[g % 2]
                for j in range(22):
                    pa = PS[(2 * j) % 4]; pb_ = PS[(2 * j + 1) % 4]
                    for k in range(8):
                        mm(pa.t[:, 0:G], Win.t[:, k, j * 128:(j + 1) * 128], xb.t[:, k, :], k == 0, k == 7,
                           Win.rl + [xb.r], [pa.r])
                    for k in range(8):
                        mm(pb_.t[:, 0:G], Win.t[:, k, DFF + j * 128:DFF + (j + 1) * 128], xb.t[:, k, :], k == 0, k == 7,
                           Win.rl + [xb.r], [pb_.r])
                    sa = SA[j % 2]
                    act(sa.t[:], pa.t[:, 0:G], AF.Silu, [pa.r], [sa.r])
                    stt(H.t[:, j, :], sa.t[:], 0.5, pb_.t[:, 0:G], ALU.mult, ALU.mult, [sa.r, pb_.r], [H.r])

            def outproj(g):
                x32, z = X32[g % 2], Z[g % 2]
                for c in range(8):
                    po = PS[4 + (c % 2)]
                    for j in range(22):
                        mm(po.t[:, 0:G], Wout.t[:, j, c * 128:(c + 1) * 128], H.t[:, j, :], j == 0, j == 21,
                           Wout.rl + [H.r], [po.r])
                    stt(z.t[:, c, :], x32.t[:, c, :], ALPHA, po.t[:, 0:G], ALU.mult, ALU.add, [x32.r, po.r], [z.r])

            load(0)
            if NG > 1:
                load(1)
            inproj(0)
            outproj(0)
            for g in range(NG):
                if g + 2 < NG:
                    load(g + 2)
                if g + 1 < NG:
                    inproj(g + 1)
                ln_part1(Z[g % 2], SQ, G, PS[6])
                if g + 1 < NG:
                    outproj(g + 1)
                ln_part2(Z[g % 2], SQ, RS, G, gT, bT, dst, r_dst, g * G, PS[6])
            P.barrier()

    C_QA, C_CKV, C_IQ, C_IK, C_IW, C_QS, C_KS, C_VS, C_CB, C_CC, C_CH, C_G = \
        0, 512, 640, 1152, 1216, 1224, 1736, 2248, 2760, 3272, 3784, 4296

    def proj_phase(l, src, r_src):
        G = 512
        NW = C_G
        with ExitStack() as st:
            Wm = alloc(st, "Wm", [128, 8, NW], BF16)
            WUK = alloc(st, "WUK", [128, 4, 128], BF16)
            CW = alloc(st, "CW", [128, 4, 3], F32)
            KVG = alloc(st, "KVG", [128, 128], F32)
            X32 = alloc(st, "X32", [128, 8, G], F32)
            Xb = alloc(st, "Xb", [128, 8, G], BF16)
            QA = alloc(st, "QA", [128, 4, G], BF16)
            OB = [alloc(st, "OB%d" % i, [128, G], BF16) for i in range(3)]
            CCs = alloc(st, "CCs", [128, G], F32)
            ZC = [alloc(st, "ZC%d" % q, [128, G + 2], F32) for q in range(4)]
            YC = alloc(st, "YC", [128, G], F32)
            JK = alloc(st, "JK", [128, 128], F32)
            SS = alloc(st, "SS", [128, 2], F32)
            CKb = alloc(st, "CKb", [128, 128], BF16)
            CKTb = alloc(st, "CKTb", [128, 128], BF16)
            IWs = alloc(st, "IWs", [128, 8], F32)
            VSb = alloc(st, "VSb", [128, 512], BF16)
            wload(Wm, 8, w_mix[l], 0, NW)
            dma("pool", WUK.t[:], wuk[l], [r_const], [WUK.r])
            dma("sp", CW.t[:], convw[l], [r_const], [CW.r])
            dma("sp", KVG.t[:], kvg[l].partition_broadcast(128), [r_const], [KVG.r])
            for q in range(4):
                memset("pool", ZC[q].t[:, 0:2], 0.0, [ZC[q].r])
            rot = [0]

            def nextps():
                rot[0] = (rot[0] + 1) % 5
                return PS[rot[0]]
            obr = [0]

            def nextob():
                obr[0] = (obr[0] + 1) % 3
                return OB[obr[0]]

            def fm_chunk(col, M=128):
                ps = nextps()
                for k in range(8):
                    mm(ps.t[0:M, 0:G], Wm.t[:, k, col:col + M], Xb.t[:, k, :], k == 0, k == 7, Wm.rl + [Xb.r], [ps.r])
                return ps

            for g in range(S // G):
                g0 = g * G
                dma("sp", X32.t[:], src[:, :, g0:g0 + G].rearrange("c p t -> p c t"), [r_src], [X32.r])
                cp("pool", Xb.t[:], X32.t[:], [X32.r], [Xb.r])
                for q in range(4):
                    ps = fm_chunk(C_QA + q * 128)
                    cp("act", QA.t[:, q, :], ps.t[:, 0:G], [ps.r], [QA.r])
                for h in range(8):
                    p0 = (h % 2) * 64
                    ps = nextps()
                    mm(ps.t[:, 0:G], WUK.t[p0:p0 + 64, h // 2, :], QA.t[p0:p0 + 64, h // 2, :], True, True,
                       [WUK.r, QA.r], [ps.r])
                    ob = nextob()
                    act(ob.t[:], ps.t[:, 0:G], AF.Copy, [ps.r], [ob.r], scale=0.125)
                    dma("sp", QL[h, :, g0:g0 + G], ob.t[:], [ob.r], [r_QL])
                for (col, dstT, r_d, sc) in ((C_IQ, IQT, r_IQT, 1.0), (C_QS, QST, r_QST, 0.125), (C_KS, KST, r_KST, 1.0)):
                    for q in range(4):
                        ps = fm_chunk(col + q * 128)
                        ob = nextob()
                        act(ob.t[:], ps.t[:, 0:G], AF.Copy, [ps.r], [ob.r], scale=sc)
                        dma("sp", dstT[q, :, g0:g0 + G], ob.t[:], [ob.r], [r_d])
                ps = fm_chunk(C_IK, 64)
                ob = nextob()
                cp("act", ob.t[0:64, :], ps.t[0:64, 0:G], [ps.r], [ob.r])
                dma("sp", IKT[:, g0:g0 + G], ob.t[0:64, :], [ob.r], [r_IKT])
                for q in range(4):
                    ps = fm_chunk(C_CC + q * 128)
                    cp("act", CCs.t[:], ps.t[:, 0:G], [ps.r], [CCs.r])
                    ps = fm_chunk(C_CH + q * 128)
                    zc = ZC[q]
                    tt("dve", zc.t[:, 2:G + 2], CCs.t[:], ps.t[:, 0:G], ALU.mult, [CCs.r, ps.r], [zc.r])
                    ts("dve", YC.t[:], zc.t[:, 2:G + 2], CW.t[:, q, 2:3], ALU.mult, [zc.r, CW.r], [YC.r])
                    stt(YC.t[:], zc.t[:, 1:G + 1], CW.t[:, q, 1:2], YC.t[:], ALU.mult, ALU.add, [zc.r, CW.r, YC.r], [YC.r])
                    stt(YC.t[:], zc.t[:, 0:G], CW.t[:, q, 0:1], YC.t[:], ALU.mult, ALU.add, [zc.r, CW.r, YC.r], [YC.r])
                    ps = fm_chunk(C_CB + q * 128)
                    ob = nextob()
                    tt("dve", ob.t[:], YC.t[:], ps.t[:, 0:G], ALU.mult, [YC.r, ps.r], [ob.r])
                    dma("sp", YCT[q, :, g0:g0 + G], ob.t[:], [ob.r], [r_YCT])
                    cp("pool", zc.t[:, 0:2], zc.t[:, G:G + 2], [zc.r], [zc.r])
                for t4 in range(G // 128):
                    t0 = g0 + t4 * 128
                    xs = slice(t4 * 128, (t4 + 1) * 128)
                    ps = nextps()
                    for k in range(8):
                        mm(ps.t[:, 0:128], Xb.t[:, k, xs], Wm.t[:, k, C_CKV:C_CKV + 128], k == 0, k == 7,
                           Wm.rl + [Xb.r], [ps.r])
                    act(JK.t[:], ps.t[:, 0:128], AF.Square, [ps.r], [JK.r, SS.r], accum=SS.t[:, 0:1])
                    act(SS.t[:, 1:2], SS.t[:, 0:1], AF.Sqrt, [SS.r, epsT.r], [SS.r], scale=1.0 / 128, bias=epsT.t[:, 0:1])
                    recip(SS.t[:, 1:2], SS.t[:, 1:2], [SS.r], [SS.r])
                    stt(CKb.t[:], ps.t[:, 0:128], SS.t[:, 1:2], KVG.t[:], ALU.mult, ALU.mult, [ps.r, SS.r, KVG.r], [CKb.r])
                    dma("sp", CKV[t0:t0 + 128, :], CKb.t[:], [CKb.r], [r_CKV])
                    tr(PB.t[:, 0:128], CKb.t[:], ident.t[:], [CKb.r, ident.r], [PB.r])
                    cp("act", CKTb.t[:], PB.t[:, 0:128], [PB.r], [CKTb.r])
                    dma("sp", CKVT[:, t0:t0 + 128], CKTb.t[:], [CKTb.r], [r_CKVT])
                    ps = nextps()
                    for k in range(8):
                        mm(ps.t[:, 0:8], Xb.t[:, k, xs], Wm.t[:, k, C_IW:C_IW + 8], k == 0, k == 7, Wm.rl + [Xb.r], [ps.r])
                    cp("act", IWs.t[:], ps.t[:, 0:8], [ps.r], [IWs.r])
                    dma("sp", IW[t0:t0 + 128, :], IWs.t[:], [IWs.r], [r_IW])
                    ps = nextps()
                    for k in range(8):
                        mm(ps.t[:, 0:512], Xb.t[:, k, xs], Wm.t[:, k, C_VS:C_VS + 512], k == 0, k == 7, Wm.rl + [Xb.r], [ps.r])
                    cp("act", VSb.t[:], ps.t[:, 0:512], [ps.r], [VSb.r])
                    dma("sp", VS[t0:t0 + 128, :], VSb.t[:], [VSb.r], [r_VS])
            P.barrier()

    def chunks(n):
        return [(c0, min(512, n - c0)) for c0 in range(0, n, 512)]

    def dsa_phase(l):
        with ExitStack() as st:
            CKVX = alloc(st, "CKVX", [128, NT, 128], BF16)
            CKT = alloc(st, "CKT", [128, S], BF16)
            IK2 = alloc(st, "IK2", [128, S], BF16)
            BI = alloc(st, "BI", [128, 8, 256], F32)
            RB = alloc(st, "RB", [128, 8], F32)
            WUVP = alloc(st, "WUVP", [128, 8, 128], BF16)
            QLg = [alloc(st, "QLg%d" % b, [128, 8, 512], BF16) for b in range(2)]
            IQg = [alloc(st, "IQg%d" % b, [128, 4, 512], BF16) for b in range(2)]
            IWg = [alloc(st, "IWg%d" % b, [128, 4, 8], F32) for b in range(2)]
            WA = [alloc(st, "WA%d" % b, [128, 8], F32) for b in range(2)]
            SG = [alloc(st, "SG%d" % b, [128, 8], F32) for b in range(2)]
            SC = [alloc(st, "SC%d" % b, [128, S], F32) for b in range(2)]
            Mk = [alloc(st, "Mk%d" % b, [128, S], BF16) for b in range(2)]
            TH = [alloc(st, "TH%d" % b, [128, 8], F32) for b in range(2)]
            WALL = [alloc(st, "WALL%d" % b, [128, NIT], F32) for b in range(2)]
            PM = [alloc(st, "PM%d" % b, [128, S], BF16) for b in range(2)]
            PT = [alloc(st, "PT%d" % b, [128, NT, 128], BF16) for b in range(2)]
            TMP = [alloc(st, "TMP%d" % i, [128, 512], F32) for i in range(4)]
            JKB = alloc(st, "JKB", [128, S], BF16)
            RSM = alloc(st, "RSM", [128, 8, 8], F32)
            RSUM = alloc(st, "RSUM", [128, 8], F32)
            OLN = alloc(st, "OLN", [128, 8, 128], BF16)
            OLT = alloc(st, "OLT", [128, 8, 128], BF16)
            YAg = alloc(st, "YAg", [128, 4, 512], BF16)
            dma("sp", CKVX.t[:], CKV.rearrange("(j p) c -> p j c", p=128), [r_CKV], [CKVX.r])
            dma("sp", CKT.t[:], CKVT, [r_CKVT], [CKT.r])
            dma("sp", IK2.t[0:64, :], IKT, [r_IKT], [IK2.r])
            dma("sp", IK2.t[64:128, :], IKT, [r_IKT], [IK2.r])
            dma("sp", BI.t[:], biasT, [r_const], [BI.r])
            dma("sp", RB.t[:], rb31.partition_broadcast(128), [r_const], [RB.r])
            memset("pool", WUVP.t[:], 0.0, [WUVP.r])
            for h in range(8):
                p0 = (h % 2) * 64
                dma("pool", WUVP.t[:, h, p0:p0 + 64], wuv[l, h], [r_const], [WUVP.r])
            tmr = [0]

            def nexttmp():
                tmr[0] = (tmr[0] + 1) % 4
                return TMP[tmr[0]]
            psr = [0]

            def nextps():
                psr[0] = (psr[0] + 1) % 3
                return PS[psr[0]]

            def load_group(g):
                b = g % 2
                g0 = g * 512
                dma("sp", QLg[b].t[:], QL[:, :, g0:g0 + 512].rearrange("h c t -> c h t"), [r_QL], [QLg[b].r])
                dma("sp", IQg[b].t[:], IQT[:, :, g0:g0 + 512].rearrange("q p t -> p q t"), [r_IQT], [IQg[b].r])
                dma("sp", IWg[b].t[:], IW[g0:g0 + 512, :].rearrange("(a p) h -> p a h", p=128), [r_IW], [IWg[b].r])

            def index_scores(i):
                g, t4 = divmod(i, 4)
                gb = g % 2
                b = i % 2
                n = (i + 1) * 128
                tsl = slice(t4 * 128, (t4 + 1) * 128)
                sc, th, wall, wa, sg, mk = SC[b], TH[b], WALL[b], WA[b], SG[b], Mk[b]
                act(wa.t[:], IWg[gb].t[:, t4, :], AF.Abs, [IWg[gb].r], [wa.r])
                P.op("act", lambda e: e.sign(out=sg.t[:], in_=IWg[gb].t[:, t4, :]), [IWg[gb].r], [sg.r])
                for (c0, w) in chunks(n):
                    for h in range(8):
                        p0 = (h % 2) * 64
                        ps = nextps()
                        mm(ps.t[:, 0:w], IQg[gb].t[p0:p0 + 64, h // 2, tsl], IK2.t[p0:p0 + 64, c0:c0 + w], True, True,
                           [IQg[gb].r, IK2.r], [ps.r])
                        R = nexttmp()
                        act(R.t[:, 0:w], ps.t[:, 0:w], AF.Relu, [ps.r, wa.r], [R.r], scale=wa.t[:, h:h + 1])
                        if h == 0:
                            ts("dve", sc.t[:, c0:c0 + w], R.t[:, 0:w], sg.t[:, 0:1], ALU.mult, [R.r, sg.r], [sc.r])
                        else:
                            stt(sc.t[:, c0:c0 + w], R.t[:, 0:w], sg.t[:, h:h + 1], sc.t[:, c0:c0 + w], ALU.mult, ALU.add,
                                [R.r, sg.r, sc.r], [sc.r])
                tt("dve", sc.t[:, n - 128:n], sc.t[:, n - 128:n], caus.t[:], ALU.add, [sc.r, caus.r], [sc.r])
                steps = []
                if i >= 2:
                    def init():
                        red(th.t[:, 0:1], sc.t[:, 0:n - 128], ALU.min, [sc.r], [th.r])
                        red(th.t[:, 1:2], sc.t[:, 0:n], ALU.max, [sc.r], [th.r])
                        tt("dve", th.t[:, 2:3], th.t[:, 1:2], th.t[:, 0:1], ALU.subtract, [th.r], [th.r])
                        ts("dve", wall.t[:], pow2.t[:], th.t[:, 2:3], ALU.mult, [pow2.r, th.r], [wall.r])
                    steps.append(init)

                    def mkstep(j):
                        def step():
                            tt("dve", th.t[:, 3:4], th.t[:, 0:1], wall.t[:, j:j + 1], ALU.add, [th.r, wall.r], [th.r])
                            ts("dve", JKB.t[:, 0:n], sc.t[:, 0:n], th.t[:, 3:4], ALU.is_ge, [sc.r, th.r], [th.r],
                               op1=ALU.add, accum=th.t[:, 4:5])
                            ts("dve", th.t[:, 5:6], th.t[:, 4:5], KTOP - 0.5, ALU.is_ge, [th.r, wall.r], [th.r],
                               s2=wall.t[:, j:j + 1], op1=ALU.mult)
                            tt("dve", th.t[:, 0:1], th.t[:, 0:1], th.t[:, 5:6], ALU.add, [th.r], [th.r])
                        return step
                    for j in range(NIT):
                        steps.append(mkstep(j))
                    steps.append(lambda: ts("dve", mk.t[:, 0:n], sc.t[:, 0:n], th.t[:, 0:1], ALU.is_ge, [sc.r, th.r], [mk.r]))
                else:
                    steps.append(lambda: ts("dve", mk.t[:, 0:n], sc.t[:, 0:n], neglo.t[:, 0:1], ALU.is_ge, [sc.r, neglo.r], [mk.r]))
                return steps

            def heads(i, pending):
                g, t4 = divmod(i, 4)
                gb = g % 2
                n = (i + 1) * 128
                tsl = slice(t4 * 128, (t4 + 1) * 128)
                mk = Mk[i % 2]
                nb0 = max(0, n - 256)
                per_head = (len(pending) + 7) // 8
                for h in range(8):
                    pm, pt = PM[h % 2], PT[h % 2]
                    ch = chunks(n)
                    for ci, (c0, w) in enumerate(ch):
                        ps = nextps()
                        mm(ps.t[:, 0:w], QLg[gb].t[:, h, tsl], CKT.t[:, c0:c0 + w], True, True, [QLg[gb].r, CKT.r], [ps.r])
                        Pe = nexttmp()
                        fa, fb = c0, min(c0 + w, nb0)
                        na, nb_ = max(c0, nb0), c0 + w
                        if fb > fa:
                            act(Pe.t[:, fa - c0:fb - c0], ps.t[:, fa - c0:fb - c0], AF.Exp, [ps.r, RB.r], [Pe.r],
                                bias=RB.t[:, h:h + 1])
                        if nb_ > na:
                            bo = 256 - (n - na)
                            tt("dve", Pe.t[:, na - c0:nb_ - c0], ps.t[:, na - c0:nb_ - c0], BI.t[:, h, bo:bo + (nb_ - na)],
                               ALU.add, [ps.r, BI.r], [Pe.r])
                            act(Pe.t[:, na - c0:nb_ - c0], Pe.t[:, na - c0:nb_ - c0], AF.Exp, [Pe.r], [Pe.r])
                        stt(pm.t[:, c0:c0 + w], Pe.t[:, 0:w], 1.0, mk.t[:, c0:c0 + w], ALU.mult, ALU.mult,
                            [Pe.r, mk.r], [pm.r, RSM.r], accum=RSM.t[:, h, ci:ci + 1])
                    red(RSUM.t[:, h:h + 1], RSM.t[:, h, 0:len(ch)], ALU.add, [RSM.r], [RSUM.r])
                    recip(RSUM.t[:, h:h + 1], RSUM.t[:, h:h + 1], [RSUM.r], [RSUM.r])
                    for _ in range(per_head):
                        if pending:
                            pending.pop(0)()
                    for j0 in range(0, i + 1, 8):
                        nbk = min(8, i + 1 - j0)
                        for jj in range(nbk):
                            j = j0 + jj
                            tr(PB.t[:, jj * 128:(jj + 1) * 128], pm.t[:, j * 128:(j + 1) * 128], ident.t[:],
                               [pm.r, ident.r], [PB.r], inc=(jj == nbk - 1))
                        cp("act", pt.t[:, j0:j0 + nbk, :], PB.t[:, 0:nbk * 128].rearrange("p (j t) -> p j t", t=128),
                           [PB.r], [pt.r])
                    ol = PS[3 + (h % 2)]
                    for j in range(i + 1):
                        mm(ol.t[:, 0:128], pt.t[:, j, :], CKVX.t[:, j, :], j == 0, j == i, [pt.r, CKVX.r], [ol.r])
                    act(OLN.t[:, h, :], ol.t[:, 0:128], AF.Copy, [ol.r, RSUM.r], [OLN.r], scale=RSUM.t[:, h:h + 1])
                while pending:
                    pending.pop(0)()
                for h in range(8):
                    tr(PB.t[:, h * 128:(h + 1) * 128], OLN.t[:, h, :], ident.t[:], [OLN.r, ident.r], [PB.r], inc=(h == 7))
                cp("act", OLT.t[:], PB.t[:].rearrange("p (j t) -> p j t", t=128), [PB.r], [OLT.r])
                for q in range(4):
                    ps = PS[5]
                    mm(ps.t[:, 0:128], WUVP.t[:, 2 * q, :], OLT.t[:, 2 * q, :], True, False, [WUVP.r, OLT.r], [ps.r])
                    mm(ps.t[:, 0:128], WUVP.t[:, 2 * q + 1, :], OLT.t[:, 2 * q + 1, :], False, True, [WUVP.r, OLT.r], [ps.r])
                    cp("act", YAg.t[:, q, tsl], ps.t[:, 0:128], [ps.r], [YAg.r])
                if t4 == 3:
                    g0 = g * 512
                    dma("sp", YAT[:, :, g0:g0 + 512].rearrange("q p t -> p q t"), YAg.t[:], [YAg.r], [r_YAT])

            load_group(0)
            for s_ in index_scores(0):
                s_()
            for i in range(NT):
                pending = []
                if i + 1 < NT:
                    if (i + 1) % 4 == 0:
                        load_group((i + 1) // 4)
                    pending = index_scores(i + 1)
                heads(i, pending)
            P.barrier()

    def sb_phase(l):
        with ExitStack() as st:
            KS_ = alloc(st, "KS_", [128, 4, S], BF16)
            VSX = alloc(st, "VSX", [128, NT, 512], BF16)
            QSg = [alloc(st, "QSg%d" % b, [128, 4, 512], BF16) for b in range(2)]
            LB = [alloc(st, "LB%d" % b, [128, S + 1], F32) for b in range(2)]
            NLX = [alloc(st, "NLX%d" % b, [128, S], F32) for b in range(2)]
            AB = [alloc(st, "AB%d" % b, [128, S], BF16) for b in range(2)]
            PT = [alloc(st, "PT2%d" % b, [128, NT, 128], BF16) for b in range(2)]
            TMP = [alloc(st, "TMQ%d" % i, [128, 512], F32) for i in range(4)]
            TSM = [alloc(st, "TSM%d" % b, [128, 16], F32) for b in range(2)]
            TOT = [alloc(st, "TOT%d" % b, [128, 2], F32) for b in range(2)]
            YBs = alloc(st, "YBs", [128, 512], BF16)
            YBg = alloc(st, "YBg", [128, 4, 512], BF16)
            dma("sp", KS_.t[:], KST.rearrange("q p t -> p q t"), [r_KST], [KS_.r])
            dma("sp", VSX.t[:], VS.rearrange("(j p) c -> p j c", p=128), [r_VS], [VSX.r])
            for b in range(2):
                memset("pool", LB[b].t[:, 0:1], 0.0, [LB[b].r])
            tmr = [0]

            def nexttmp():
                tmr[0] = (tmr[0] + 1) % 4
                return TMP[tmr[0]]
            psr = [0]

            def nextps():
                psr[0] = (psr[0] + 1) % 4
                return PS[psr[0]]
            yb = PS[4]

            def pass1(i, h):
                g, t4 = divmod(i, 4)
                qs = QSg[g % 2]
                n = (i + 1) * 128
                tsl = slice(t4 * 128, (t4 + 1) * 128)
                p0 = (h % 2) * 64
                lb, tsm, tot = LB[h % 2], TSM[h % 2], TOT[h % 2]
                memset("dve", tsm.t[:], 0.0, [tsm.r])
                for ci, (c0, w) in enumerate(chunks(n)):
                    ps = nextps()
                    mm(ps.t[:, 0:w], qs.t[p0:p0 + 64, h // 2, tsl], KS_.t[p0:p0 + 64, h // 2, c0:c0 + w], True, True,
                       [qs.r, KS_.r], [ps.r])
                    E1 = nexttmp()
                    act(E1.t[:, 0:w], ps.t[:, 0:w], AF.Exp, [ps.r], [E1.r])
                    last = (c0 + w == n)
                    wf = w - 128 if last else w
                    if wf > 0:
                        act(lb.t[:, 1 + c0:1 + c0 + wf], E1.t[:, 0:wf], AF.Ln, [E1.r, oneT.r], [lb.r, tsm.r],
                            bias=oneT.t[:, 0:1], accum=tsm.t[:, ci:ci + 1])
                    if last:
                        act(lb.t[:, 1 + n - 128:1 + n], E1.t[:, w - 128:w], AF.Ln, [E1.r, oneT.r], [lb.r], bias=oneT.t[:, 0:1])
                        tt("dve", lb.t[:, 1 + n - 128:1 + n], lb.t[:, 1 + n - 128:1 + n], strict.t[:], ALU.mult,
                           [lb.r, strict.r], [lb.r])
                        red(tsm.t[:, 15:16], lb.t[:, 1 + n - 128:1 + n], ALU.add, [lb.r], [tsm.r])
                red(tot.t[:, 0:1], tsm.t[:], ALU.add, [tsm.r], [tot.r])
                ts("dve", tot.t[:, 1:2], tot.t[:, 0:1], -1.0, ALU.mult, [tot.r], [tot.r])

            def pass2(i, h):
                g, t4 = divmod(i, 4)
                qs = QSg[g % 2]
                n = (i + 1) * 128
                tsl = slice(t4 * 128, (t4 + 1) * 128)
                p0 = (h % 2) * 64
                lb, tot, nlx, ab, pt = LB[h % 2], TOT[h % 2], NLX[h % 2], AB[h % 2], PT[h % 2]
                P.op("dve", lambda e: e.tensor_tensor_scan(out=nlx.t[:, 0:n], data0=lb.t[:, 0:n], data1=lb.t[:, 0:n],
                                                           initial=tot.t[:, 1:2], op0=ALU.add, op1=ALU.min),
                     [lb.r, tot.r], [nlx.r])
                for ci, (c0, w) in enumerate(chunks(n)):
                    ps = nextps()
                    mm(ps.t[:, 0:w], qs.t[p0:p0 + 64, h // 2, tsl], KS_.t[p0:p0 + 64, h // 2, c0:c0 + w], True, True,
                       [qs.r, KS_.r], [ps.r])
                    EB = nexttmp()
                    tt("dve", EB.t[:, 0:w], ps.t[:, 0:w], nlx.t[:, c0:c0 + w], ALU.add, [ps.r, nlx.r], [EB.r])
                    act(ab.t[:, c0:c0 + w], EB.t[:, 0:w], AF.Exp, [EB.r], [ab.r])
                tt("dve", ab.t[:, n - 128:n], ab.t[:, n - 128:n], strict.t[:], ALU.mult, [ab.r, strict.r], [ab.r])
                for j0 in range(0, i + 1, 8):
                    nbk = min(8, i + 1 - j0)
                    for jj in range(nbk):
                        j = j0 + jj
                        tr(PB.t[:, jj * 128:(jj + 1) * 128], ab.t[:, j * 128:(j + 1) * 128], ident.t[:],
                           [ab.r, ident.r], [PB.r], inc=(jj == nbk - 1))
                    cp("act", pt.t[:, j0:j0 + nbk, :], PB.t[:, 0:nbk * 128].rearrange("p (j t) -> p j t", t=128),
                       [PB.r], [pt.r])
                for j in range(i + 1):
                    mm(yb.t[:, h * 64:(h + 1) * 64], pt.t[:, j, :], VSX.t[:, j, h * 64:(h + 1) * 64], j == 0, j == i,
                       [pt.r, VSX.r], [yb.r])

            def load_group(g):
                g0 = g * 512
                dma("sp", QSg[g % 2].t[:], QST[:, :, g0:g0 + 512].rearrange("q p t -> p q t"), [r_QST], [QSg[g % 2].r])

            load_group(0)
            pass1(0, 0)
            for i in range(NT):
                g, t4 = divmod(i, 4)
                tsl = slice(t4 * 128, (t4 + 1) * 128)
                for h in range(8):
                    if h < 7:
                        pass1(i, h + 1)
                    elif i + 1 < NT:
                        if (i + 1) % 4 == 0:
                            load_group((i + 1) // 4)
                        pass1(i + 1, 0)
                    pass2(i, h)
                cp("act", YBs.t[:], yb.t[:, 0:512], [yb.r], [YBs.r])
                for q in range(4):
                    tr(PB.t[:, q * 128:(q + 1) * 128], YBs.t[:, q * 128:(q + 1) * 128], ident.t[:], [YBs.r, ident.r], [PB.r], inc=(q == 3))
                cp("act", YBg.t[:, :, tsl], PB.t[:, 0:512].rearrange("p (q t) -> p q t", t=128), [PB.r], [YBg.r])
                if t4 == 3:
                    g0 = g * 512
                    dma("sp", YBT[:, :, g0:g0 + 512].rearrange("q p t -> p q t"), YBg.t[:], [YBg.r], [r_YBT])
            P.barrier()

    def merge_phase(l, src, r_src, dst, r_dst):
        G = 256
        NG = S // G
        with ExitStack() as st:
            WG = alloc(st, "WG", [128, 8, 3 * D], BF16)
            WBR = alloc(st, "WBR", [128, 12, D], BF16)
            WO = alloc(st, "WO", [128, 8, D], BF16)
            BG = alloc(st, "BG", [128, 3, 8], F32)
            gT = alloc(st, "gT", [128, 8], F32); bT = alloc(st, "bT", [128, 8], F32)
            X32 = [alloc(st, "X32%d" % b, [128, 8, G], F32) for b in range(2)]
            Xb = [alloc(st, "Xb%d" % b, [128, 8, G], BF16) for b in range(2)]
            Y = [[alloc(st, "Y%d_%d" % (i, b), [128, 4, G], BF16) for i in range(3)] for b in range(2)]
            SGt = [alloc(st, "SGt%d" % i, [128, G], F32) for i in range(2)]
            TM = [alloc(st, "TM%d" % i, [128, G], F32) for i in range(2)]
            MGf = alloc(st, "MGf", [128, 8, G], F32)
            MGb = alloc(st, "MGb", [128, 8, G], BF16)
            Z = [alloc(st, "Z%d" % b, [128, 8, G], F32) for b in range(2)]
            SQ = alloc(st, "SQ", [128, 8, G], F32)
            RS = alloc(st, "RS", [128, G], F32)
            dma("sp", gT.t[:], lng[l, 1], [r_const], [gT.r])
            dma("sp", bT.t[:], lnb[l, 1], [r_const], [bT.r])
            dma("sp", BG.t[:], bgate[l].rearrange("i p c -> p i c"), [r_const], [BG.r])
            wload(WG, 8, w_mix[l], C_G, PMIX)
            for i in range(3):
                for k in range(4):
                    dma("pool", WBR.t[:, i * 4 + k, :], wbr[l, i, k * 128:(k + 1) * 128, :], [r_const], [WBR.r], max_dma_last_dim=8192)
            wload(WO, 8, wmo[l], 0, D)
            srcs = ((YAT, r_YAT), (YBT, r_YBT), (YCT, r_YCT))

            def load(g):
                b = g % 2
                g0 = g * G
                dma("sp", X32[b].t[:], src[:, :, g0:g0 + G].rearrange("c p t -> p c t"), [r_src], [X32[b].r])
                cp("pool", Xb[b].t[:], X32[b].t[:], [X32[b].r], [Xb[b].r])
                for i in range(3):
                    dma("sp", Y[b][i].t[:], srcs[i][0][:, :, g0:g0 + G].rearrange("q p t -> p q t"), [srcs[i][1]], [Y[b][i].r])

            def stage1(g):
                xb, y = Xb[g % 2], Y[g % 2]
                n_ = 0
                for c in range(8):
                    for i in range(3):
                        pg = PS[n_ % 2]; pb_ = PS[2 + n_ % 2]; sg = SGt[n_ % 2]; tm = TM[n_ % 2]
                        n_ += 1
                        col = i * D + c * 128
                        for k in range(8):
                            mm(pg.t[:, 0:G], WG.t[:, k, col:col + 128], xb.t[:, k, :], k == 0, k == 7, WG.rl + [xb.r], [pg.r])
                        act(sg.t[:], pg.t[:, 0:G], AF.Sigmoid, [pg.r, BG.r], [sg.r], bias=BG.t[:, i, c:c + 1])
                        for k in range(4):
                            mm(pb_.t[:, 0:G], WBR.t[:, i * 4 + k, c * 128:(c + 1) * 128], y[i].t[:, k, :], k == 0, k == 3,
                               [WBR.r, y[i].r], [pb_.r])
                        if i == 0:
                            tt("dve", MGf.t[:, c, :], sg.t[:], pb_.t[:, 0:G], ALU.mult, [sg.r, pb_.r], [MGf.r])
                        else:
                            tt("dve", tm.t[:], sg.t[:], pb_.t[:, 0:G], ALU.mult, [sg.r, pb_.r], [tm.r])
                            tt("pool", MGf.t[:, c, :], MGf.t[:, c, :], tm.t[:], ALU.add, [MGf.r, tm.r], [MGf.r])
                cp("pool", MGb.t[:], MGf.t[:], [MGf.r], [MGb.r])

            def stage2(g):
                x32, z = X32[g % 2], Z[g % 2]
                for c in range(8):
                    po = PS[4 + (c % 2)]
                    for k in range(8):
                        mm(po.t[:, 0:G], WO.t[:, k, c * 128:(c + 1) * 128], MGb.t[:, k, :], k == 0, k == 7, WO.rl + [MGb.r], [po.r])
                    stt(z.t[:, c, :], x32.t[:, c, :], ALPHA, po.t[:, 0:G], ALU.mult, ALU.add, [x32.r, po.r], [z.r])

            load(0)
            if NG > 1:
                load(1)
            stage1(0)
            stage2(0)
            for g in range(NG):
                if g + 2 < NG:
                    load(g + 2)
                if g + 1 < NG:
                    stage1(g + 1)
                ln_part1(Z[g % 2], SQ, G, PS[6])
                if g + 1 < NG:
                    stage2(g + 1)
                ln_part2(Z[g % 2], SQ, RS, G, gT, bT, dst, r_dst, g * G, PS[6])
            P.barrier()

    def xattn_phase(l, src, r_src, dst, r_dst):
        G = 256
        with ExitStack() as st:
            WQ = alloc(st, "WQ", [128, 8, 512], BF16)
            WKV = alloc(st, "WKV", [128, 8, D], BF16)
            WO = alloc(st, "WOx", [128, 4, D], BF16)
            gT = alloc(st, "gT", [128, 8], F32); bT = alloc(st, "bT", [128, 8], F32)
            MT = alloc(st, "MT", [128, 8, 256], BF16)
            KT = alloc(st, "KT", [128, 4, 256], BF16)
            VX = alloc(st, "VX", [128, 2, 512], BF16)
            X32 = [alloc(st, "X32%d" % b_, [128, 8, G], F32) for b_ in range(2)]
            Xb = [alloc(st, "Xb%d" % b_, [128, 8, G], BF16) for b_ in range(2)]
            QT = alloc(st, "QT", [128, 4, G], BF16)
            PTm = [alloc(st, "PTm%d" % i, [128, G], BF16) for i in range(2)]
            RD = alloc(st, "RD", [128, G], F32)
            OT = alloc(st, "OT", [128, 4, G], BF16)
            Z = [alloc(st, "Z%d" % b_, [128, 8, G], F32) for b_ in range(2)]
            SQ = alloc(st, "SQ", [128, 8, G], F32)
            RS = alloc(st, "RS", [128, G], F32)
            dma("sp", gT.t[:], lng[l, 2], [r_const], [gT.r])
            dma("sp", bT.t[:], lnb[l, 2], [r_const], [bT.r])
            wload(WQ, 8, xwq[l], 0, 512)
            wload(WKV, 8, xwkv[l], 0, D)
            wload(WO, 4, xwo[l], 0, D)
            dma("pool", MT.t[:], memT.rearrange("c p m -> p c m"), [r_const], [MT.r])
            for h in range(4):
                ps = PS[h % 2]
                for k in range(8):
                    mm(ps.t[:, 0:256], WKV.t[:, k, h * 128:(h + 1) * 128], MT.t[:, k, :], k == 0, k == 7, WKV.rl + [MT.r], [ps.r])
                cp("act", KT.t[:, h, :], ps.t[:, 0:256], [ps.r], [KT.r])
            for mt in range(2):
                ps = PS[2 + mt]
                for k in range(8):
                    mm(ps.t[:, 0:512], MT.t[:, k, mt * 128:(mt + 1) * 128], WKV.t[:, k, 512:1024], k == 0, k == 7, WKV.rl + [MT.r], [ps.r])
                cp("act", VX.t[:, mt, :], ps.t[:, 0:512], [ps.r], [VX.r])
            def load(g):
                b_ = g % 2
                g0 = g * G
                dma("sp", X32[b_].t[:], src[:, :, g0:g0 + G].rearrange("c p t -> p c t"), [r_src], [X32[b_].r])
                cp("pool", Xb[b_].t[:], X32[b_].t[:], [X32[b_].r], [Xb[b_].r])

            def attn(g):
                xb = Xb[g % 2]
                for h in range(4):
                    ps = PS[h % 2]
                    for k in range(8):
                        mm(ps.t[:, 0:G], WQ.t[:, k, h * 128:(h + 1) * 128], xb.t[:, k, :], k == 0, k == 7, WQ.rl + [xb.r], [ps.r])
                    act(QT.t[:, h, :], ps.t[:, 0:G], AF.Copy, [ps.r], [QT.r], scale=128.0 ** -0.5)
                for h in range(4):
                    for mt in range(2):
                        ps = PS[mt]
                        mm(ps.t[:, 0:G], KT.t[:, h, mt * 128:(mt + 1) * 128], QT.t[:, h, :], True, True, [KT.r, QT.r], [ps.r])
                        act(PTm[mt].t[:], ps.t[:, 0:G], AF.Exp, [ps.r], [PTm[mt].r])
                    po = PS[2]; pd = PS[3]
                    for mt in range(2):
                        mm(po.t[:, 0:G], VX.t[:, mt, h * 128:(h + 1) * 128], PTm[mt].t[:], mt == 0, mt == 1, [VX.r, PTm[mt].r], [po.r])
                    for mt in range(2):
                        mm(pd.t[:, 0:G], onesB.t[:], PTm[mt].t[:], mt == 0, mt == 1, [onesB.r, PTm[mt].r], [pd.r])
                    recip(RD.t[:], pd.t[:, 0:G], [pd.r], [RD.r])
                    tt("dve", OT.t[:, h, :], po.t[:, 0:G], RD.t[:], ALU.mult, [po.r, RD.r], [OT.r])

            def outp(g):
                x32, z = X32[g % 2], Z[g % 2]
                for c in range(8):
                    po = PS[4 + (c % 2)]
                    for k in range(4):
                        mm(po.t[:, 0:G], WO.t[:, k, c * 128:(c + 1) * 128], OT.t[:, k, :], k == 0, k == 3, WO.rl + [OT.r], [po.r])
                    stt(z.t[:, c, :], x32.t[:, c, :], ALPHA, po.t[:, 0:G], ALU.mult, ALU.add, [x32.r, po.r], [z.r])

            NG = S // G
            load(0)
            if NG > 1:
                load(1)
            attn(0)
            outp(0)
            for g in range(NG):
                if g + 2 < NG:
                    load(g + 2)
                if g + 1 < NG:
                    attn(g + 1)
                ln_part1(Z[g % 2], SQ, G, PS[6])
                if g + 1 < NG:
                    outp(g + 1)
                ln_part2(Z[g % 2], SQ, RS, G, gT, bT, dst, r_dst, g * G, PS[6])
            P.barrier()

    P.barrier()
    cur, r_cur = xT, r_xT
    nph = [0]

    def run(f, *a):
        if nph[0] < stop_after:
            f(*a)
        nph[0] += 1
    for l in range(L):
        last = (l == L - 1)
        run(ffn_phase, l, 0, 0, cur, r_cur, XB, r_XB)
        run(proj_phase, l, XB, r_XB)
        run(dsa_phase, l)
        run(sb_phase, l)
        run(merge_phase, l, XB, r_XB, XA, r_XA)
        run(xattn_phase, l, XA, r_XA, XB, r_XB)
        if last:
            run(ffn_phase, l, 1, 3, XB, r_XB, outT, r_outT)
        else:
            run(ffn_phase, l, 1, 3, XB, r_XB, XA, r_XA)
        cur, r_cur = XA, r_XA
    P.emit()
    top.close()
    return nc, P


def _bucket_table():
    import math
    n = np.arange(256)
    nf = np.maximum(n, 1).astype(np.float32)
    large = 16 + (np.log(nf / np.float32(16)) / np.float32(math.log(8.0)) * np.float32(16)).astype(np.int32)
    large = np.minimum(large, 31)
    return np.where(n < 16, n, large)


def prep_inputs(x, mem, ln_g, ln_b, ffn_w_in, ffn_w_out, w_mix_in, b_gate, kv_norm_g, w_uk, w_uv,
                conv_w, w_branch, w_mix_out, xa_wq, xa_wkv, xa_wo, rel_bias):
    f = lambda a: np.ascontiguousarray(np.asarray(a, dtype=np.float32))
    B, S, _ = x.shape
    L = ln_g.shape[0]
    bt = _bucket_table()
    tl = np.arange(128)[:, None]
    col = np.arange(256)[None, :]
    nrel = np.where(col < 128, tl - col + 128, tl - (col - 128))
    idx = bt[np.clip(nrel, 0, 255)]
    rel_bias = f(rel_bias)
    biasT = f(np.transpose(rel_bias[idx], (0, 2, 1)))
    shared = {
        "lng": f(np.transpose(f(ln_g).reshape(L, 4, 8, 128), (0, 1, 3, 2))),
        "lnb": f(np.transpose(f(ln_b).reshape(L, 4, 8, 128), (0, 1, 3, 2))),
        "ffn_w_in": f(ffn_w_in), "ffn_w_out": f(ffn_w_out), "w_mix_in": f(w_mix_in),
        "bgate": f(np.transpose(f(b_gate).reshape(L, 3, 8, 128), (0, 1, 3, 2))),
        "kvg": f(kv_norm_g),
        "wuk": f(np.transpose(f(w_uk).reshape(L, 4, 2, 64, 128), (0, 2, 3, 1, 4)).reshape(L, 128, 4, 128)),
        "wuv": f(w_uv),
        "convw": f(np.transpose(f(conv_w).reshape(L, 3, 4, 128), (0, 3, 2, 1))),
        "w_branch": f(w_branch), "w_mix_out": f(w_mix_out),
        "xa_wq": f(xa_wq), "xa_wkv": f(xa_wkv), "xa_wo": f(xa_wo),
        "biasT": biasT, "rb31": f(rel_bias[31]),
    }
    maps = []
    for b in range(B):
        m = dict(shared)
        m["xT"] = f(f(x[b]).T.reshape(8, 128, S))
        m["memT"] = f(f(mem[b]).T.reshape(8, 128, 256))
        maps.append(m)
    return maps


_NC_CACHE = {}


def kernel(**inputs):
    x = np.asarray(inputs["x"])
    B, S, _ = x.shape
    L = np.asarray(inputs["ln_g"]).shape[0]
    key = (S, L)
    if key not in _NC_CACHE:
        _NC_CACHE[key] = build(S, L)[0]
    nc = _NC_CACHE[key]
    maps = prep_inputs(**inputs)
    res = run_bass_kernel_spmd(nc, maps, core_ids=list(range(B)))
    out = np.empty((B, S, D), dtype=np.float32)
    for b in range(B):
        out[b] = np.asarray(res.results[b]["outT"]).reshape(D, S).T
    return out
```

```python
import numpy as np
from contextlib import ExitStack
import concourse.bass as bass
import concourse.mybir as mybir
from concourse.bass_utils import run_bass_kernel_spmd

F32 = mybir.dt.float32
BF16 = mybir.dt.bfloat16
AF = mybir.ActivationFunctionType
ALU = mybir.AluOpType
AX = mybir.AxisListType


class Res:
    __slots__ = ("name", "w", "r", "excl", "dram")

    def __init__(self, name, excl=False):
        self.name = name
        self.w = None
        self.r = {}
        self.excl = excl
        self.dram = False


class Prog:
    ENG = ("pe", "act", "dve", "pool", "sp")

    def __init__(self, nc, n_slots=12):
        self.nc = nc
        self.ops = {e: [] for e in self.ENG}
        self.cnt = {e: 0 for e in self.ENG}
        self.seen = {e: {} for e in self.ENG}
        self.n_slots = n_slots
        self.slot_uses = {q: [0] * n_slots for q in ("sp", "pool", "act")}
        self.slot_rr = {q: 0 for q in ("sp", "pool", "act")}
        self.pe_pending = False
        self.nops = 0

    def _deps(self, eng, reads, writes):
        deps = {}

        def add(kv):
            if kv is None:
                return
            k, v = kv
            if deps.get(k, 0) < v:
                deps[k] = v
        for r in reads:
            add(r.w)
            if r.excl:
                for k, v in r.r.items():
                    add((k, v))
        for w in writes:
            add(w.w)
            for k, v in w.r.items():
                add((k, v))
        out = []
        seen = self.seen[eng]
        for k, v in deps.items():
            if k == "pe" and eng == "pe":
                continue
            if seen.get(k, 0) >= v:
                continue
            seen[k] = v
            out.append((k, v))
        return out

    def _register(self, key, val, reads, writes):
        for r in reads:
            if r.excl:
                r.w = (key, val)
                r.r = {}
            else:
                if r.r.get(key, 0) < val:
                    r.r[key] = val
        for w in writes:
            w.w = (key, val)
            w.r = {}

    def op(self, eng, emit, reads=(), writes=(), inc=True):
        waits = self._deps(eng, reads, writes)
        if inc:
            self.cnt[eng] += 1
            val = self.cnt[eng]
            if eng == "pe":
                self.pe_pending = False
        else:
            assert eng == "pe"
            val = self.cnt[eng] + 1
            self.pe_pending = True
        self._register(eng, val, reads, writes)
        self.ops[eng].append((waits, emit, (eng, 1) if inc else None))
        self.nops += 1

    def dma(self, q, emit, reads=(), writes=()):
        j = self.slot_rr[q]
        self.slot_rr[q] = (j + 1) % self.n_slots
        n = self.slot_uses[q][j]
        key = ("dma", q, j)
        waits = self._deps(q, reads, writes)
        if n > 0 and self.seen[q].get(key, 0) < 16 * n:
            self.seen[q][key] = 16 * n
            waits.append((key, 16 * n))
        self.slot_uses[q][j] = n + 1
        val = 16 * (n + 1)
        self._register(key, val, reads, writes)
        self.ops[q].append((waits, emit, (key, 16)))
        self.nops += 1

    def barrier(self):
        assert not self.pe_pending
        targets = [(e, self.cnt[e]) for e in ("pe", "act", "dve", "pool") if self.cnt[e] > 0]
        for q in self.slot_uses:
            for j, n in enumerate(self.slot_uses[q]):
                if n > 0:
                    targets.append((("dma", q, j), 16 * n))
        for e in self.ENG:
            waits = []
            for k, v in targets:
                if k == "pe" and e == "pe":
                    continue
                if self.seen[e].get(k, 0) < v:
                    self.seen[e][k] = v
                    waits.append((k, v))
            self.ops[e].append((waits, None, None))

    def wait_all(self, eng, resources):
        waits = self._deps(eng, resources, ())
        self.ops[eng].append((waits, None, None))

    def emit(self):
        nc = self.nc
        keys = set(self.ENG)
        for q in self.slot_uses:
            for j in range(self.n_slots):
                if self.slot_uses[q][j] > 0:
                    keys.add(("dma", q, j))
        with ExitStack() as es:
            sems = {}
            for k in sorted(keys, key=str):
                nm = k if isinstance(k, str) else "d_%s_%d" % (k[1], k[2])
                sems[k] = es.enter_context(nc.semaphore("s_" + nm))
            block = es.enter_context(nc.Block())

            def run(e, name):
                for waits, emit, inc in self.ops[name]:
                    for k, v in waits:
                        e.wait_ge(sems[k], v)
                    if emit is None:
                        continue
                    ins = emit(e)
                    if inc is not None:
                        ins.then_inc(sems[inc[0]], inc[1])

            @block.tensor
            def _(e):
                run(e, "pe")

            @block.scalar
            def _(e):
                run(e, "act")

            @block.vector
            def _(e):
                run(e, "dve")

            @block.gpsimd
            def _(e):
                run(e, "pool")

            @block.sync
            def _(e):
                run(e, "sp")

D = 1024
DFF = 2816
PMIX = 7368
ALPHA = 4.0 ** 0.25
EPS = 1e-5
KTOP = 256
NIT = 14
NEG = -1.0e30


class Tl:
    def __init__(self, t, name, excl=False):
        self.t = t
        self.r = Res(name, excl)
        self.rl = [self.r]

    def __getitem__(self, k):
        return self.t[k]


def build(S=4096, L=2, dbg=False, stop_after=99):
    NT = S // 128
    nc = bass.Bass("TRN2", target_bir_lowering=False)
    P = Prog(nc)

    def din(name, shape, dt=F32):
        return nc.dram_tensor(name, list(shape), dt, kind="ExternalInput").ap()

    def dres(name):
        r = Res(name)
        r.dram = True
        return r

    def dscr(name, shape, dt):
        return nc.dram_tensor(name, list(shape), dt, kind=("ExternalOutput" if dbg else "Internal")).ap(), dres(name)

    xT = din("xT", [8, 128, S]); r_xT = dres("xT")
    memT = din("memT", [8, 128, 256])
    lng = din("lng", [L, 4, 128, 8]); lnb = din("lnb", [L, 4, 128, 8])
    w_in = din("ffn_w_in", [L, 2, D, 2 * DFF]); w_out = din("ffn_w_out", [L, 2, DFF, D])
    w_mix = din("w_mix_in", [L, D, PMIX])
    bgate = din("bgate", [L, 3, 128, 8])
    kvg = din("kvg", [L, 128])
    wuk = din("wuk", [L, 128, 4, 128])
    wuv = din("wuv", [L, 8, 128, 64])
    convw = din("convw", [L, 128, 4, 3])
    wbr = din("w_branch", [L, 3, 512, D])
    wmo = din("w_mix_out", [L, D, D])
    xwq = din("xa_wq", [L, D, 512]); xwkv = din("xa_wkv", [L, D, D]); xwo = din("xa_wo", [L, 512, D])
    biasT = din("biasT", [128, 8, 256])
    rb31 = din("rb31", [8])
    outT = nc.dram_tensor("outT", [8, 128, S], F32, kind="ExternalOutput").ap(); r_outT = dres("outT")
    r_const = dres("const_in")

    XA, r_XA = dscr("XA", [8, 128, S], F32)
    XB, r_XB = dscr("XB", [8, 128, S], F32)
    QL, r_QL = dscr("QL", [8, 128, S], BF16)
    IQT, r_IQT = dscr("IQT", [4, 128, S], BF16)
    IKT, r_IKT = dscr("IKT", [64, S], BF16)
    QST, r_QST = dscr("QST", [4, 128, S], BF16)
    KST, r_KST = dscr("KST", [4, 128, S], BF16)
    VS, r_VS = dscr("VS", [S, 512], BF16)
    CKV, r_CKV = dscr("CKV", [S, 128], BF16)
    CKVT, r_CKVT = dscr("CKVT", [128, S], BF16)
    IW, r_IW = dscr("IW", [S, 8], F32)
    YCT, r_YCT = dscr("YCT", [4, 128, S], BF16)
    YAT, r_YAT = dscr("YAT", [4, 128, S], BF16)
    YBT, r_YBT = dscr("YBT", [4, 128, S], BF16)

    def mm(out, lhsT, rhs, start, stop, reads, writes, inc=None):
        P.op("pe", lambda e: e.matmul(out, lhsT=lhsT, rhs=rhs, start=start, stop=stop),
             reads, writes, inc=(stop if inc is None else inc))

    def tr(out, in_, ident, reads, writes, inc=True):
        P.op("pe", lambda e: e.transpose(out=out, in_=in_, identity=ident), reads, writes, inc=inc)

    def act(out, in_, func, reads, writes, bias=None, scale=None, accum=None):
        kw = {}
        if bias is not None:
            kw["bias"] = bias
        if scale is not None:
            kw["scale"] = scale
        if accum is not None:
            kw["accum_out"] = accum
        P.op("act", lambda e: e.activation(out=out, in_=in_, func=func, **kw), reads, writes)

    def tt(eng, out, a, b, op, reads, writes):
        P.op(eng, lambda e: e.tensor_tensor(out=out, in0=a, in1=b, op=op), reads, writes)

    def ts(eng, out, a, s1, op0, reads, writes, s2=None, op1=None, accum=None):
        kw = {}
        if op1 is not None:
            kw["op1"] = op1
        if accum is not None:
            kw["accum_out"] = accum
        P.op(eng, lambda e: e.tensor_scalar(out=out, in0=a, scalar1=s1, scalar2=s2, op0=op0, **kw), reads, writes)

    def stt(out, in0, scalar, in1, op0, op1, reads, writes, accum=None):
        kw = {}
        if accum is not None:
            kw["accum_out"] = accum
        P.op("dve", lambda e: e.scalar_tensor_tensor(out=out, in0=in0, scalar=scalar, in1=in1, op0=op0, op1=op1, **kw),
             reads, writes)

    def cp(eng, out, in_, reads, writes):
        if eng == "act":
            P.op("act", lambda e: e.copy(out=out, in_=in_), reads, writes)
        else:
            P.op(eng, lambda e: e.tensor_copy(out=out, in_=in_), reads, writes)

    def red(out, in_, op, reads, writes):
        P.op("dve", lambda e: e.tensor_reduce(out=out, in_=in_, axis=AX.X, op=op), reads, writes)

    def recip(out, in_, reads, writes):
        P.op("dve", lambda e: e.reciprocal(out=out, in_=in_), reads, writes)

    def memset(eng, out, val, writes):
        P.op(eng, lambda e: e.memset(out, val), (), writes)

    def dma(q, out, in_, reads, writes, **kw):
        reads = [r for r in reads if not r.dram]
        writes = [r for r in writes if not r.dram]
        P.dma(q, lambda e: e.dma_start(out=out, in_=in_, **kw), reads, writes)

    def wload(dst, k_chunks, src2d, c0, c1, rows0=0):
        dst.rl = [Res("%s_k%d" % (dst.r.name, k)) for k in range(k_chunks)]
        for k in range(k_chunks):
            dma("pool", dst.t[:, k, 0:c1 - c0], src2d[rows0 + k * 128: rows0 + (k + 1) * 128, c0:c1],
                [r_const] + ([dst.rl[k - 1]] if k > 0 else []), [dst.rl[k]], max_dma_last_dim=8192)

    top = ExitStack()

    uid = [0]

    def alloc(stack, name, shape, dt):
        uid[0] += 1
        name = "%s_%d" % (name, uid[0])
        return Tl(stack.enter_context(nc.sbuf_tensor(name, list(shape), dt)), name)

    PS = [Tl(top.enter_context(nc.psum_tensor("ps%d" % i, [128, 512], F32)), "ps%d" % i, True) for i in range(7)]
    PB = Tl(top.enter_context(nc.psum_tensor("pb", [128, 1024], BF16)), "pb", True)

    identf = alloc(top, "identf", [128, 128], F32)
    ident = alloc(top, "ident", [128, 128], BF16)
    onesM = alloc(top, "onesM", [128, 128], F32)
    onesB = alloc(top, "onesB", [128, 128], BF16)
    caus = alloc(top, "caus", [128, 128], F32)
    strict = alloc(top, "strict", [128, 128], F32)
    pow2 = alloc(top, "pow2", [128, NIT], F32)
    neglo = alloc(top, "neglo", [128, 1], F32)
    epsT = alloc(top, "epsT", [128, 1], F32)
    oneT = alloc(top, "oneT", [128, 1], F32)
    memset("pool", identf.t[:], 0.0, [identf.r])
    P.op("pool", lambda e: e.affine_select(out=identf.t[:], in_=identf.t[:], pattern=[[-1, 128]],
                                           compare_op=ALU.not_equal, fill=1.0, base=0, channel_multiplier=1),
         [identf.r], [identf.r])
    cp("dve", ident.t[:], identf.t[:], [identf.r], [ident.r])
    memset("pool", onesM.t[:], 1.0 / D, [onesM.r])
    memset("pool", onesB.t[:], 1.0, [onesB.r])
    memset("pool", caus.t[:], 0.0, [caus.r])
    P.op("pool", lambda e: e.affine_select(out=caus.t[:], in_=caus.t[:], pattern=[[-1, 128]],
                                           compare_op=ALU.is_ge, fill=NEG, base=0, channel_multiplier=1),
         [caus.r], [caus.r])
    memset("pool", strict.t[:], 1.0, [strict.r])
    P.op("pool", lambda e: e.affine_select(out=strict.t[:], in_=strict.t[:], pattern=[[-1, 128]],
                                           compare_op=ALU.is_gt, fill=0.0, base=0, channel_multiplier=1),
         [strict.r], [strict.r])
    for j in range(NIT):
        memset("pool", pow2.t[:, j:j + 1], 0.5 ** (j + 1), [pow2.r])
    memset("pool", neglo.t[:], -1.0e29, [neglo.r])
    memset("pool", epsT.t[:], EPS, [epsT.r])
    memset("pool", oneT.t[:], 1.0, [oneT.r])

    def ln_part1(Z, SQ, G, pa):
        for c in range(8):
            mm(pa.t[:, 0:G], onesM.t[:], Z.t[:, c, :], c == 0, c == 7, [onesM.r, Z.r], [pa.r])
        for c in range(8):
            tt("dve", Z.t[:, c, :], Z.t[:, c, :], pa.t[:, 0:G], ALU.subtract, [Z.r, pa.r], [Z.r])
        act(SQ.t[:], Z.t[:], AF.Square, [Z.r], [SQ.r])

    def ln_part2(Z, SQ, RS, G, gT, bT, dst, r_dst, g0, pb_):
        for c in range(8):
            mm(pb_.t[:, 0:G], onesM.t[:], SQ.t[:, c, :], c == 0, c == 7, [onesM.r, SQ.r], [pb_.r])
        act(RS.t[:], pb_.t[:, 0:G], AF.Sqrt, [pb_.r, epsT.r], [RS.r], bias=epsT.t[:, 0:1])
        recip(RS.t[:], RS.t[:], [RS.r], [RS.r])
        for c in range(8):
            tt("pool", Z.t[:, c, :], Z.t[:, c, :], RS.t[:], ALU.mult, [Z.r, RS.r], [Z.r])
        for c in range(8):
            act(SQ.t[:, c, :], Z.t[:, c, :], AF.Identity, [Z.r, gT.r, bT.r], [SQ.r],
                scale=gT.t[:, c:c + 1], bias=bT.t[:, c:c + 1])
        dma("sp", dst[:, :, g0:g0 + G].rearrange("c p t -> p c t"), SQ.t[:], [SQ.r], [r_dst])

    def layer_norm(Z, SQ, RS, G, gT, bT, dst, r_dst, g0, pa, pb_):
        ln_part1(Z, SQ, G, pa)
        ln_part2(Z, SQ, RS, G, gT, bT, dst, r_dst, g0, pb_)

    def ffn_phase(l, which, ln_i, src, r_src, dst, r_dst):
        G = 256
        NG = S // G
        with ExitStack() as st:
            Win = alloc(st, "Win", [128, 8, 2 * DFF], BF16)
            Wout = alloc(st, "Wout", [128, 22, D], BF16)
            gT = alloc(st, "gT", [128, 8], F32); bT = alloc(st, "bT", [128, 8], F32)
            X32 = [alloc(st, "X32%d" % b, [128, 8, G], F32) for b in range(2)]
            Xb = [alloc(st, "Xb%d" % b, [128, 8, G], BF16) for b in range(2)]
            H = alloc(st, "H", [128, 22, G], BF16)
            SA = [alloc(st, "SA%d" % i, [128, G], F32) for i in range(2)]
            Z = [alloc(st, "Z%d" % b, [128, 8, G], F32) for b in range(2)]
            SQ = alloc(st, "SQ", [128, 8, G], F32)
            RS = alloc(st, "RS", [128, G], F32)
            dma("sp", gT.t[:], lng[l, ln_i], [r_const], [gT.r])
            dma("sp", bT.t[:], lnb[l, ln_i], [r_const], [bT.r])
            wload(Win, 8, w_in[l, which], 0, 2 * DFF)
            wload(Wout, 22, w_out[l, which], 0, D)

            def load(g):
                b = g % 2
                g0 = g * G
                dma("sp", X32[b].t[:], src[:, :, g0:g0 + G].rearrange("c p t -> p c t"), [r_src], [X32[b].r])
                cp("pool", Xb[b].t[:], X32[b].t[:], [X32[b].r], [Xb[b].r])

            def inproj(g):
                xb = Xb[g % 2]
                for j in range(22):
                    pa = PS[(2 * j) % 4]; pb_ = PS[(2 * j + 1) % 4]
                    for k in range(8):
                        mm(pa.t[:, 0:G], Win.t[:, k, j * 128:(j + 1) * 128], xb.t[:, k, :], k == 0, k == 7,
                           Win.rl + [xb.r], [pa.r])
                    for k in range(8):
                        mm(pb_.t[:, 0:G], Win.t[:, k, DFF + j * 128:DFF + (j + 1) * 128], xb.t[:, k, :], k == 0, k == 7,
                           Win.rl + [xb.r], [pb_.r])
                    sa = SA[j % 2]
                    act(sa.t[:], pa.t[:, 0:G], AF.Silu, [pa.r], [sa.r])
                    stt(H.t[:, j, :], sa.t[:], 0.5, pb_.t[:, 0:G], ALU.mult, ALU.mult, [sa.r, pb_.r], [H.r])

            def outproj(g):
                x32, z = X32[g % 2], Z[g % 2]
                for c in range(8):
                    po = PS[4 + (c % 2)]
                    for j in range(22):
                        mm(po.t[:, 0:G], Wout.t[:, j, c * 128:(c + 1) * 128], H.t[:, j, :], j == 0, j == 21,
                           Wout.rl + [H.r], [po.r])
                    stt(z.t[:, c, :], x32.t[:, c, :], ALPHA, po.t[:, 0:G], ALU.mult, ALU.add, [x32.r, po.r], [z.r])

            load(0)
            if NG > 1:
                load(1)
            inproj(0)
            outproj(0)
            for g in range(NG):
                if g + 2 < NG:
                    load(g + 2)
                if g + 1 < NG:
                    inproj(g + 1)
                ln_part1(Z[g % 2], SQ, G, PS[6])
                if g + 1 < NG:
                    outproj(g + 1)
                ln_part2(Z[g % 2], SQ, RS, G, gT, bT, dst, r_dst, g * G, PS[6])
            P.barrier()

    C_QA, C_CKV, C_IQ, C_IK, C_IW, C_QS, C_KS, C_VS, C_CB, C_CC, C_CH, C_G = \
        0, 512, 640, 1152, 1216, 1224, 1736, 2248, 2760, 3272, 3784, 4296

    def proj_phase(l, src, r_src):
        G = 512
        NW = C_G
        with ExitStack() as st:
            Wm = alloc(st, "Wm", [128, 8, NW], BF16)
            WUK = alloc(st, "WUK", [128, 4, 128], BF16)
            CW = alloc(st, "CW", [128, 4, 3], F32)
            KVG = alloc(st, "KVG", [128, 128], F32)
            X32 = alloc(st, "X32", [128, 8, G], F32)
            Xb = alloc(st, "Xb", [128, 8, G], BF16)
            QA = alloc(st, "QA", [128, 4, G], BF16)
            OB = [alloc(st, "OB%d" % i, [128, G], BF16) for i in range(3)]
            CCs = alloc(st, "CCs", [128, G], F32)
            ZC = [alloc(st, "ZC%d" % q, [128, G + 2], F32) for q in range(4)]
            YC = alloc(st, "YC", [128, G], F32)
            JK = alloc(st, "JK", [128, 128], F32)
            SS = alloc(st, "SS", [128, 2], F32)
            CKb = alloc(st, "CKb", [128, 128], BF16)
            CKTb = alloc(st, "CKTb", [128, 128], BF16)
            IWs = alloc(st, "IWs", [128, 8], F32)
            VSb = alloc(st, "VSb", [128, 512], BF16)
            wload(Wm, 8, w_mix[l], 0, NW)
            dma("pool", WUK.t[:], wuk[l], [r_const], [WUK.r])
            dma("sp", CW.t[:], convw[l], [r_const], [CW.r])
            dma("sp", KVG.t[:], kvg[l].partition_broadcast(128), [r_const], [KVG.r])
            for q in range(4):
                memset("pool", ZC[q].t[:, 0:2], 0.0, [ZC[q].r])
            rot = [0]

            def nextps():
                rot[0] = (rot[0] + 1) % 5
                return PS[rot[0]]
            obr = [0]

            def nextob():
                obr[0] = (obr[0] + 1) % 3
                return OB[obr[0]]

            def fm_chunk(col, M=128):
                ps = nextps()
                for k in range(8):
                    mm(ps.t[0:M, 0:G], Wm.t[:, k, col:col + M], Xb.t[:, k, :], k == 0, k == 7, Wm.rl + [Xb.r], [ps.r])
                return ps

            for g in range(S // G):
                g0 = g * G
                dma("sp", X32.t[:], src[:, :, g0:g0 + G].rearrange("c p t -> p c t"), [r_src], [X32.r])
                cp("pool", Xb.t[:], X32.t[:], [X32.r], [Xb.r])
                for q in range(4):
                    ps = fm_chunk(C_QA + q * 128)
                    cp("act", QA.t[:, q, :], ps.t[:, 0:G], [ps.r], [QA.r])
                for h in range(8):
                    p0 = (h % 2) * 64
                    ps = nextps()
                    mm(ps.t[:, 0:G], WUK.t[p0:p0 + 64, h // 2, :], QA.t[p0:p0 + 64, h // 2, :], True, True,
                       [WUK.r, QA.r], [ps.r])
                    ob = nextob()
                    act(ob.t[:], ps.t[:, 0:G], AF.Copy, [ps.r], [ob.r], scale=0.125)
                    dma("sp", QL[h, :, g0:g0 + G], ob.t[:], [ob.r], [r_QL])
                for (col, dstT, r_d, sc) in ((C_IQ, IQT, r_IQT, 1.0), (C_QS, QST, r_QST, 0.125), (C_KS, KST, r_KST, 1.0)):
                    for q in range(4):
                        ps = fm_chunk(col + q * 128)
                        ob = nextob()
                        act(ob.t[:], ps.t[:, 0:G], AF.Copy, [ps.r], [ob.r], scale=sc)
                        dma("sp", dstT[q, :, g0:g0 + G], ob.t[:], [ob.r], [r_d])
                ps = fm_chunk(C_IK, 64)
                ob = nextob()
                cp("act", ob.t[0:64, :], ps.t[0:64, 0:G], [ps.r], [ob.r])
                dma("sp", IKT[:, g0:g0 + G], ob.t[0:64, :], [ob.r], [r_IKT])
                for q in range(4):
                    ps = fm_chunk(C_CC + q * 128)
                    cp("act", CCs.t[:], ps.t[:, 0:G], [ps.r], [CCs.r])
                    ps = fm_chunk(C_CH + q * 128)
                    zc = ZC[q]
                    tt("dve", zc.t[:, 2:G + 2], CCs.t[:], ps.t[:, 0:G], ALU.mult, [CCs.r, ps.r], [zc.r])
                    ts("dve", YC.t[:], zc.t[:, 2:G + 2], CW.t[:, q, 2:3], ALU.mult, [zc.r, CW.r], [YC.r])
                    stt(YC.t[:], zc.t[:, 1:G + 1], CW.t[:, q, 1:2], YC.t[:], ALU.mult, ALU.add, [zc.r, CW.r, YC.r], [YC.r])
                    stt(YC.t[:], zc.t[:, 0:G], CW.t[:, q, 0:1], YC.t[:], ALU.mult, ALU.add, [zc.r, CW.r, YC.r], [YC.r])
                    ps = fm_chunk(C_CB + q * 128)
                    ob = nextob()
                    tt("dve", ob.t[:], YC.t[:], ps.t[:, 0:G], ALU.mult, [YC.r, ps.r], [ob.r])
                    dma("sp", YCT[q, :, g0:g0 + G], ob.t[:], [ob.r], [r_YCT])
                    cp("pool", zc.t[:, 0:2], zc.t[:, G:G + 2], [zc.r], [zc.r])
                for t4 in range(G // 128):
                    t0 = g0 + t4 * 128
                    xs = slice(t4 * 128, (t4 + 1) * 128)
                    ps = nextps()
                    for k in range(8):
                        mm(ps.t[:, 0:128], Xb.t[:, k, xs], Wm.t[:, k, C_CKV:C_CKV + 128], k == 0, k == 7,
                           Wm.rl + [Xb.r], [ps.r])
                    act(JK.t[:], ps.t[:, 0:128], AF.Square, [ps.r], [JK.r, SS.r], accum=SS.t[:, 0:1])
                    act(SS.t[:, 1:2], SS.t[:, 0:1], AF.Sqrt, [SS.r, epsT.r], [SS.r], scale=1.0 / 128, bias=epsT.t[:, 0:1])
                    recip(SS.t[:, 1:2], SS.t[:, 1:2], [SS.r], [SS.r])
                    stt(CKb.t[:], ps.t[:, 0:128], SS.t[:, 1:2], KVG.t[:], ALU.mult, ALU.mult, [ps.r, SS.r, KVG.r], [CKb.r])
                    dma("sp", CKV[t0:t0 + 128, :], CKb.t[:], [CKb.r], [r_CKV])
                    tr(PB.t[:, 0:128], CKb.t[:], ident.t[:], [CKb.r, ident.r], [PB.r])
                    cp("act", CKTb.t[:], PB.t[:, 0:128], [PB.r], [CKTb.r])
                    dma("sp", CKVT[:, t0:t0 + 128], CKTb.t[:], [CKTb.r], [r_CKVT])
                    ps = nextps()
                    for k in range(8):
                        mm(ps.t[:, 0:8], Xb.t[:, k, xs], Wm.t[:, k, C_IW:C_IW + 8], k == 0, k == 7, Wm.rl + [Xb.r], [ps.r])
                    cp("act", IWs.t[:], ps.t[:, 0:8], [ps.r], [IWs.r])
                    dma("sp", IW[t0:t0 + 128, :], IWs.t[:], [IWs.r], [r_IW])
                    ps = nextps()
                    for k in range(8):
                        mm(ps.t[:, 0:512], Xb.t[:, k, xs], Wm.t[:, k, C_VS:C_VS + 512], k == 0, k == 7, Wm.rl + [Xb.r], [ps.r])
                    cp("act", VSb.t[:], ps.t[:, 0:512], [ps.r], [VSb.r])
                    dma("sp", VS[t0:t0 + 128, :], VSb.t[:], [VSb.r], [r_VS])
            P.barrier()

    def chunks(n):
        return [(c0, min(512, n - c0)) for c0 in range(0, n, 512)]

    def dsa_phase(l):
        with ExitStack() as st:
            CKVX = alloc(st, "CKVX", [128, NT, 128], BF16)
            CKT = alloc(st, "CKT", [128, S], BF16)
            IK2 = alloc(st, "IK2", [128, S], BF16)
            BI = alloc(st, "BI", [128, 8, 256], F32)
            RB = alloc(st, "RB", [128, 8], F32)
            WUVP = alloc(st, "WUVP", [128, 8, 128], BF16)
            QLg = [alloc(st, "QLg%d" % b, [128, 8, 512], BF16) for b in range(2)]
            IQg = [alloc(st, "IQg%d" % b, [128, 4, 512], BF16) for b in range(2)]
            IWg = [alloc(st, "IWg%d" % b, [128, 4, 8], F32) for b in range(2)]
            WA = [alloc(st, "WA%d" % b, [128, 8], F32) for b in range(2)]
            SG = [alloc(st, "SG%d" % b, [128, 8], F32) for b in range(2)]
            SC = [alloc(st, "SC%d" % b, [128, S], F32) for b in range(2)]
            Mk = [alloc(st, "Mk%d" % b, [128, S], BF16) for b in range(2)]
            TH = [alloc(st, "TH%d" % b, [128, 8], F32) for b in range(2)]
            WALL = [alloc(st, "WALL%d" % b, [128, NIT], F32) for b in range(2)]
            PM = [alloc(st, "PM%d" % b, [128, S], BF16) for b in range(2)]
            PT = [alloc(st, "PT%d" % b, [128, NT, 128], BF16) for b in range(2)]
            TMP = [alloc(st, "TMP%d" % i, [128, 512], F32) for i in range(4)]
            JKB = alloc(st, "JKB", [128, S], BF16)
            RSM = alloc(st, "RSM", [128, 8, 8], F32)
            RSUM = alloc(st, "RSUM", [128, 8], F32)
            OLN = alloc(st, "OLN", [128, 8, 128], BF16)
            OLT = alloc(st, "OLT", [128, 8, 128], BF16)
            YAg = alloc(st, "YAg", [128, 4, 512], BF16)
            dma("sp", CKVX.t[:], CKV.rearrange("(j p) c -> p j c", p=128), [r_CKV], [CKVX.r])
            dma("sp", CKT.t[:], CKVT, [r_CKVT], [CKT.r])
            dma("sp", IK2.t[0:64, :], IKT, [r_IKT], [IK2.r])
            dma("sp", IK2.t[64:128, :], IKT, [r_IKT], [IK2.r])
            dma("sp", BI.t[:], biasT, [r_const], [BI.r])
            dma("sp", RB.t[:], rb31.partition_broadcast(128), [r_const], [RB.r])
            memset("pool", WUVP.t[:], 0.0, [WUVP.r])
            for h in range(8):
                p0 = (h % 2) * 64
                dma("pool", WUVP.t[:, h, p0:p0 + 64], wuv[l, h], [r_const], [WUVP.r])
            tmr = [0]

            def nexttmp():
                tmr[0] = (tmr[0] + 1) % 4
                return TMP[tmr[0]]
            psr = [0]

            def nextps():
                psr[0] = (psr[0] + 1) % 3
                return PS[psr[0]]

            def load_group(g):
                b = g % 2
                g0 = g * 512
                dma("sp", QLg[b].t[:], QL[:, :, g0:g0 + 512].rearrange("h c t -> c h t"), [r_QL], [QLg[b].r])
                dma("sp", IQg[b].t[:], IQT[:, :, g0:g0 + 512].rearrange("q p t -> p q t"), [r_IQT], [IQg[b].r])
                dma("sp", IWg[b].t[:], IW[g0:g0 + 512, :].rearrange("(a p) h -> p a h", p=128), [r_IW], [IWg[b].r])

            def index_scores(i):
                g, t4 = divmod(i, 4)
                gb = g % 2
                b = i % 2
                n = (i + 1) * 128
                tsl = slice(t4 * 128, (t4 + 1) * 128)
                sc, th, wall, wa, sg, mk = SC[b], TH[b], WALL[b], WA[b], SG[b], Mk[b]
                act(wa.t[:], IWg[gb].t[:, t4, :], AF.Abs, [IWg[gb].r], [wa.r])
                P.op("act", lambda e: e.sign(out=sg.t[:], in_=IWg[gb].t[:, t4, :]), [IWg[gb].r], [sg.r])
                for (c0, w) in chunks(n):
                    for h in range(8):
                        p0 = (h % 2) * 64
                        ps = nextps()
                        mm(ps.t[:, 0:w], IQg[gb].t[p0:p0 + 64, h // 2, tsl], IK2.t[p0:p0 + 64, c0:c0 + w], True, True,
                           [IQg[gb].r, IK2.r], [ps.r])
                        R = nexttmp()
                        act(R.t[:, 0:w], ps.t[:, 0:w], AF.Relu, [ps.r, wa.r], [R.r], scale=wa.t[:, h:h + 1])
                        if h == 0:
                            ts("dve", sc.t[:, c0:c0 + w], R.t[:, 0:w], sg.t[:, 0:1], ALU.mult, [R.r, sg.r], [sc.r])
                        else:
                            stt(sc.t[:, c0:c0 + w], R.t[:, 0:w], sg.t[:, h:h + 1], sc.t[:, c0:c0 + w], ALU.mult, ALU.add,
                                [R.r, sg.r, sc.r], [sc.r])
                tt("dve", sc.t[:, n - 128:n], sc.t[:, n - 128:n], caus.t[:], ALU.add, [sc.r, caus.r], [sc.r])
                steps = []
                if i >= 2:
                    def init():
                        red(th.t[:, 0:1], sc.t[:, 0:n - 128], ALU.min, [sc.r], [th.r])
                        red(th.t[:, 1:2], sc.t[:, 0:n], ALU.max, [sc.r], [th.r])
                        tt("dve", th.t[:, 2:3], th.t[:, 1:2], th.t[:, 0:1], ALU.subtract, [th.r], [th.r])
                        ts("dve", wall.t[:], pow2.t[:], th.t[:, 2:3], ALU.mult, [pow2.r, th.r], [wall.r])
                        tt("dve", th.t[:, 3:4], th.t[:, 0:1], wall.t[:, 0:1], ALU.add, [th.r, wall.r], [th.r])
                    steps.append(init)

                    def mkstep(j):
                        def step():
                            ts("dve", JKB.t[:, 0:n], sc.t[:, 0:n], th.t[:, 3:4], ALU.is_ge, [sc.r, th.r], [th.r],
                               op1=ALU.add, accum=th.t[:, 4:5])
                            ts("dve", th.t[:, 5:6], th.t[:, 4:5], KTOP - 0.5, ALU.is_ge, [th.r, wall.r], [th.r],
                               s2=wall.t[:, j:j + 1], op1=ALU.mult)
                            if j + 1 < NIT:
                                stt(th.t[:, 3:4], th.t[:, 5:6], wall.t[:, j + 1:j + 2], th.t[:, 3:4], ALU.subtract, ALU.add,
                                    [th.r, wall.r], [th.r])
                            else:
                                stt(th.t[:, 0:1], th.t[:, 5:6], wall.t[:, j:j + 1], th.t[:, 3:4], ALU.subtract, ALU.add,
                                    [th.r, wall.r], [th.r])
                        return step
                    for j in range(NIT):
                        steps.append(mkstep(j))
                    steps.append(lambda: ts("dve", mk.t[:, 0:n], sc.t[:, 0:n], th.t[:, 0:1], ALU.is_ge, [sc.r, th.r], [mk.r]))
                else:
                    steps.append(lambda: ts("dve", mk.t[:, 0:n], sc.t[:, 0:n], neglo.t[:, 0:1], ALU.is_ge, [sc.r, neglo.r], [mk.r]))
                return steps

            def heads(i, pending):
                g, t4 = divmod(i, 4)
                gb = g % 2
                n = (i + 1) * 128
                tsl = slice(t4 * 128, (t4 + 1) * 128)
                mk = Mk[i % 2]
                nb0 = max(0, n - 256)
                per_head = (len(pending) + 7) // 8
                for h in range(8):
                    pm, pt = PM[h % 2], PT[h % 2]
                    ch = chunks(n)
                    for ci, (c0, w) in enumerate(ch):
                        ps = nextps()
                        mm(ps.t[:, 0:w], QLg[gb].t[:, h, tsl], CKT.t[:, c0:c0 + w], True, True, [QLg[gb].r, CKT.r], [ps.r])
                        Pe = nexttmp()
                        fa, fb = c0, min(c0 + w, nb0)
                        na, nb_ = max(c0, nb0), c0 + w
                        if fb > fa:
                            act(Pe.t[:, fa - c0:fb - c0], ps.t[:, fa - c0:fb - c0], AF.Exp, [ps.r, RB.r], [Pe.r],
                                bias=RB.t[:, h:h + 1])
                        if nb_ > na:
                            bo = 256 - (n - na)
                            tt("dve", Pe.t[:, na - c0:nb_ - c0], ps.t[:, na - c0:nb_ - c0], BI.t[:, h, bo:bo + (nb_ - na)],
                               ALU.add, [ps.r, BI.r], [Pe.r])
                            act(Pe.t[:, na - c0:nb_ - c0], Pe.t[:, na - c0:nb_ - c0], AF.Exp, [Pe.r], [Pe.r])
                        stt(pm.t[:, c0:c0 + w], Pe.t[:, 0:w], 1.0, mk.t[:, c0:c0 + w], ALU.mult, ALU.mult,
                            [Pe.r, mk.r], [pm.r, RSM.r], accum=RSM.t[:, h, ci:ci + 1])
                    red(RSUM.t[:, h:h + 1], RSM.t[:, h, 0:len(ch)], ALU.add, [RSM.r], [RSUM.r])
                    recip(RSUM.t[:, h:h + 1], RSUM.t[:, h:h + 1], [RSUM.r], [RSUM.r])
                    for _ in range(per_head):
                        if pending:
                            pending.pop(0)()
                    for j0 in range(0, i + 1, 8):
                        nbk = min(8, i + 1 - j0)
                        for jj in range(nbk):
                            j = j0 + jj
                            tr(PB.t[:, jj * 128:(jj + 1) * 128], pm.t[:, j * 128:(j + 1) * 128], ident.t[:],
                               [pm.r, ident.r], [PB.r], inc=(jj == nbk - 1))
                        cp("act", pt.t[:, j0:j0 + nbk, :], PB.t[:, 0:nbk * 128].rearrange("p (j t) -> p j t", t=128),
                           [PB.r], [pt.r])
                    ol = PS[3 + (h % 2)]
                    for j in range(i + 1):
                        mm(ol.t[:, 0:128], pt.t[:, j, :], CKVX.t[:, j, :], j == 0, j == i, [pt.r, CKVX.r], [ol.r])
                    act(OLN.t[:, h, :], ol.t[:, 0:128], AF.Copy, [ol.r, RSUM.r], [OLN.r], scale=RSUM.t[:, h:h + 1])
                while pending:
                    pending.pop(0)()
                for h in range(8):
                    tr(PB.t[:, h * 128:(h + 1) * 128], OLN.t[:, h, :], ident.t[:], [OLN.r, ident.r], [PB.r], inc=(h == 7))
                cp("act", OLT.t[:], PB.t[:].rearrange("p (j t) -> p j t", t=128), [PB.r], [OLT.r])
                for q in range(4):
                    ps = PS[5]
                    mm(ps.t[:, 0:128], WUVP.t[:, 2 * q, :], OLT.t[:, 2 * q, :], True, False, [WUVP.r, OLT.r], [ps.r])
                    mm(ps.t[:, 0:128], WUVP.t[:, 2 * q + 1, :], OLT.t[:, 2 * q + 1, :], False, True, [WUVP.r, OLT.r], [ps.r])
                    cp("act", YAg.t[:, q, tsl], ps.t[:, 0:128], [ps.r], [YAg.r])
                if t4 == 3:
                    g0 = g * 512
                    dma("sp", YAT[:, :, g0:g0 + 512].rearrange("q p t -> p q t"), YAg.t[:], [YAg.r], [r_YAT])

            load_group(0)
            for s_ in index_scores(0):
                s_()
            for i in range(NT):
                pending = []
                if i + 1 < NT:
                    if (i + 1) % 4 == 0:
                        load_group((i + 1) // 4)
                    pending = index_scores(i + 1)
                heads(i, pending)
            P.barrier()

    def sb_phase(l):
        with ExitStack() as st:
            KS_ = alloc(st, "KS_", [128, 4, S], BF16)
            VSX = alloc(st, "VSX", [128, NT, 512], BF16)
            QSg = [alloc(st, "QSg%d" % b, [128, 4, 512], BF16) for b in range(2)]
            LB = [alloc(st, "LB%d" % b, [128, S + 1], F32) for b in range(2)]
            NLX = [alloc(st, "NLX%d" % b, [128, S], F32) for b in range(2)]
            AB = [alloc(st, "AB%d" % b, [128, S], BF16) for b in range(2)]
            PT = [alloc(st, "PT2%d" % b, [128, NT, 128], BF16) for b in range(2)]
            TMP = [alloc(st, "TMQ%d" % i, [128, 512], F32) for i in range(4)]
            TSM = [alloc(st, "TSM%d" % b, [128, 16], F32) for b in range(2)]
            TOT = [alloc(st, "TOT%d" % b, [128, 2], F32) for b in range(2)]
            YBs = alloc(st, "YBs", [128, 512], BF16)
            YBg = alloc(st, "YBg", [128, 4, 512], BF16)
            dma("sp", KS_.t[:], KST.rearrange("q p t -> p q t"), [r_KST], [KS_.r])
            dma("sp", VSX.t[:], VS.rearrange("(j p) c -> p j c", p=128), [r_VS], [VSX.r])
            for b in range(2):
                memset("pool", LB[b].t[:, 0:1], 0.0, [LB[b].r])
            tmr = [0]

            def nexttmp():
                tmr[0] = (tmr[0] + 1) % 4
                return TMP[tmr[0]]
            psr = [0]

            def nextps():
                psr[0] = (psr[0] + 1) % 4
                return PS[psr[0]]
            yb = PS[4]

            def pass1(i, h):
                g, t4 = divmod(i, 4)
                qs = QSg[g % 2]
                n = (i + 1) * 128
                tsl = slice(t4 * 128, (t4 + 1) * 128)
                p0 = (h % 2) * 64
                lb, tsm, tot = LB[h % 2], TSM[h % 2], TOT[h % 2]
                memset("dve", tsm.t[:], 0.0, [tsm.r])
                for ci, (c0, w) in enumerate(chunks(n)):
                    ps = nextps()
                    mm(ps.t[:, 0:w], qs.t[p0:p0 + 64, h // 2, tsl], KS_.t[p0:p0 + 64, h // 2, c0:c0 + w], True, True,
                       [qs.r, KS_.r], [ps.r])
                    E1 = nexttmp()
                    act(E1.t[:, 0:w], ps.t[:, 0:w], AF.Exp, [ps.r], [E1.r])
                    last = (c0 + w == n)
                    wf = w - 128 if last else w
                    if wf > 0:
                        act(lb.t[:, 1 + c0:1 + c0 + wf], E1.t[:, 0:wf], AF.Ln, [E1.r, oneT.r], [lb.r, tsm.r],
                            bias=oneT.t[:, 0:1], accum=tsm.t[:, ci:ci + 1])
                    if last:
                        act(lb.t[:, 1 + n - 128:1 + n], E1.t[:, w - 128:w], AF.Ln, [E1.r, oneT.r], [lb.r], bias=oneT.t[:, 0:1])
                        tt("dve", lb.t[:, 1 + n - 128:1 + n], lb.t[:, 1 + n - 128:1 + n], strict.t[:], ALU.mult,
                           [lb.r, strict.r], [lb.r])
                        red(tsm.t[:, 15:16], lb.t[:, 1 + n - 128:1 + n], ALU.add, [lb.r], [tsm.r])
                red(tot.t[:, 0:1], tsm.t[:], ALU.add, [tsm.r], [tot.r])
                ts("dve", tot.t[:, 1:2], tot.t[:, 0:1], -1.0, ALU.mult, [tot.r], [tot.r])

            def pass2(i, h):
                g, t4 = divmod(i, 4)
                qs = QSg[g % 2]
                n = (i + 1) * 128
                tsl = slice(t4 * 128, (t4 + 1) * 128)
                p0 = (h % 2) * 64
                lb, tot, nlx, ab, pt = LB[h % 2], TOT[h % 2], NLX[h % 2], AB[h % 2], PT[h % 2]
                P.op("dve", lambda e: e.tensor_tensor_scan(out=nlx.t[:, 0:n], data0=lb.t[:, 0:n], data1=lb.t[:, 0:n],
                                                           initial=tot.t[:, 1:2], op0=ALU.add, op1=ALU.min),
                     [lb.r, tot.r], [nlx.r])
                for ci, (c0, w) in enumerate(chunks(n)):
                    ps = nextps()
                    mm(ps.t[:, 0:w], qs.t[p0:p0 + 64, h // 2, tsl], KS_.t[p0:p0 + 64, h // 2, c0:c0 + w], True, True,
                       [qs.r, KS_.r], [ps.r])
                    EB = nexttmp()
                    tt("dve", EB.t[:, 0:w], ps.t[:, 0:w], nlx.t[:, c0:c0 + w], ALU.add, [ps.r, nlx.r], [EB.r])
                    act(ab.t[:, c0:c0 + w], EB.t[:, 0:w], AF.Exp, [EB.r], [ab.r])
                tt("dve", ab.t[:, n - 128:n], ab.t[:, n - 128:n], strict.t[:], ALU.mult, [ab.r, strict.r], [ab.r])
                for j0 in range(0, i + 1, 8):
                    nbk = min(8, i + 1 - j0)
                    for jj in range(nbk):
                        j = j0 + jj
                        tr(PB.t[:, jj * 128:(jj + 1) * 128], ab.t[:, j * 128:(j + 1) * 128], ident.t[:],
                           [ab.r, ident.r], [PB.r], inc=(jj == nbk - 1))
                    cp("act", pt.t[:, j0:j0 + nbk, :], PB.t[:, 0:nbk * 128].rearrange("p (j t) -> p j t", t=128),
                       [PB.r], [pt.r])
                for j in range(i + 1):
                    mm(yb.t[:, h * 64:(h + 1) * 64], pt.t[:, j, :], VSX.t[:, j, h * 64:(h + 1) * 64], j == 0, j == i,
                       [pt.r, VSX.r], [yb.r])

            def load_group(g):
                g0 = g * 512
                dma("sp", QSg[g % 2].t[:], QST[:, :, g0:g0 + 512].rearrange("q p t -> p q t"), [r_QST], [QSg[g % 2].r])

            load_group(0)
            pass1(0, 0)
            for i in range(NT):
                g, t4 = divmod(i, 4)
                tsl = slice(t4 * 128, (t4 + 1) * 128)
                for h in range(8):
                    if h < 7:
                        pass1(i, h + 1)
                    elif i + 1 < NT:
                        if (i + 1) % 4 == 0:
                            load_group((i + 1) // 4)
                        pass1(i + 1, 0)
                    pass2(i, h)
                cp("act", YBs.t[:], yb.t[:, 0:512], [yb.r], [YBs.r])
                for q in range(4):
                    tr(PB.t[:, q * 128:(q + 1) * 128], YBs.t[:, q * 128:(q + 1) * 128], ident.t[:], [YBs.r, ident.r], [PB.r], inc=(q == 3))
                cp("act", YBg.t[:, :, tsl], PB.t[:, 0:512].rearrange("p (q t) -> p q t", t=128), [PB.r], [YBg.r])
                if t4 == 3:
                    g0 = g * 512
                    dma("sp", YBT[:, :, g0:g0 + 512].rearrange("q p t -> p q t"), YBg.t[:], [YBg.r], [r_YBT])
            P.barrier()

    def merge_phase(l, src, r_src, dst, r_dst):
        G = 256
        NG = S // G
        with ExitStack() as st:
            WG = alloc(st, "WG", [128, 8, 3 * D], BF16)
            WBR = alloc(st, "WBR", [128, 12, D], BF16)
            WO = alloc(st, "WO", [128, 8, D], BF16)
            BG = alloc(st, "BG", [128, 3, 8], F32)
            gT = alloc(st, "gT", [128, 8], F32); bT = alloc(st, "bT", [128, 8], F32)
            X32 = [alloc(st, "X32%d" % b, [128, 8, G], F32) for b in range(2)]
            Xb = [alloc(st, "Xb%d" % b, [128, 8, G], BF16) for b in range(2)]
            Y = [[alloc(st, "Y%d_%d" % (i, b), [128, 4, G], BF16) for i in range(3)] for b in range(2)]
            SGt = [alloc(st, "SGt%d" % i, [128, G], F32) for i in range(2)]
            TM = [alloc(st, "TM%d" % i, [128, G], F32) for i in range(2)]
            MGf = alloc(st, "MGf", [128, 8, G], F32)
            MGb = alloc(st, "MGb", [128, 8, G], BF16)
            Z = [alloc(st, "Z%d" % b, [128, 8, G], F32) for b in range(2)]
            SQ = alloc(st, "SQ", [128, 8, G], F32)
            RS = alloc(st, "RS", [128, G], F32)
            dma("sp", gT.t[:], lng[l, 1], [r_const], [gT.r])
            dma("sp", bT.t[:], lnb[l, 1], [r_const], [bT.r])
            dma("sp", BG.t[:], bgate[l].rearrange("i p c -> p i c"), [r_const], [BG.r])
            wload(WG, 8, w_mix[l], C_G, PMIX)
            for i in range(3):
                for k in range(4):
                    dma("pool", WBR.t[:, i * 4 + k, :], wbr[l, i, k * 128:(k + 1) * 128, :], [r_const], [WBR.r], max_dma_last_dim=8192)
            wload(WO, 8, wmo[l], 0, D)
            srcs = ((YAT, r_YAT), (YBT, r_YBT), (YCT, r_YCT))

            def load(g):
                b = g % 2
                g0 = g * G
                dma("sp", X32[b].t[:], src[:, :, g0:g0 + G].rearrange("c p t -> p c t"), [r_src], [X32[b].r])
                cp("pool", Xb[b].t[:], X32[b].t[:], [X32[b].r], [Xb[b].r])
                for i in range(3):
                    dma("sp", Y[b][i].t[:], srcs[i][0][:, :, g0:g0 + G].rearrange("q p t -> p q t"), [srcs[i][1]], [Y[b][i].r])

            def stage1(g):
                xb, y = Xb[g % 2], Y[g % 2]
                n_ = 0
                for c in range(8):
                    for i in range(3):
                        pg = PS[n_ % 2]; pb_ = PS[2 + n_ % 2]; sg = SGt[n_ % 2]; tm = TM[n_ % 2]
                        n_ += 1
                        col = i * D + c * 128
                        for k in range(8):
                            mm(pg.t[:, 0:G], WG.t[:, k, col:col + 128], xb.t[:, k, :], k == 0, k == 7, WG.rl + [xb.r], [pg.r])
                        act(sg.t[:], pg.t[:, 0:G], AF.Sigmoid, [pg.r, BG.r], [sg.r], bias=BG.t[:, i, c:c + 1])
                        for k in range(4):
                            mm(pb_.t[:, 0:G], WBR.t[:, i * 4 + k, c * 128:(c + 1) * 128], y[i].t[:, k, :], k == 0, k == 3,
                               [WBR.r, y[i].r], [pb_.r])
                        if i == 0:
                            tt("dve", MGf.t[:, c, :], sg.t[:], pb_.t[:, 0:G], ALU.mult, [sg.r, pb_.r], [MGf.r])
                        else:
                            tt("dve", tm.t[:], sg.t[:], pb_.t[:, 0:G], ALU.mult, [sg.r, pb_.r], [tm.r])
                            tt("pool", MGf.t[:, c, :], MGf.t[:, c, :], tm.t[:], ALU.add, [MGf.r, tm.r], [MGf.r])
                cp("pool", MGb.t[:], MGf.t[:], [MGf.r], [MGb.r])

            def stage2(g):
                x32, z = X32[g % 2], Z[g % 2]
                for c in range(8):
                    po = PS[4 + (c % 2)]
                    for k in range(8):
                        mm(po.t[:, 0:G], WO.t[:, k, c * 128:(c + 1) * 128], MGb.t[:, k, :], k == 0, k == 7, WO.rl + [MGb.r], [po.r])
                    stt(z.t[:, c, :], x32.t[:, c, :], ALPHA, po.t[:, 0:G], ALU.mult, ALU.add, [x32.r, po.r], [z.r])

            load(0)
            if NG > 1:
                load(1)
            stage1(0)
            stage2(0)
            for g in range(NG):
                if g + 2 < NG:
                    load(g + 2)
                if g + 1 < NG:
                    stage1(g + 1)
                ln_part1(Z[g % 2], SQ, G, PS[6])
                if g + 1 < NG:
                    stage2(g + 1)
                ln_part2(Z[g % 2], SQ, RS, G, gT, bT, dst, r_dst, g * G, PS[6])
            P.barrier()

    def xattn_phase(l, src, r_src, dst, r_dst):
        G = 256
        with ExitStack() as st:
            WQ = alloc(st, "WQ", [128, 8, 512], BF16)
            WKV = alloc(st, "WKV", [128, 8, D], BF16)
            WO = alloc(st, "WOx", [128, 4, D], BF16)
            gT = alloc(st, "gT", [128, 8], F32); bT = alloc(st, "bT", [128, 8], F32)
            MT = alloc(st, "MT", [128, 8, 256], BF16)
            KT = alloc(st, "KT", [128, 4, 256], BF16)
            VX = alloc(st, "VX", [128, 2, 512], BF16)
            X32 = [alloc(st, "X32%d" % b_, [128, 8, G], F32) for b_ in range(2)]
            Xb = [alloc(st, "Xb%d" % b_, [128, 8, G], BF16) for b_ in range(2)]
            QT = alloc(st, "QT", [128, 4, G], BF16)
            PTm = [alloc(st, "PTm%d" % i, [128, G], BF16) for i in range(2)]
            RD = alloc(st, "RD", [128, G], F32)
            OT = alloc(st, "OT", [128, 4, G], BF16)
            Z = [alloc(st, "Z%d" % b_, [128, 8, G], F32) for b_ in range(2)]
            SQ = alloc(st, "SQ", [128, 8, G], F32)
            RS = alloc(st, "RS", [128, G], F32)
            dma("sp", gT.t[:], lng[l, 2], [r_const], [gT.r])
            dma("sp", bT.t[:], lnb[l, 2], [r_const], [bT.r])
            wload(WQ, 8, xwq[l], 0, 512)
            wload(WKV, 8, xwkv[l], 0, D)
            wload(WO, 4, xwo[l], 0, D)
            dma("pool", MT.t[:], memT.rearrange("c p m -> p c m"), [r_const], [MT.r])
            for h in range(4):
                ps = PS[h % 2]
                for k in range(8):
                    mm(ps.t[:, 0:256], WKV.t[:, k, h * 128:(h + 1) * 128], MT.t[:, k, :], k == 0, k == 7, WKV.rl + [MT.r], [ps.r])
                cp("act", KT.t[:, h, :], ps.t[:, 0:256], [ps.r], [KT.r])
            for mt in range(2):
                ps = PS[2 + mt]
                for k in range(8):
                    mm(ps.t[:, 0:512], MT.t[:, k, mt * 128:(mt + 1) * 128], WKV.t[:, k, 512:1024], k == 0, k == 7, WKV.rl + [MT.r], [ps.r])
                cp("act", VX.t[:, mt, :], ps.t[:, 0:512], [ps.r], [VX.r])
            def load(g):
                b_ = g % 2
                g0 = g * G
                dma("sp", X32[b_].t[:], src[:, :, g0:g0 + G].rearrange("c p t -> p c t"), [r_src], [X32[b_].r])
                cp("pool", Xb[b_].t[:], X32[b_].t[:], [X32[b_].r], [Xb[b_].r])

            def attn(g):
                xb = Xb[g % 2]
                for h in range(4):
                    ps = PS[h % 2]
                    for k in range(8):
                        mm(ps.t[:, 0:G], WQ.t[:, k, h * 128:(h + 1) * 128], xb.t[:, k, :], k == 0, k == 7, WQ.rl + [xb.r], [ps.r])
                    act(QT.t[:, h, :], ps.t[:, 0:G], AF.Copy, [ps.r], [QT.r], scale=128.0 ** -0.5)
                for h in range(4):
                    for mt in range(2):
                        ps = PS[mt]
                        mm(ps.t[:, 0:G], KT.t[:, h, mt * 128:(mt + 1) * 128], QT.t[:, h, :], True, True, [KT.r, QT.r], [ps.r])
                        act(PTm[mt].t[:], ps.t[:, 0:G], AF.Exp, [ps.r], [PTm[mt].r])
                    po = PS[2]; pd = PS[3]
                    for mt in range(2):
                        mm(po.t[:, 0:G], VX.t[:, mt, h * 128:(h + 1) * 128], PTm[mt].t[:], mt == 0, mt == 1, [VX.r, PTm[mt].r], [po.r])
                    for mt in range(2):
                        mm(pd.t[:, 0:G], onesB.t[:], PTm[mt].t[:], mt == 0, mt == 1, [onesB.r, PTm[mt].r], [pd.r])
                    recip(RD.t[:], pd.t[:, 0:G], [pd.r], [RD.r])
                    tt("dve", OT.t[:, h, :], po.t[:, 0:G], RD.t[:], ALU.mult, [po.r, RD.r], [OT.r])

            def outp(g):
                x32, z = X32[g % 2], Z[g % 2]
                for c in range(8):
                    po = PS[4 + (c % 2)]
                    for k in range(4):
                        mm(po.t[:, 0:G], WO.t[:, k, c * 128:(c + 1) * 128], OT.t[:, k, :], k == 0, k == 3, WO.rl + [OT.r], [po.r])
                    stt(z.t[:, c, :], x32.t[:, c, :], ALPHA, po.t[:, 0:G], ALU.mult, ALU.add, [x32.r, po.r], [z.r])

            NG = S // G
            load(0)
            if NG > 1:
                load(1)
            attn(0)
            outp(0)
            for g in range(NG):
                if g + 2 < NG:
                    load(g + 2)
                if g + 1 < NG:
                    attn(g + 1)
                ln_part1(Z[g % 2], SQ, G, PS[6])
                if g + 1 < NG:
                    outp(g + 1)
                ln_part2(Z[g % 2], SQ, RS, G, gT, bT, dst, r_dst, g * G, PS[6])
            P.barrier()

    P.barrier()
    cur, r_cur = xT, r_xT
    nph = [0]

    def run(f, *a):
        if nph[0] < stop_after:
            f(*a)
        nph[0] += 1
    for l in range(L):
        last = (l == L - 1)
        run(ffn_phase, l, 0, 0, cur, r_cur, XB, r_XB)
        run(proj_phase, l, XB, r_XB)
        run(dsa_phase, l)
        run(sb_phase, l)
        run(merge_phase, l, XB, r_XB, XA, r_XA)
        run(xattn_phase, l, XA, r_XA, XB, r_XB)
        if last:
            run(ffn_phase, l, 1, 3, XB, r_XB, outT, r_outT)
        else:
            run(ffn_phase, l, 1, 3, XB, r_XB, XA, r_XA)
        cur, r_cur = XA, r_XA
    P.emit()
    top.close()
    return nc, P


def _bucket_table():
    import math
    n = np.arange(256)
    nf = np.maximum(n, 1).astype(np.float32)
    large = 16 + (np.log(nf / np.float32(16)) / np.float32(math.log(8.0)) * np.float32(16)).astype(np.int32)
    large = np.minimum(large, 31)
    return np.where(n < 16, n, large)


def prep_inputs(x, mem, ln_g, ln_b, ffn_w_in, ffn_w_out, w_mix_in, b_gate, kv_norm_g, w_uk, w_uv,
                conv_w, w_branch, w_mix_out, xa_wq, xa_wkv, xa_wo, rel_bias):
    f = lambda a: np.ascontiguousarray(np.asarray(a, dtype=np.float32))
    B, S, _ = x.shape
    L = ln_g.shape[0]
    bt = _bucket_table()
    tl = np.arange(128)[:, None]
    col = np.arange(256)[None, :]
    nrel = np.where(col < 128, tl - col + 128, tl - (col - 128))
    idx = bt[np.clip(nrel, 0, 255)]
    rel_bias = f(rel_bias)
    biasT = f(np.transpose(rel_bias[idx], (0, 2, 1)))
    shared = {
        "lng": f(np.transpose(f(ln_g).reshape(L, 4, 8, 128), (0, 1, 3, 2))),
        "lnb": f(np.transpose(f(ln_b).reshape(L, 4, 8, 128), (0, 1, 3, 2))),
        "ffn_w_in": f(ffn_w_in), "ffn_w_out": f(ffn_w_out), "w_mix_in": f(w_mix_in),
        "bgate": f(np.transpose(f(b_gate).reshape(L, 3, 8, 128), (0, 1, 3, 2))),
        "kvg": f(kv_norm_g),
        "wuk": f(np.transpose(f(w_uk).reshape(L, 4, 2, 64, 128), (0, 2, 3, 1, 4)).reshape(L, 128, 4, 128)),
        "wuv": f(w_uv),
        "convw": f(np.transpose(f(conv_w).reshape(L, 3, 4, 128), (0, 3, 2, 1))),
        "w_branch": f(w_branch), "w_mix_out": f(w_mix_out),
        "xa_wq": f(xa_wq), "xa_wkv": f(xa_wkv), "xa_wo": f(xa_wo),
        "biasT": biasT, "rb31": f(rel_bias[31]),
    }
    maps = []
    for b in range(B):
        m = dict(shared)
        m["xT"] = f(f(x[b]).T.reshape(8, 128, S))
        m["memT"] = f(f(mem[b]).T.reshape(8, 128, 256))
        maps.append(m)
    return maps


_NC_CACHE = {}


def kernel(**inputs):
    x = np.asarray(inputs["x"])
    B, S, _ = x.shape
    L = np.asarray(inputs["ln_g"]).shape[0]
    key = (S, L)
    if key not in _NC_CACHE:
        _NC_CACHE[key] = build(S, L)[0]
    nc = _NC_CACHE[key]
    maps = prep_inputs(**inputs)
    res = run_bass_kernel_spmd(nc, maps, core_ids=list(range(B)))
    out = np.empty((B, S, D), dtype=np.float32)
    for b in range(B):
        out[b] = np.asarray(res.results[b]["outT"]).reshape(D, S).T
    return out
```

```python
import numpy as np
from contextlib import ExitStack
import concourse.bass as bass
import concourse.mybir as mybir
from concourse.bass_utils import run_bass_kernel_spmd

F32 = mybir.dt.float32
BF16 = mybir.dt.bfloat16
AF = mybir.ActivationFunctionType
ALU = mybir.AluOpType
AX = mybir.AxisListType


class Res:
    __slots__ = ("name", "w", "r", "excl", "dram")

    def __init__(self, name, excl=False):
        self.name = name
        self.w = None
        self.r = {}
        self.excl = excl
        self.dram = False


class Prog:
    ENG = ("pe", "act", "dve", "pool", "sp")

    def __init__(self, nc, n_slots=12):
        self.nc = nc
        self.ops = {e: [] for e in self.ENG}
        self.cnt = {e: 0 for e in self.ENG}
        self.seen = {e: {} for e in self.ENG}
        self.n_slots = n_slots
        self.slot_uses = {q: [0] * n_slots for q in ("sp", "pool", "act")}
        self.slot_rr = {q: 0 for q in ("sp", "pool", "act")}
        self.pe_pending = False
        self.nops = 0

    def _deps(self, eng, reads, writes):
        deps = {}

        def add(kv):
            if kv is None:
                return
            k, v = kv
            if deps.get(k, 0) < v:
                deps[k] = v
        for r in reads:
            add(r.w)
            if r.excl:
                for k, v in r.r.items():
                    add((k, v))
        for w in writes:
            add(w.w)
            for k, v in w.r.items():
                add((k, v))
        out = []
        seen = self.seen[eng]
        for k, v in deps.items():
            if k == "pe" and eng == "pe":
                continue
            if seen.get(k, 0) >= v:
                continue
            seen[k] = v
            out.append((k, v))
        return out

    def _register(self, key, val, reads, writes):
        for r in reads:
            if r.excl:
                r.w = (key, val)
                r.r = {}
            else:
                if r.r.get(key, 0) < val:
                    r.r[key] = val
        for w in writes:
            w.w = (key, val)
            w.r = {}

    def op(self, eng, emit, reads=(), writes=(), inc=True):
        waits = self._deps(eng, reads, writes)
        if inc:
            self.cnt[eng] += 1
            val = self.cnt[eng]
            if eng == "pe":
                self.pe_pending = False
        else:
            assert eng == "pe"
            val = self.cnt[eng] + 1
            self.pe_pending = True
        self._register(eng, val, reads, writes)
        self.ops[eng].append((waits, emit, (eng, 1) if inc else None))
        self.nops += 1

    def dma(self, q, emit, reads=(), writes=()):
        j = self.slot_rr[q]
        self.slot_rr[q] = (j + 1) % self.n_slots
        n = self.slot_uses[q][j]
        key = ("dma", q, j)
        waits = self._deps(q, reads, writes)
        if n > 0 and self.seen[q].get(key, 0) < 16 * n:
            self.seen[q][key] = 16 * n
            waits.append((key, 16 * n))
        self.slot_uses[q][j] = n + 1
        val = 16 * (n + 1)
        self._register(key, val, reads, writes)
        self.ops[q].append((waits, emit, (key, 16)))
        self.nops += 1

    def barrier(self):
        assert not self.pe_pending
        targets = [(e, self.cnt[e]) for e in ("pe", "act", "dve", "pool") if self.cnt[e] > 0]
        for q in self.slot_uses:
            for j, n in enumerate(self.slot_uses[q]):
                if n > 0:
                    targets.append((("dma", q, j), 16 * n))
        for e in self.ENG:
            waits = []
            for k, v in targets:
                if k == "pe" and e == "pe":
                    continue
                if self.seen[e].get(k, 0) < v:
                    self.seen[e][k] = v
                    waits.append((k, v))
            self.ops[e].append((waits, None, None))

    def wait_all(self, eng, resources):
        waits = self._deps(eng, resources, ())
        self.ops[eng].append((waits, None, None))

    def emit(self):
        nc = self.nc
        keys = set(self.ENG)
        for q in self.slot_uses:
            for j in range(self.n_slots):
                if self.slot_uses[q][j] > 0:
                    keys.add(("dma", q, j))
        with ExitStack() as es:
            sems = {}
            for k in sorted(keys, key=str):
                nm = k if isinstance(k, str) else "d_%s_%d" % (k[1], k[2])
                sems[k] = es.enter_context(nc.semaphore("s_" + nm))
            block = es.enter_context(nc.Block())

            def run(e, name):
                for waits, emit, inc in self.ops[name]:
                    for k, v in waits:
                        e.wait_ge(sems[k], v)
                    if emit is None:
                        continue
                    ins = emit(e)
                    if inc is not None:
                        ins.then_inc(sems[inc[0]], inc[1])

            @block.tensor
            def _(e):
                run(e, "pe")

            @block.scalar
            def _(e):
                run(e, "act")

            @block.vector
            def _(e):
                run(e, "dve")

            @block.gpsimd
            def _(e):
                run(e, "pool")

            @block.sync
            def _(e):
                run(e, "sp")

D = 1024
DFF = 2816
PMIX = 7368
ALPHA = 4.0 ** 0.25
EPS = 1e-5
KTOP = 256
NIT = 14
NEG = -1.0e30


class Tl:
    def __init__(self, t, name, excl=False):
        self.t = t
        self.r = Res(name, excl)
        self.rl = [self.r]

    def __getitem__(self, k):
        return self.t[k]


def build(S=4096, L=2, dbg=False, stop_after=99):
    NT = S // 128
    nc = bass.Bass("TRN2", target_bir_lowering=False)
    P = Prog(nc)

    def din(name, shape, dt=F32):
        return nc.dram_tensor(name, list(shape), dt, kind="ExternalInput").ap()

    def dres(name):
        r = Res(name)
        r.dram = True
        return r

    def dscr(name, shape, dt):
        return nc.dram_tensor(name, list(shape), dt, kind=("ExternalOutput" if dbg else "Internal")).ap(), dres(name)

    xT = din("xT", [8, 128, S]); r_xT = dres("xT")
    memT = din("memT", [8, 128, 256])
    lng = din("lng", [L, 4, 128, 8]); lnb = din("lnb", [L, 4, 128, 8])
    w_in = din("ffn_w_in", [L, 2, D, 2 * DFF]); w_out = din("ffn_w_out", [L, 2, DFF, D])
    w_mix = din("w_mix_in", [L, D, PMIX])
    bgate = din("bgate", [L, 3, 128, 8])
    kvg = din("kvg", [L, 128])
    wuk = din("wuk", [L, 128, 4, 128])
    wuv = din("wuv", [L, 8, 128, 64])
    convw = din("convw", [L, 128, 4, 3])
    wbr = din("w_branch", [L, 3, 512, D])
    wmo = din("w_mix_out", [L, D, D])
    xwq = din("xa_wq", [L, D, 512]); xwkv = din("xa_wkv", [L, D, D]); xwo = din("xa_wo", [L, 512, D])
    biasT = din("biasT", [128, 8, 256])
    rb31 = din("rb31", [8])
    outT = nc.dram_tensor("outT", [8, 128, S], F32, kind="ExternalOutput").ap(); r_outT = dres("outT")
    r_const = dres("const_in")

    XA, r_XA = dscr("XA", [8, 128, S], F32)
    XB, r_XB = dscr("XB", [8, 128, S], F32)
    QL, r_QL = dscr("QL", [8, 128, S], BF16)
    IQT, r_IQT = dscr("IQT", [4, 128, S], BF16)
    IKT, r_IKT = dscr("IKT", [64, S], BF16)
    QST, r_QST = dscr("QST", [4, 128, S], BF16)
    KST, r_KST = dscr("KST", [4, 128, S], BF16)
    VS, r_VS = dscr("VS", [S, 512], BF16)
    CKV, r_CKV = dscr("CKV", [S, 128], BF16)
    CKVT, r_CKVT = dscr("CKVT", [128, S], BF16)
    IW, r_IW = dscr("IW", [S, 8], F32)
    YCT, r_YCT = dscr("YCT", [4, 128, S], BF16)
    YAT, r_YAT = dscr("YAT", [4, 128, S], BF16)
    YBT, r_YBT = dscr("YBT", [4, 128, S], BF16)

    def mm(out, lhsT, rhs, start, stop, reads, writes, inc=None):
        P.op("pe", lambda e: e.matmul(out, lhsT=lhsT, rhs=rhs, start=start, stop=stop),
             reads, writes, inc=(stop if inc is None else inc))

    def tr(out, in_, ident, reads, writes, inc=True):
        P.op("pe", lambda e: e.transpose(out=out, in_=in_, identity=ident), reads, writes, inc=inc)

    def act(out, in_, func, reads, writes, bias=None, scale=None, accum=None):
        kw = {}
        if bias is not None:
            kw["bias"] = bias
        if scale is not None:
            kw["scale"] = scale
        if accum is not None:
            kw["accum_out"] = accum
        P.op("act", lambda e: e.activation(out=out, in_=in_, func=func, **kw), reads, writes)

    def tt(eng, out, a, b, op, reads, writes):
        P.op(eng, lambda e: e.tensor_tensor(out=out, in0=a, in1=b, op=op), reads, writes)

    def ts(eng, out, a, s1, op0, reads, writes, s2=None, op1=None, accum=None):
        kw = {}
        if op1 is not None:
            kw["op1"] = op1
        if accum is not None:
            kw["accum_out"] = accum
        P.op(eng, lambda e: e.tensor_scalar(out=out, in0=a, scalar1=s1, scalar2=s2, op0=op0, **kw), reads, writes)

    def stt(out, in0, scalar, in1, op0, op1, reads, writes, accum=None):
        kw = {}
        if accum is not None:
            kw["accum_out"] = accum
        P.op("dve", lambda e: e.scalar_tensor_tensor(out=out, in0=in0, scalar=scalar, in1=in1, op0=op0, op1=op1, **kw),
             reads, writes)

    def cp(eng, out, in_, reads, writes):
        if eng == "act":
            P.op("act", lambda e: e.copy(out=out, in_=in_), reads, writes)
        else:
            P.op(eng, lambda e: e.tensor_copy(out=out, in_=in_), reads, writes)

    def red(out, in_, op, reads, writes):
        P.op("dve", lambda e: e.tensor_reduce(out=out, in_=in_, axis=AX.X, op=op), reads, writes)

    def recip(out, in_, reads, writes):
        P.op("dve", lambda e: e.reciprocal(out=out, in_=in_), reads, writes)

    def memset(eng, out, val, writes):
        P.op(eng, lambda e: e.memset(out, val), (), writes)

    def dma(q, out, in_, reads, writes, **kw):
        reads = [r for r in reads if not r.dram]
        writes = [r for r in writes if not r.dram]
        P.dma(q, lambda e: e.dma_start(out=out, in_=in_, **kw), reads, writes)

    def wload(dst, k_chunks, src2d, c0, c1, rows0=0):
        dst.rl = [Res("%s_k%d" % (dst.r.name, k)) for k in range(k_chunks)]
        for k in range(k_chunks):
            dma("pool", dst.t[:, k, 0:c1 - c0], src2d[rows0 + k * 128: rows0 + (k + 1) * 128, c0:c1],
                [r_const] + ([dst.rl[k - 1]] if k > 0 else []), [dst.rl[k]], max_dma_last_dim=8192)

    top = ExitStack()

    uid = [0]

    def alloc(stack, name, shape, dt):
        uid[0] += 1
        name = "%s_%d" % (name, uid[0])
        return Tl(stack.enter_context(nc.sbuf_tensor(name, list(shape), dt)), name)

    PS = [Tl(top.enter_context(nc.psum_tensor("ps%d" % i, [128, 512], F32)), "ps%d" % i, True) for i in range(7)]
    PB = Tl(top.enter_context(nc.psum_tensor("pb", [128, 1024], BF16)), "pb", True)

    identf = alloc(top, "identf", [128, 128], F32)
    ident = alloc(top, "ident", [128, 128], BF16)
    onesM = alloc(top, "onesM", [128, 128], F32)
    onesB = alloc(top, "onesB", [128, 128], BF16)
    caus = alloc(top, "caus", [128, 128], F32)
    strict = alloc(top, "strict", [128, 128], F32)
    pow2 = alloc(top, "pow2", [128, NIT], F32)
    neglo = alloc(top, "neglo", [128, 1], F32)
    epsT = alloc(top, "epsT", [128, 1], F32)
    oneT = alloc(top, "oneT", [128, 1], F32)
    memset("pool", identf.t[:], 0.0, [identf.r])
    P.op("pool", lambda e: e.affine_select(out=identf.t[:], in_=identf.t[:], pattern=[[-1, 128]],
                                           compare_op=ALU.not_equal, fill=1.0, base=0, channel_multiplier=1),
         [identf.r], [identf.r])
    cp("dve", ident.t[:], identf.t[:], [identf.r], [ident.r])
    memset("pool", onesM.t[:], 1.0 / D, [onesM.r])
    memset("pool", onesB.t[:], 1.0, [onesB.r])
    memset("pool", caus.t[:], 0.0, [caus.r])
    P.op("pool", lambda e: e.affine_select(out=caus.t[:], in_=caus.t[:], pattern=[[-1, 128]],
                                           compare_op=ALU.is_ge, fill=NEG, base=0, channel_multiplier=1),
         [caus.r], [caus.r])
    memset("pool", strict.t[:], 1.0, [strict.r])
    P.op("pool", lambda e: e.affine_select(out=strict.t[:], in_=strict.t[:], pattern=[[-1, 128]],
                                           compare_op=ALU.is_gt, fill=0.0, base=0, channel_multiplier=1),
         [strict.r], [strict.r])
    for j in range(NIT):
        memset("pool", pow2.t[:, j:j + 1], 0.5 ** (j + 1), [pow2.r])
    memset("pool", neglo.t[:], -1.0e29, [neglo.r])
    memset("pool", epsT.t[:], EPS, [epsT.r])
    memset("pool", oneT.t[:], 1.0, [oneT.r])

    def ln_part1(Z, SQ, G, pa):
        for c in range(8):
            mm(pa.t[:, 0:G], onesM.t[:], Z.t[:, c, :], c == 0, c == 7, [onesM.r, Z.r], [pa.r])
        for c in range(8):
            tt("dve", Z.t[:, c, :], Z.t[:, c, :], pa.t[:, 0:G], ALU.subtract, [Z.r, pa.r], [Z.r])
        act(SQ.t[:], Z.t[:], AF.Square, [Z.r], [SQ.r])

    def ln_part2(Z, SQ, RS, G, gT, bT, dst, r_dst, g0, pb_):
        for c in range(8):
            mm(pb_.t[:, 0:G], onesM.t[:], SQ.t[:, c, :], c == 0, c == 7, [onesM.r, SQ.r], [pb_.r])
        act(RS.t[:], pb_.t[:, 0:G], AF.Sqrt, [pb_.r, epsT.r], [RS.r], bias=epsT.t[:, 0:1])
        recip(RS.t[:], RS.t[:], [RS.r], [RS.r])
        for c in range(8):
            tt("pool", Z.t[:, c, :], Z.t[:, c, :], RS.t[:], ALU.mult, [Z.r, RS.r], [Z.r])
        for c in range(8):
            act(SQ.t[:, c, :], Z.t[:, c, :], AF.Identity, [Z.r, gT.r, bT.r], [SQ.r],
                scale=gT.t[:, c:c + 1], bias=bT.t[:, c:c + 1])
        dma("sp", dst[:, :, g0:g0 + G].rearrange("c p t -> p c t"), SQ.t[:], [SQ.r], [r_dst])

    def layer_norm(Z, SQ, RS, G, gT, bT, dst, r_dst, g0, pa, pb_):
        ln_part1(Z, SQ, G, pa)
        ln_part2(Z, SQ, RS, G, gT, bT, dst, r_dst, g0, pb_)

    def ffn_phase(l, which, ln_i, src, r_src, dst, r_dst):
        G = 256
        NG = S // G
        with ExitStack() as st:
            Win = alloc(st, "Win", [128, 8, 2 * DFF], BF16)
            Wout = alloc(st, "Wout", [128, 22, D], BF16)
            gT = alloc(st, "gT", [128, 8], F32); bT = alloc(st, "bT", [128, 8], F32)
            X32 = [alloc(st, "X32%d" % b, [128, 8, G], F32) for b in range(2)]
            Xb = [alloc(st, "Xb%d" % b, [128, 8, G], BF16) for b in range(2)]
            H = alloc(st, "H", [128, 22, G], BF16)
            SA = [alloc(st, "SA%d" % i, [128, G], F32) for i in range(2)]
            Z = [alloc(st, "Z%d" % b, [128, 8, G], F32) for b in range(2)]
            SQ = alloc(st, "SQ", [128, 8, G], F32)
            RS = alloc(st, "RS", [128, G], F32)
            dma("sp", gT.t[:], lng[l, ln_i], [r_const], [gT.r])
            dma("sp", bT.t[:], lnb[l, ln_i], [r_const], [bT.r])
            wload(Win, 8, w_in[l, which], 0, 2 * DFF)
            wload(Wout, 22, w_out[l, which], 0, D)

            def load(g):
                b = g % 2
                g0 = g * G
                dma("sp", X32[b].t[:], src[:, :, g0:g0 + G].rearrange("c p t -> p c t"), [r_src], [X32[b].r])
                cp("pool", Xb[b].t[:], X32[b].t[:], [X32[b].r], [Xb[b].r])

            def inproj(g):
                xb = Xb[g % 2]
                for j in range(22):
                    pa = PS[(2 * j) % 4]; pb_ = PS[(2 * j + 1) % 4]
                    for k in range(8):
                        mm(pa.t[:, 0:G], Win.t[:, k, j * 128:(j + 1) * 128], xb.t[:, k, :], k == 0, k == 7,
                           Win.rl + [xb.r], [pa.r])
                    for k in range(8):
                        mm(pb_.t[:, 0:G], Win.t[:, k, DFF + j * 128:DFF + (j + 1) * 128], xb.t[:, k, :], k == 0, k == 7,
                           Win.rl + [xb.r], [pb_.r])
                    sa = SA[j % 2]
                    act(sa.t[:], pa.t[:, 0:G], AF.Silu, [pa.r], [sa.r])
                    stt(H.t[:, j, :], sa.t[:], 0.5, pb_.t[:, 0:G], ALU.mult, ALU.mult, [sa.r, pb_.r], [H.r])

            def outproj(g):
                x32, z = X32[g % 2], Z[g % 2]
                for c in range(8):
                    po = PS[4 + (c % 2)]
                    for j in range(22):
                        mm(po.t[:, 0:G], Wout.t[:, j, c * 128:(c + 1) * 128], H.t[:, j, :], j == 0, j == 21,
                           Wout.rl + [H.r], [po.r])
                    stt(z.t[:, c, :], x32.t[:, c, :], ALPHA, po.t[:, 0:G], ALU.mult, ALU.add, [x32.r, po.r], [z.r])

            load(0)
            if NG > 1:
                load(1)
            inproj(0)
            outproj(0)
            for g in range(NG):
                if g + 2 < NG:
                    load(g + 2)
                if g + 1 < NG:
                    inproj(g + 1)
                ln_part1(Z[g % 2], SQ, G, PS[6])
                if g + 1 < NG:
                    outproj(g + 1)
                ln_part2(Z[g % 2], SQ, RS, G, gT, bT, dst, r_dst, g * G, PS[6])
            P.barrier()

    C_QA, C_CKV, C_IQ, C_IK, C_IW, C_QS, C_KS, C_VS, C_CB, C_CC, C_CH, C_G = \
        0, 512, 640, 1152, 1216, 1224, 1736, 2248, 2760, 3272, 3784, 4296

    def proj_phase(l, src, r_src):
        G = 512
        NW = C_G
        with ExitStack() as st:
            Wm = alloc(st, "Wm", [128, 8, NW], BF16)
            WUK = alloc(st, "WUK", [128, 4, 128], BF16)
            CW = alloc(st, "CW", [128, 4, 3], F32)
            KVG = alloc(st, "KVG", [128, 128], F32)
            X32 = alloc(st, "X32", [128, 8, G], F32)
            Xb = alloc(st, "Xb", [128, 8, G], BF16)
            QA = alloc(st, "QA", [128, 4, G], BF16)
            OB = [alloc(st, "OB%d" % i, [128, G], BF16) for i in range(3)]
            CCs = alloc(st, "CCs", [128, G], F32)
            ZC = [alloc(st, "ZC%d" % q, [128, G + 2], F32) for q in range(4)]
            YC = alloc(st, "YC", [128, G], F32)
            JK = alloc(st, "JK", [128, 128], F32)
            SS = alloc(st, "SS", [128, 2], F32)
            CKb = alloc(st, "CKb", [128, 128], BF16)
            CKTb = alloc(st, "CKTb", [128, 128], BF16)
            IWs = alloc(st, "IWs", [128, 8], F32)
            VSb = alloc(st, "VSb", [128, 512], BF16)
            wload(Wm, 8, w_mix[l], 0, NW)
            dma("pool", WUK.t[:], wuk[l], [r_const], [WUK.r])
            dma("sp", CW.t[:], convw[l], [r_const], [CW.r])
            dma("sp", KVG.t[:], kvg[l].partition_broadcast(128), [r_const], [KVG.r])
            for q in range(4):
                memset("pool", ZC[q].t[:, 0:2], 0.0, [ZC[q].r])
            rot = [0]

            def nextps():
                rot[0] = (rot[0] + 1) % 5
                return PS[rot[0]]
            obr = [0]

            def nextob():
                obr[0] = (obr[0] + 1) % 3
                return OB[obr[0]]

            def fm_chunk(col, M=128):
                ps = nextps()
                for k in range(8):
                    mm(ps.t[0:M, 0:G], Wm.t[:, k, col:col + M], Xb.t[:, k, :], k == 0, k == 7, Wm.rl + [Xb.r], [ps.r])
                return ps

            for g in range(S // G):
                g0 = g * G
                dma("sp", X32.t[:], src[:, :, g0:g0 + G].rearrange("c p t -> p c t"), [r_src], [X32.r])
                cp("pool", Xb.t[:], X32.t[:], [X32.r], [Xb.r])
                for q in range(4):
                    ps = fm_chunk(C_QA + q * 128)
                    cp("act", QA.t[:, q, :], ps.t[:, 0:G], [ps.r], [QA.r])
                for h in range(8):
                    p0 = (h % 2) * 64
                    ps = nextps()
                    mm(ps.t[:, 0:G], WUK.t[p0:p0 + 64, h // 2, :], QA.t[p0:p0 + 64, h // 2, :], True, True,
                       [WUK.r, QA.r], [ps.r])
                    ob = nextob()
                    act(ob.t[:], ps.t[:, 0:G], AF.Copy, [ps.r], [ob.r], scale=0.125)
                    dma("sp", QL[h, :, g0:g0 + G], ob.t[:], [ob.r], [r_QL])
                for (col, dstT, r_d, sc) in ((C_IQ, IQT, r_IQT, 1.0), (C_QS, QST, r_QST, 0.125), (C_KS, KST, r_KST, 1.0)):
                    for q in range(4):
                        ps = fm_chunk(col + q * 128)
                        ob = nextob()
                        act(ob.t[:], ps.t[:, 0:G], AF.Copy, [ps.r], [ob.r], scale=sc)
                        dma("sp", dstT[q, :, g0:g0 + G], ob.t[:], [ob.r], [r_d])
                ps = fm_chunk(C_IK, 64)
                ob = nextob()
                cp("act", ob.t[0:64, :], ps.t[0:64, 0:G], [ps.r], [ob.r])
                dma("sp", IKT[:, g0:g0 + G], ob.t[0:64, :], [ob.r], [r_IKT])
                for q in range(4):
                    ps = fm_chunk(C_CC + q * 128)
                    cp("act", CCs.t[:], ps.t[:, 0:G], [ps.r], [CCs.r])
                    ps = fm_chunk(C_CH + q * 128)
                    zc = ZC[q]
                    tt("dve", zc.t[:, 2:G + 2], CCs.t[:], ps.t[:, 0:G], ALU.mult, [CCs.r, ps.r], [zc.r])
                    ts("dve", YC.t[:], zc.t[:, 2:G + 2], CW.t[:, q, 2:3], ALU.mult, [zc.r, CW.r], [YC.r])
                    stt(YC.t[:], zc.t[:, 1:G + 1], CW.t[:, q, 1:2], YC.t[:], ALU.mult, ALU.add, [zc.r, CW.r, YC.r], [YC.r])
                    stt(YC.t[:], zc.t[:, 0:G], CW.t[:, q, 0:1], YC.t[:], ALU.mult, ALU.add, [zc.r, CW.r, YC.r], [YC.r])
                    ps = fm_chunk(C_CB + q * 128)
                    ob = nextob()
                    tt("dve", ob.t[:], YC.t[:], ps.t[:, 0:G], ALU.mult, [YC.r, ps.r], [ob.r])
                    dma("sp", YCT[q, :, g0:g0 + G], ob.t[:], [ob.r], [r_YCT])
                    cp("pool", zc.t[:, 0:2], zc.t[:, G:G + 2], [zc.r], [zc.r])
                for t4 in range(G // 128):
                    t0 = g0 + t4 * 128
                    xs = slice(t4 * 128, (t4 + 1) * 128)
                    ps = nextps()
                    for k in range(8):
                        mm(ps.t[:, 0:128], Xb.t[:, k, xs], Wm.t[:, k, C_CKV:C_CKV + 128], k == 0, k == 7,
                           Wm.rl + [Xb.r], [ps.r])
                    act(JK.t[:], ps.t[:, 0:128], AF.Square, [ps.r], [JK.r, SS.r], accum=SS.t[:, 0:1])
                    act(SS.t[:, 1:2], SS.t[:, 0:1], AF.Sqrt, [SS.r, epsT.r], [SS.r], scale=1.0 / 128, bias=epsT.t[:, 0:1])
                    recip(SS.t[:, 1:2], SS.t[:, 1:2], [SS.r], [SS.r])
                    stt(CKb.t[:], ps.t[:, 0:128], SS.t[:, 1:2], KVG.t[:], ALU.mult, ALU.mult, [ps.r, SS.r, KVG.r], [CKb.r])
                    dma("sp", CKV[t0:t0 + 128, :], CKb.t[:], [CKb.r], [r_CKV])
                    tr(PB.t[:, 0:128], CKb.t[:], ident.t[:], [CKb.r, ident.r], [PB.r])
                    cp("act", CKTb.t[:], PB.t[:, 0:128], [PB.r], [CKTb.r])
                    dma("sp", CKVT[:, t0:t0 + 128], CKTb.t[:], [CKTb.r], [r_CKVT])
                    ps = nextps()
                    for k in range(8):
                        mm(ps.t[:, 0:8], Xb.t[:, k, xs], Wm.t[:, k, C_IW:C_IW + 8], k == 0, k == 7, Wm.rl + [Xb.r], [ps.r])
                    cp("act", IWs.t[:], ps.t[:, 0:8], [ps.r], [IWs.r])
                    dma("sp", IW[t0:t0 + 128, :], IWs.t[:], [IWs.r], [r_IW])
                    ps = nextps()
                    for k in range(8):
                        mm(ps.t[:, 0:512], Xb.t[:, k, xs], Wm.t[:, k, C_VS:C_VS + 512], k == 0, k == 7, Wm.rl + [Xb.r], [ps.r])
                    cp("act", VSb.t[:], ps.t[:, 0:512], [ps.r], [VSb.r])
                    dma("sp", VS[t0:t0 + 128, :], VSb.t[:], [VSb.r], [r_VS])
            P.barrier()

    def chunks(n):
        return [(c0, min(512, n - c0)) for c0 in range(0, n, 512)]

    def dsa_phase(l):
        with ExitStack() as st:
            CKVX = alloc(st, "CKVX", [128, NT, 128], BF16)
            CKT = alloc(st, "CKT", [128, S], BF16)
            IK2 = alloc(st, "IK2", [128, S], BF16)
            BI = alloc(st, "BI", [128, 8, 256], F32)
            RB = alloc(st, "RB", [128, 8], F32)
            WUVP = alloc(st, "WUVP", [128, 8, 128], BF16)
            QLg = [alloc(st, "QLg%d" % b, [128, 8, 512], BF16) for b in range(2)]
            IQg = [alloc(st, "IQg%d" % b, [128, 4, 512], BF16) for b in range(2)]
            IWg = [alloc(st, "IWg%d" % b, [128, 4, 8], F32) for b in range(2)]
            WA = [alloc(st, "WA%d" % b, [128, 8], F32) for b in range(2)]
            SG = [alloc(st, "SG%d" % b, [128, 8], F32) for b in range(2)]
            SC = [alloc(st, "SC%d" % b, [128, S], F32) for b in range(2)]
            Mk = [alloc(st, "Mk%d" % b, [128, S], BF16) for b in range(2)]
            TH = [alloc(st, "TH%d" % b, [128, 8], F32) for b in range(2)]
            WALL = [alloc(st, "WALL%d" % b, [128, NIT], F32) for b in range(2)]
            PM = [alloc(st, "PM%d" % b, [128, S], BF16) for b in range(2)]
            PT = [alloc(st, "PT%d" % b, [128, NT, 128], BF16) for b in range(2)]
            TMP = [alloc(st, "TMP%d" % i, [128, 512], F32) for i in range(4)]
            JKB = alloc(st, "JKB", [128, S], BF16)
            RSM = alloc(st, "RSM", [128, 8, 8], F32)
            RSUM = alloc(st, "RSUM", [128, 8], F32)
            OLN = alloc(st, "OLN", [128, 8, 128], BF16)
            OLT = alloc(st, "OLT", [128, 8, 128], BF16)
            YAg = alloc(st, "YAg", [128, 4, 512], BF16)
            dma("sp", CKVX.t[:], CKV.rearrange("(j p) c -> p j c", p=128), [r_CKV], [CKVX.r])
            dma("sp", CKT.t[:], CKVT, [r_CKVT], [CKT.r])
            dma("sp", IK2.t[0:64, :], IKT, [r_IKT], [IK2.r])
            dma("sp", IK2.t[64:128, :], IKT, [r_IKT], [IK2.r])
            dma("sp", BI.t[:], biasT, [r_const], [BI.r])
            dma("sp", RB.t[:], rb31.partition_broadcast(128), [r_const], [RB.r])
            memset("pool", WUVP.t[:], 0.0, [WUVP.r])
            for h in range(8):
                p0 = (h % 2) * 64
                dma("pool", WUVP.t[:, h, p0:p0 + 64], wuv[l, h], [r_const], [WUVP.r])
            tmr = [0]

            def nexttmp():
                tmr[0] = (tmr[0] + 1) % 4
                return TMP[tmr[0]]
            psr = [0]

            def nextps():
                psr[0] = (psr[0] + 1) % 3
                return PS[psr[0]]

            def load_group(g):
                b = g % 2
                g0 = g * 512
                dma("sp", QLg[b].t[:], QL[:, :, g0:g0 + 512].rearrange("h c t -> c h t"), [r_QL], [QLg[b].r])
                dma("sp", IQg[b].t[:], IQT[:, :, g0:g0 + 512].rearrange("q p t -> p q t"), [r_IQT], [IQg[b].r])
                dma("sp", IWg[b].t[:], IW[g0:g0 + 512, :].rearrange("(a p) h -> p a h", p=128), [r_IW], [IWg[b].r])

            def index_scores(i):
                g, t4 = divmod(i, 4)
                gb = g % 2
                b = i % 2
                n = (i + 1) * 128
                tsl = slice(t4 * 128, (t4 + 1) * 128)
                sc, th, wall, wa, sg, mk = SC[b], TH[b], WALL[b], WA[b], SG[b], Mk[b]
                act(wa.t[:], IWg[gb].t[:, t4, :], AF.Abs, [IWg[gb].r], [wa.r])
                P.op("act", lambda e: e.sign(out=sg.t[:], in_=IWg[gb].t[:, t4, :]), [IWg[gb].r], [sg.r])
                for (c0, w) in chunks(n):
                    for h in range(8):
                        p0 = (h % 2) * 64
                        ps = nextps()
                        mm(ps.t[:, 0:w], IQg[gb].t[p0:p0 + 64, h // 2, tsl], IK2.t[p0:p0 + 64, c0:c0 + w], True, True,
                           [IQg[gb].r, IK2.r], [ps.r])
                        R = nexttmp()
                        act(R.t[:, 0:w], ps.t[:, 0:w], AF.Relu, [ps.r, wa.r], [R.r], scale=wa.t[:, h:h + 1])
                        if h == 0:
                            ts("dve", sc.t[:, c0:c0 + w], R.t[:, 0:w], sg.t[:, 0:1], ALU.mult, [R.r, sg.r], [sc.r])
                        else:
                            stt(sc.t[:, c0:c0 + w], R.t[:, 0:w], sg.t[:, h:h + 1], sc.t[:, c0:c0 + w], ALU.mult, ALU.add,
                                [R.r, sg.r, sc.r], [sc.r])
                tt("dve", sc.t[:, n - 128:n], sc.t[:, n - 128:n], caus.t[:], ALU.add, [sc.r, caus.r], [sc.r])
                steps = []
                if i >= 2:
                    def init():
                        red(th.t[:, 0:1], sc.t[:, 0:n - 128], ALU.min, [sc.r], [th.r])
                        red(th.t[:, 1:2], sc.t[:, 0:n], ALU.max, [sc.r], [th.r])
                        tt("dve", th.t[:, 2:3], th.t[:, 1:2], th.t[:, 0:1], ALU.subtract, [th.r], [th.r])
                        ts("dve", wall.t[:], pow2.t[:], th.t[:, 2:3], ALU.mult, [pow2.r, th.r], [wall.r])
                        tt("dve", th.t[:, 3:4], th.t[:, 0:1], wall.t[:, 0:1], ALU.add, [th.r, wall.r], [th.r])
                    steps.append(init)

                    def mkstep(j):
                        def step():
                            ts("dve", JKB.t[:, 0:n], sc.t[:, 0:n], th.t[:, 3:4], ALU.is_ge, [sc.r, th.r], [th.r],
                               op1=ALU.add, accum=th.t[:, 4:5])
                            ts("dve", th.t[:, 5:6], th.t[:, 4:5], KTOP - 0.5, ALU.is_ge, [th.r, wall.r], [th.r],
                               s2=wall.t[:, j:j + 1], op1=ALU.mult)
                            if j + 1 < NIT:
                                stt(th.t[:, 3:4], th.t[:, 5:6], wall.t[:, j + 1:j + 2], th.t[:, 3:4], ALU.subtract, ALU.add,
                                    [th.r, wall.r], [th.r])
                            else:
                                stt(th.t[:, 0:1], th.t[:, 5:6], wall.t[:, j:j + 1], th.t[:, 3:4], ALU.subtract, ALU.add,
                                    [th.r, wall.r], [th.r])
                        return step
                    for j in range(NIT):
                        steps.append(mkstep(j))
                    steps.append(lambda: ts("dve", mk.t[:, 0:n], sc.t[:, 0:n], th.t[:, 0:1], ALU.is_ge, [sc.r, th.r], [mk.r]))
                else:
                    steps.append(lambda: ts("dve", mk.t[:, 0:n], sc.t[:, 0:n], neglo.t[:, 0:1], ALU.is_ge, [sc.r, neglo.r], [mk.r]))
                return steps

            def heads(i, pending):
                g, t4 = divmod(i, 4)
                gb = g % 2
                n = (i + 1) * 128
                tsl = slice(t4 * 128, (t4 + 1) * 128)
                mk = Mk[i % 2]
                nb0 = max(0, n - 256)
                per_head = (len(pending) + 7) // 8
                for h in range(8):
                    pm, pt = PM[h % 2], PT[h % 2]
                    ch = chunks(n)
                    for ci, (c0, w) in enumerate(ch):
                        ps = nextps()
                        mm(ps.t[:, 0:w], QLg[gb].t[:, h, tsl], CKT.t[:, c0:c0 + w], True, True, [QLg[gb].r, CKT.r], [ps.r])
                        Pe = nexttmp()
                        fa, fb = c0, min(c0 + w, nb0)
                        na, nb_ = max(c0, nb0), c0 + w
                        if fb > fa:
                            act(Pe.t[:, fa - c0:fb - c0], ps.t[:, fa - c0:fb - c0], AF.Exp, [ps.r, RB.r], [Pe.r],
                                bias=RB.t[:, h:h + 1])
                        if nb_ > na:
                            bo = 256 - (n - na)
                            tt("dve", Pe.t[:, na - c0:nb_ - c0], ps.t[:, na - c0:nb_ - c0], BI.t[:, h, bo:bo + (nb_ - na)],
                               ALU.add, [ps.r, BI.r], [Pe.r])
                            act(Pe.t[:, na - c0:nb_ - c0], Pe.t[:, na - c0:nb_ - c0], AF.Exp, [Pe.r], [Pe.r])
                        stt(pm.t[:, c0:c0 + w], Pe.t[:, 0:w], 1.0, mk.t[:, c0:c0 + w], ALU.mult, ALU.mult,
                            [Pe.r, mk.r], [pm.r, RSM.r], accum=RSM.t[:, h, ci:ci + 1])
                    red(RSUM.t[:, h:h + 1], RSM.t[:, h, 0:len(ch)], ALU.add, [RSM.r], [RSUM.r])
                    recip(RSUM.t[:, h:h + 1], RSUM.t[:, h:h + 1], [RSUM.r], [RSUM.r])
                    for _ in range(per_head):
                        if pending:
                            pending.pop(0)()
                    for j0 in range(0, i + 1, 8):
                        nbk = min(8, i + 1 - j0)
                        for jj in range(nbk):
                            j = j0 + jj
                            tr(PB.t[:, jj * 128:(jj + 1) * 128], pm.t[:, j * 128:(j + 1) * 128], ident.t[:],
                               [pm.r, ident.r], [PB.r], inc=(jj == nbk - 1))
                        cp("act", pt.t[:, j0:j0 + nbk, :], PB.t[:, 0:nbk * 128].rearrange("p (j t) -> p j t", t=128),
                           [PB.r], [pt.r])
                    ol = PS[3 + (h % 2)]
                    for j in range(i + 1):
                        mm(ol.t[:, 0:128], pt.t[:, j, :], CKVX.t[:, j, :], j == 0, j == i, [pt.r, CKVX.r], [ol.r])
                    act(OLN.t[:, h, :], ol.t[:, 0:128], AF.Copy, [ol.r, RSUM.r], [OLN.r], scale=RSUM.t[:, h:h + 1])
                while pending:
                    pending.pop(0)()
                for h in range(8):
                    tr(PB.t[:, h * 128:(h + 1) * 128], OLN.t[:, h, :], ident.t[:], [OLN.r, ident.r], [PB.r], inc=(h == 7))
                cp("act", OLT.t[:], PB.t[:].rearrange("p (j t) -> p j t", t=128), [PB.r], [OLT.r])
                for q in range(4):
                    ps = PS[5]
                    mm(ps.t[:, 0:128], WUVP.t[:, 2 * q, :], OLT.t[:, 2 * q, :], True, False, [WUVP.r, OLT.r], [ps.r])
                    mm(ps.t[:, 0:128], WUVP.t[:, 2 * q + 1, :], OLT.t[:, 2 * q + 1, :], False, True, [WUVP.r, OLT.r], [ps.r])
                    cp("act", YAg.t[:, q, tsl], ps.t[:, 0:128], [ps.r], [YAg.r])
                if t4 == 3:
                    g0 = g * 512
                    dma("sp", YAT[:, :, g0:g0 + 512].rearrange("q p t -> p q t"), YAg.t[:], [YAg.r], [r_YAT])

            load_group(0)
            for s_ in index_scores(0):
                s_()
            for i in range(NT):
                pending = []
                if i + 1 < NT:
                    if (i + 1) % 4 == 0:
                        load_group((i + 1) // 4)
                    pending = index_scores(i + 1)
                heads(i, pending)
            P.barrier()

    def sb_phase(l):
        with ExitStack() as st:
            KS_ = alloc(st, "KS_", [128, 4, S], BF16)
            VSX = alloc(st, "VSX", [128, NT, 512], BF16)
            QSg = [alloc(st, "QSg%d" % b, [128, 4, 512], BF16) for b in range(2)]
            LB = [alloc(st, "LB%d" % b, [128, S + 1], F32) for b in range(2)]
            NLX = [alloc(st, "NLX", [128, S], F32)] * 2
            ZS = [alloc(st, "ZS%d" % b, [128, S], F32) for b in range(2)]
            AB = [alloc(st, "AB%d" % b, [128, S], BF16) for b in range(2)]
            PT = [alloc(st, "PT2%d" % b, [128, NT, 128], BF16) for b in range(2)]
            TMP = [alloc(st, "TMQ%d" % i, [128, 512], F32) for i in range(4)]
            TSM = [alloc(st, "TSM%d" % b, [128, 16], F32) for b in range(2)]
            TOT = [alloc(st, "TOT%d" % b, [128, 2], F32) for b in range(2)]
            YBs = alloc(st, "YBs", [128, 512], BF16)
            YBg = alloc(st, "YBg", [128, 4, 512], BF16)
            dma("sp", KS_.t[:], KST.rearrange("q p t -> p q t"), [r_KST], [KS_.r])
            dma("sp", VSX.t[:], VS.rearrange("(j p) c -> p j c", p=128), [r_VS], [VSX.r])
            for b in range(2):
                memset("pool", LB[b].t[:, 0:1], 0.0, [LB[b].r])
            tmr = [0]

            def nexttmp():
                tmr[0] = (tmr[0] + 1) % 4
                return TMP[tmr[0]]
            psr = [0]

            def nextps():
                psr[0] = (psr[0] + 1) % 4
                return PS[psr[0]]
            yb = PS[4]

            def pass1(i, h):
                g, t4 = divmod(i, 4)
                qs = QSg[g % 2]
                n = (i + 1) * 128
                tsl = slice(t4 * 128, (t4 + 1) * 128)
                p0 = (h % 2) * 64
                lb, tsm, tot, zs = LB[h % 2], TSM[h % 2], TOT[h % 2], ZS[h % 2]
                memset("dve", tsm.t[:], 0.0, [tsm.r])
                for ci, (c0, w) in enumerate(chunks(n)):
                    ps = nextps()
                    mm(ps.t[:, 0:w], qs.t[p0:p0 + 64, h // 2, tsl], KS_.t[p0:p0 + 64, h // 2, c0:c0 + w], True, True,
                       [qs.r, KS_.r], [ps.r])
                    E1 = nexttmp()
                    act(E1.t[:, 0:w], ps.t[:, 0:w], AF.Exp, [ps.r], [E1.r])
                    cp("dve", zs.t[:, c0:c0 + w], ps.t[:, 0:w], [ps.r], [zs.r])
                    last = (c0 + w == n)
                    wf = w - 128 if last else w
                    if wf > 0:
                        act(lb.t[:, 1 + c0:1 + c0 + wf], E1.t[:, 0:wf], AF.Ln, [E1.r, oneT.r], [lb.r, tsm.r],
                            bias=oneT.t[:, 0:1], accum=tsm.t[:, ci:ci + 1])
                    if last:
                        act(lb.t[:, 1 + n - 128:1 + n], E1.t[:, w - 128:w], AF.Ln, [E1.r, oneT.r], [lb.r], bias=oneT.t[:, 0:1])
                        tt("dve", lb.t[:, 1 + n - 128:1 + n], lb.t[:, 1 + n - 128:1 + n], strict.t[:], ALU.mult,
                           [lb.r, strict.r], [lb.r])
                        red(tsm.t[:, 15:16], lb.t[:, 1 + n - 128:1 + n], ALU.add, [lb.r], [tsm.r])
                red(tot.t[:, 0:1], tsm.t[:], ALU.add, [tsm.r], [tot.r])
                ts("dve", tot.t[:, 1:2], tot.t[:, 0:1], -1.0, ALU.mult, [tot.r], [tot.r])

            def pass2(i, h):
                g, t4 = divmod(i, 4)
                qs = QSg[g % 2]
                n = (i + 1) * 128
                tsl = slice(t4 * 128, (t4 + 1) * 128)
                p0 = (h % 2) * 64
                lb, tot, nlx, ab, pt = LB[h % 2], TOT[h % 2], NLX[h % 2], AB[h % 2], PT[h % 2]
                P.op("dve", lambda e: e.tensor_tensor_scan(out=nlx.t[:, 0:n], data0=lb.t[:, 0:n], data1=lb.t[:, 0:n],
                                                           initial=tot.t[:, 1:2], op0=ALU.add, op1=ALU.min),
                     [lb.r, tot.r], [nlx.r])
                zs = ZS[h % 2]
                for ci, (c0, w) in enumerate(chunks(n)):
                    EB = nexttmp()
                    tt("dve", EB.t[:, 0:w], zs.t[:, c0:c0 + w], nlx.t[:, c0:c0 + w], ALU.add, [zs.r, nlx.r], [EB.r])
                    act(ab.t[:, c0:c0 + w], EB.t[:, 0:w], AF.Exp, [EB.r], [ab.r])
                tt("dve", ab.t[:, n - 128:n], ab.t[:, n - 128:n], strict.t[:], ALU.mult, [ab.r, strict.r], [ab.r])
                for j0 in range(0, i + 1, 8):
                    nbk = min(8, i + 1 - j0)
                    for jj in range(nbk):
                        j = j0 + jj
                        tr(PB.t[:, jj * 128:(jj + 1) * 128], ab.t[:, j * 128:(j + 1) * 128], ident.t[:],
                           [ab.r, ident.r], [PB.r], inc=(jj == nbk - 1))
                    cp("act", pt.t[:, j0:j0 + nbk, :], PB.t[:, 0:nbk * 128].rearrange("p (j t) -> p j t", t=128),
                       [PB.r], [pt.r])
                for j in range(i + 1):
                    mm(yb.t[:, h * 64:(h + 1) * 64], pt.t[:, j, :], VSX.t[:, j, h * 64:(h + 1) * 64], j == 0, j == i,
                       [pt.r, VSX.r], [yb.r])

            def load_group(g):
                g0 = g * 512
                dma("sp", QSg[g % 2].t[:], QST[:, :, g0:g0 + 512].rearrange("q p t -> p q t"), [r_QST], [QSg[g % 2].r])

            load_group(0)
            pass1(0, 0)
            for i in range(NT):
                g, t4 = divmod(i, 4)
                tsl = slice(t4 * 128, (t4 + 1) * 128)
                for h in range(8):
                    if h < 7:
                        pass1(i, h + 1)
                    elif i + 1 < NT:
                        if (i + 1) % 4 == 0:
                            load_group((i + 1) // 4)
                        pass1(i + 1, 0)
                    pass2(i, h)
                cp("act", YBs.t[:], yb.t[:, 0:512], [yb.r], [YBs.r])
                for q in range(4):
                    tr(PB.t[:, q * 128:(q + 1) * 128], YBs.t[:, q * 128:(q + 1) * 128], ident.t[:], [YBs.r, ident.r], [PB.r], inc=(q == 3))
                cp("act", YBg.t[:, :, tsl], PB.t[:, 0:512].rearrange("p (q t) -> p q t", t=128), [PB.r], [YBg.r])
                if t4 == 3:
                    g0 = g * 512
                    dma("sp", YBT[:, :, g0:g0 + 512].rearrange("q p t -> p q t"), YBg.t[:], [YBg.r], [r_YBT])
            P.barrier()

    def merge_phase(l, src, r_src, dst, r_dst):
        G = 256
        NG = S // G
        with ExitStack() as st:
            WG = alloc(st, "WG", [128, 8, 3 * D], BF16)
            WBR = alloc(st, "WBR", [128, 12, D], BF16)
            WO = alloc(st, "WO", [128, 8, D], BF16)
            BG = alloc(st, "BG", [128, 3, 8], F32)
            gT = alloc(st, "gT", [128, 8], F32); bT = alloc(st, "bT", [128, 8], F32)
            X32 = [alloc(st, "X32%d" % b, [128, 8, G], F32) for b in range(2)]
            Xb = [alloc(st, "Xb%d" % b, [128, 8, G], BF16) for b in range(2)]
            Y = [[alloc(st, "Y%d_%d" % (i, b), [128, 4, G], BF16) for i in range(3)] for b in range(2)]
            SGt = [alloc(st, "SGt%d" % i, [128, G], F32) for i in range(2)]
            TM = [alloc(st, "TM%d" % i, [128, G], F32) for i in range(2)]
            MGf = alloc(st, "MGf", [128, 8, G], F32)
            MGb = alloc(st, "MGb", [128, 8, G], BF16)
            Z = [alloc(st, "Z%d" % b, [128, 8, G], F32) for b in range(2)]
            SQ = alloc(st, "SQ", [128, 8, G], F32)
            RS = alloc(st, "RS", [128, G], F32)
            dma("sp", gT.t[:], lng[l, 1], [r_const], [gT.r])
            dma("sp", bT.t[:], lnb[l, 1], [r_const], [bT.r])
            dma("sp", BG.t[:], bgate[l].rearrange("i p c -> p i c"), [r_const], [BG.r])
            wload(WG, 8, w_mix[l], C_G, PMIX)
            for i in range(3):
                for k in range(4):
                    dma("pool", WBR.t[:, i * 4 + k, :], wbr[l, i, k * 128:(k + 1) * 128, :], [r_const], [WBR.r], max_dma_last_dim=8192)
            wload(WO, 8, wmo[l], 0, D)
            srcs = ((YAT, r_YAT), (YBT, r_YBT), (YCT, r_YCT))

            def load(g):
                b = g % 2
                g0 = g * G
                dma("sp", X32[b].t[:], src[:, :, g0:g0 + G].rearrange("c p t -> p c t"), [r_src], [X32[b].r])
                cp("pool", Xb[b].t[:], X32[b].t[:], [X32[b].r], [Xb[b].r])
                for i in range(3):
                    dma("sp", Y[b][i].t[:], srcs[i][0][:, :, g0:g0 + G].rearrange("q p t -> p q t"), [srcs[i][1]], [Y[b][i].r])

            def stage1(g):
                xb, y = Xb[g % 2], Y[g % 2]
                n_ = 0
                for c in range(8):
                    for i in range(3):
                        pg = PS[n_ % 2]; pb_ = PS[2 + n_ % 2]; sg = SGt[n_ % 2]; tm = TM[n_ % 2]
                        n_ += 1
                        col = i * D + c * 128
                        for k in range(8):
                            mm(pg.t[:, 0:G], WG.t[:, k, col:col + 128], xb.t[:, k, :], k == 0, k == 7, WG.rl + [xb.r], [pg.r])
                        act(sg.t[:], pg.t[:, 0:G], AF.Sigmoid, [pg.r, BG.r], [sg.r], bias=BG.t[:, i, c:c + 1])
                        for k in range(4):
                            mm(pb_.t[:, 0:G], WBR.t[:, i * 4 + k, c * 128:(c + 1) * 128], y[i].t[:, k, :], k == 0, k == 3,
                               [WBR.r, y[i].r], [pb_.r])
                        if i == 0:
                            tt("dve", MGf.t[:, c, :], sg.t[:], pb_.t[:, 0:G], ALU.mult, [sg.r, pb_.r], [MGf.r])
                        else:
                            tt("dve", tm.t[:], sg.t[:], pb_.t[:, 0:G], ALU.mult, [sg.r, pb_.r], [tm.r])
                            tt("pool", MGf.t[:, c, :], MGf.t[:, c, :], tm.t[:], ALU.add, [MGf.r, tm.r], [MGf.r])
                cp("pool", MGb.t[:], MGf.t[:], [MGf.r], [MGb.r])

            def stage2(g):
                x32, z = X32[g % 2], Z[g % 2]
                for c in range(8):
                    po = PS[4 + (c % 2)]
                    for k in range(8):
                        mm(po.t[:, 0:G], WO.t[:, k, c * 128:(c + 1) * 128], MGb.t[:, k, :], k == 0, k == 7, WO.rl + [MGb.r], [po.r])
                    stt(z.t[:, c, :], x32.t[:, c, :], ALPHA, po.t[:, 0:G], ALU.mult, ALU.add, [x32.r, po.r], [z.r])

            load(0)
            if NG > 1:
                load(1)
            stage1(0)
            stage2(0)
            for g in range(NG):
                if g + 2 < NG:
                    load(g + 2)
                if g + 1 < NG:
                    stage1(g + 1)
                ln_part1(Z[g % 2], SQ, G, PS[6])
                if g + 1 < NG:
                    stage2(g + 1)
                ln_part2(Z[g % 2], SQ, RS, G, gT, bT, dst, r_dst, g * G, PS[6])
            P.barrier()

    def xattn_phase(l, src, r_src, dst, r_dst):
        G = 256
        with ExitStack() as st:
            WQ = alloc(st, "WQ", [128, 8, 512], BF16)
            WKV = alloc(st, "WKV", [128, 8, D], BF16)
            WO = alloc(st, "WOx", [128, 4, D], BF16)
            gT = alloc(st, "gT", [128, 8], F32); bT = alloc(st, "bT", [128, 8], F32)
            MT = alloc(st, "MT", [128, 8, 256], BF16)
            KT = alloc(st, "KT", [128, 4, 256], BF16)
            VX = alloc(st, "VX", [128, 2, 512], BF16)
            X32 = [alloc(st, "X32%d" % b_, [128, 8, G], F32) for b_ in range(2)]
            Xb = [alloc(st, "Xb%d" % b_, [128, 8, G], BF16) for b_ in range(2)]
            QT = alloc(st, "QT", [128, 4, G], BF16)
            PTm = [alloc(st, "PTm%d" % i, [128, G], BF16) for i in range(2)]
            RD = alloc(st, "RD", [128, G], F32)
            OT = alloc(st, "OT", [128, 4, G], BF16)
            Z = [alloc(st, "Z%d" % b_, [128, 8, G], F32) for b_ in range(2)]
            SQ = alloc(st, "SQ", [128, 8, G], F32)
            RS = alloc(st, "RS", [128, G], F32)
            dma("sp", gT.t[:], lng[l, 2], [r_const], [gT.r])
            dma("sp", bT.t[:], lnb[l, 2], [r_const], [bT.r])
            wload(WQ, 8, xwq[l], 0, 512)
            wload(WKV, 8, xwkv[l], 0, D)
            wload(WO, 4, xwo[l], 0, D)
            dma("pool", MT.t[:], memT.rearrange("c p m -> p c m"), [r_const], [MT.r])
            for h in range(4):
                ps = PS[h % 2]
                for k in range(8):
                    mm(ps.t[:, 0:256], WKV.t[:, k, h * 128:(h + 1) * 128], MT.t[:, k, :], k == 0, k == 7, WKV.rl + [MT.r], [ps.r])
                cp("act", KT.t[:, h, :], ps.t[:, 0:256], [ps.r], [KT.r])
            for mt in range(2):
                ps = PS[2 + mt]
                for k in range(8):
                    mm(ps.t[:, 0:512], MT.t[:, k, mt * 128:(mt + 1) * 128], WKV.t[:, k, 512:1024], k == 0, k == 7, WKV.rl + [MT.r], [ps.r])
                cp("act", VX.t[:, mt, :], ps.t[:, 0:512], [ps.r], [VX.r])
            def load(g):
                b_ = g % 2
                g0 = g * G
                dma("sp", X32[b_].t[:], src[:, :, g0:g0 + G].rearrange("c p t -> p c t"), [r_src], [X32[b_].r])
                cp("pool", Xb[b_].t[:], X32[b_].t[:], [X32[b_].r], [Xb[b_].r])

            def attn(g):
                xb = Xb[g % 2]
                for h in range(4):
                    ps = PS[h % 2]
                    for k in range(8):
                        mm(ps.t[:, 0:G], WQ.t[:, k, h * 128:(h + 1) * 128], xb.t[:, k, :], k == 0, k == 7, WQ.rl + [xb.r], [ps.r])
                    act(QT.t[:, h, :], ps.t[:, 0:G], AF.Copy, [ps.r], [QT.r], scale=128.0 ** -0.5)
                for h in range(4):
                    for mt in range(2):
                        ps = PS[mt]
                        mm(ps.t[:, 0:G], KT.t[:, h, mt * 128:(mt + 1) * 128], QT.t[:, h, :], True, True, [KT.r, QT.r], [ps.r])
                        act(PTm[mt].t[:], ps.t[:, 0:G], AF.Exp, [ps.r], [PTm[mt].r])
                    po = PS[2]; pd = PS[3]
                    for mt in range(2):
                        mm(po.t[:, 0:G], VX.t[:, mt, h * 128:(h + 1) * 128], PTm[mt].t[:], mt == 0, mt == 1, [VX.r, PTm[mt].r], [po.r])
                    for mt in range(2):
                        mm(pd.t[:, 0:G], onesB.t[:], PTm[mt].t[:], mt == 0, mt == 1, [onesB.r, PTm[mt].r], [pd.r])
                    recip(RD.t[:], pd.t[:, 0:G], [pd.r], [RD.r])
                    tt("dve", OT.t[:, h, :], po.t[:, 0:G], RD.t[:], ALU.mult, [po.r, RD.r], [OT.r])

            def outp(g):
                x32, z = X32[g % 2], Z[g % 2]
                for c in range(8):
                    po = PS[4 + (c % 2)]
                    for k in range(4):
                        mm(po.t[:, 0:G], WO.t[:, k, c * 128:(c + 1) * 128], OT.t[:, k, :], k == 0, k == 3, WO.rl + [OT.r], [po.r])
                    stt(z.t[:, c, :], x32.t[:, c, :], ALPHA, po.t[:, 0:G], ALU.mult, ALU.add, [x32.r, po.r], [z.r])

            NG = S // G
            load(0)
            if NG > 1:
                load(1)
            attn(0)
            outp(0)
            for g in range(NG):
                if g + 2 < NG:
                    load(g + 2)
                if g + 1 < NG:
                    attn(g + 1)
                ln_part1(Z[g % 2], SQ, G, PS[6])
                if g + 1 < NG:
                    outp(g + 1)
                ln_part2(Z[g % 2], SQ, RS, G, gT, bT, dst, r_dst, g * G, PS[6])
            P.barrier()

    P.barrier()
    cur, r_cur = xT, r_xT
    nph = [0]

    def run(f, *a):
        if nph[0] < stop_after:
            f(*a)
        nph[0] += 1
    for l in range(L):
        last = (l == L - 1)
        run(ffn_phase, l, 0, 0, cur, r_cur, XB, r_XB)
        run(proj_phase, l, XB, r_XB)
        run(dsa_phase, l)
        run(sb_phase, l)
        run(merge_phase, l, XB, r_XB, XA, r_XA)
        run(xattn_phase, l, XA, r_XA, XB, r_XB)
        if last:
            run(ffn_phase, l, 1, 3, XB, r_XB, outT, r_outT)
        else:
            run(ffn_phase, l, 1, 3, XB, r_XB, XA, r_XA)
        cur, r_cur = XA, r_XA
    P.emit()
    top.close()
    return nc, P


def _bucket_table():
    import math
    n = np.arange(256)
    nf = np.maximum(n, 1).astype(np.float32)
    large = 16 + (np.log(nf / np.float32(16)) / np.float32(math.log(8.0)) * np.float32(16)).astype(np.int32)
    large = np.minimum(large, 31)
    return np.where(n < 16, n, large)


def prep_inputs(x, mem, ln_g, ln_b, ffn_w_in, ffn_w_out, w_mix_in, b_gate, kv_norm_g, w_uk, w_uv,
                conv_w, w_branch, w_mix_out, xa_wq, xa_wkv, xa_wo, rel_bias):
    f = lambda a: np.ascontiguousarray(np.asarray(a, dtype=np.float32))
    B, S, _ = x.shape
    L = ln_g.shape[0]
    bt = _bucket_table()
    tl = np.arange(128)[:, None]
    col = np.arange(256)[None, :]
    nrel = np.where(col < 128, tl - col + 128, tl - (col - 128))
    idx = bt[np.clip(nrel, 0, 255)]
    rel_bias = f(rel_bias)
    biasT = f(np.transpose(rel_bias[idx], (0, 2, 1)))
    shared = {
        "lng": f(np.transpose(f(ln_g).reshape(L, 4, 8, 128), (0, 1, 3, 2))),
        "lnb": f(np.transpose(f(ln_b).reshape(L, 4, 8, 128), (0, 1, 3, 2))),
        "ffn_w_in": f(ffn_w_in), "ffn_w_out": f(ffn_w_out), "w_mix_in": f(w_mix_in),
        "bgate": f(np.transpose(f(b_gate).reshape(L, 3, 8, 128), (0, 1, 3, 2))),
        "kvg": f(kv_norm_g),
        "wuk": f(np.transpose(f(w_uk).reshape(L, 4, 2, 64, 128), (0, 2, 3, 1, 4)).reshape(L, 128, 4, 128)),
        "wuv": f(w_uv),
        "convw": f(np.transpose(f(conv_w).reshape(L, 3, 4, 128), (0, 3, 2, 1))),
        "w_branch": f(w_branch), "w_mix_out": f(w_mix_out),
        "xa_wq": f(xa_wq), "xa_wkv": f(xa_wkv), "xa_wo": f(xa_wo),
        "biasT": biasT, "rb31": f(rel_bias[31]),
    }
    maps = []
    for b in range(B):
        m = dict(shared)
        m["xT"] = f(f(x[b]).T.reshape(8, 128, S))
        m["memT"] = f(f(mem[b]).T.reshape(8, 128, 256))
        maps.append(m)
    return maps


_NC_CACHE = {}


def kernel(**inputs):
    x = np.asarray(inputs["x"])
    B, S, _ = x.shape
    L = np.asarray(inputs["ln_g"]).shape[0]
    key = (S, L)
    if key not in _NC_CACHE:
        _NC_CACHE[key] = build(S, L)[0]
    nc = _NC_CACHE[key]
    maps = prep_inputs(**inputs)
    res = run_bass_kernel_spmd(nc, maps, core_ids=list(range(B)))
    out = np.empty((B, S, D), dtype=np.float32)
    for b in range(B):
        out[b] = np.asarray(res.results[b]["outT"]).reshape(D, S).T
    return out
```

```python
import numpy as np
from contextlib import ExitStack
import concourse.bass as bass
import concourse.mybir as mybir
from concourse.bass_utils import run_bass_kernel_spmd

F32 = mybir.dt.float32
BF16 = mybir.dt.bfloat16
AF = mybir.ActivationFunctionType
ALU = mybir.AluOpType
AX = mybir.AxisListType


class Res:
    __slots__ = ("name", "w", "r", "excl", "dram")

    def __init__(self, name, excl=False):
        self.name = name
        self.w = None
        self.r = {}
        self.excl = excl
        self.dram = False


class Prog:
    ENG = ("pe", "act", "dve", "pool", "sp")

    def __init__(self, nc, n_slots=12):
        self.nc = nc
        self.ops = {e: [] for e in self.ENG}
        self.cnt = {e: 0 for e in self.ENG}
        self.seen = {e: {} for e in self.ENG}
        self.n_slots = n_slots
        self.slot_uses = {q: [0] * n_slots for q in ("sp", "pool", "act")}
        self.slot_rr = {q: 0 for q in ("sp", "pool", "act")}
        self.pe_pending = False
        self.nops = 0

    def _deps(self, eng, reads, writes):
        deps = {}

        def add(kv):
            if kv is None:
                return
            k, v = kv
            if deps.get(k, 0) < v:
                deps[k] = v
        for r in reads:
            add(r.w)
            if r.excl:
                for k, v in r.r.items():
                    add((k, v))
        for w in writes:
            add(w.w)
            for k, v in w.r.items():
                add((k, v))
        out = []
        seen = self.seen[eng]
        for k, v in deps.items():
            if k == "pe" and eng == "pe":
                continue
            if seen.get(k, 0) >= v:
                continue
            seen[k] = v
            out.append((k, v))
        return out

    def _register(self, key, val, reads, writes):
        for r in reads:
            if r.excl:
                r.w = (key, val)
                r.r = {}
            else:
                if r.r.get(key, 0) < val:
                    r.r[key] = val
        for w in writes:
            w.w = (key, val)
            w.r = {}

    def op(self, eng, emit, reads=(), writes=(), inc=True):
        waits = self._deps(eng, reads, writes)
        if inc:
            self.cnt[eng] += 1
            val = self.cnt[eng]
            if eng == "pe":
                self.pe_pending = False
        else:
            assert eng == "pe"
            val = self.cnt[eng] + 1
            self.pe_pending = True
        self._register(eng, val, reads, writes)
        self.ops[eng].append((waits, emit, (eng, 1) if inc else None))
        self.nops += 1

    def dma(self, q, emit, reads=(), writes=()):
        j = self.slot_rr[q]
        self.slot_rr[q] = (j + 1) % self.n_slots
        n = self.slot_uses[q][j]
        key = ("dma", q, j)
        waits = self._deps(q, reads, writes)
        if n > 0 and self.seen[q].get(key, 0) < 16 * n:
            self.seen[q][key] = 16 * n
            waits.append((key, 16 * n))
        self.slot_uses[q][j] = n + 1
        val = 16 * (n + 1)
        self._register(key, val, reads, writes)
        self.ops[q].append((waits, emit, (key, 16)))
        self.nops += 1

    def barrier(self):
        assert not self.pe_pending
        targets = [(e, self.cnt[e]) for e in ("pe", "act", "dve", "pool") if self.cnt[e] > 0]
        for q in self.slot_uses:
            for j, n in enumerate(self.slot_uses[q]):
                if n > 0:
                    targets.append((("dma", q, j), 16 * n))
        for e in self.ENG:
            waits = []
            for k, v in targets:
                if k == "pe" and e == "pe":
                    continue
                if self.seen[e].get(k, 0) < v:
                    self.seen[e][k] = v
                    waits.append((k, v))
            self.ops[e].append((waits, None, None))

    def wait_all(self, eng, resources):
        waits = self._deps(eng, resources, ())
        self.ops[eng].append((waits, None, None))

    def emit(self):
        nc = self.nc
        keys = set(self.ENG)
        for q in self.slot_uses:
            for j in range(self.n_slots):
                if self.slot_uses[q][j] > 0:
                    keys.add(("dma", q, j))
        with ExitStack() as es:
            sems = {}
            for k in sorted(keys, key=str):
                nm = k if isinstance(k, str) else "d_%s_%d" % (k[1], k[2])
                sems[k] = es.enter_context(nc.semaphore("s_" + nm))
            block = es.enter_context(nc.Block())

            def run(e, name):
                for waits, emit, inc in self.ops[name]:
                    for k, v in waits:
                        e.wait_ge(sems[k], v)
                    if emit is None:
                        continue
                    ins = emit(e)
                    if inc is not None:
                        ins.then_inc(sems[inc[0]], inc[1])

            @block.tensor
            def _(e):
                run(e, "pe")

            @block.scalar
            def _(e):
                run(e, "act")

            @block.vector
            def _(e):
                run(e, "dve")

            @block.gpsimd
            def _(e):
                run(e, "pool")

            @block.sync
            def _(e):
                run(e, "sp")

D = 1024
DFF = 2816
PMIX = 7368
ALPHA = 4.0 ** 0.25
EPS = 1e-5
KTOP = 256
NIT = 14
NEG = -1.0e30


class Tl:
    def __init__(self, t, name, excl=False):
        self.t = t
        self.r = Res(name, excl)
        self.rl = [self.r]

    def __getitem__(self, k):
        return self.t[k]


def build(S=4096, L=2, dbg=False, stop_after=99):
    NT = S // 128
    nc = bass.Bass("TRN2", target_bir_lowering=False)
    P = Prog(nc)

    def din(name, shape, dt=F32):
        return nc.dram_tensor(name, list(shape), dt, kind="ExternalInput").ap()

    def dres(name):
        r = Res(name)
        r.dram = True
        return r

    def dscr(name, shape, dt):
        return nc.dram_tensor(name, list(shape), dt, kind=("ExternalOutput" if dbg else "Internal")).ap(), dres(name)

    xT = din("xT", [8, 128, S]); r_xT = dres("xT")
    memT = din("memT", [8, 128, 256])
    lng = din("lng", [L, 4, 128, 8]); lnb = din("lnb", [L, 4, 128, 8])
    w_in = din("ffn_w_in", [L, 2, D, 2 * DFF]); w_out = din("ffn_w_out", [L, 2, DFF, D])
    w_mix = din("w_mix_in", [L, D, PMIX])
    bgate = din("bgate", [L, 3, 128, 8])
    kvg = din("kvg", [L, 128])
    wuk = din("wuk", [L, 128, 4, 128])
    wuv = din("wuv", [L, 8, 128, 64])
    convw = din("convw", [L, 128, 4, 3])
    wbr = din("w_branch", [L, 3, 512, D])
    wmo = din("w_mix_out", [L, D, D])
    xwq = din("xa_wq", [L, D, 512]); xwkv = din("xa_wkv", [L, D, D]); xwo = din("xa_wo", [L, 512, D])
    biasT = din("biasT", [128, 8, 256])
    rb31 = din("rb31", [8])
    outT = nc.dram_tensor("outT", [8, 128, S], F32, kind="ExternalOutput").ap(); r_outT = dres("outT")
    r_const = dres("const_in")

    XA, r_XA = dscr("XA", [8, 128, S], F32)
    XB, r_XB = dscr("XB", [8, 128, S], F32)
    QL, r_QL = dscr("QL", [8, 128, S], BF16)
    IQT, r_IQT = dscr("IQT", [4, 128, S], BF16)
    IKT, r_IKT = dscr("IKT", [64, S], BF16)
    QST, r_QST = dscr("QST", [4, 128, S], BF16)
    KST, r_KST = dscr("KST", [4, 128, S], BF16)
    VS, r_VS = dscr("VS", [S, 512], BF16)
    CKV, r_CKV = dscr("CKV", [S, 128], BF16)
    CKVT, r_CKVT = dscr("CKVT", [128, S], BF16)
    IW, r_IW = dscr("IW", [S, 8], F32)
    YCT, r_YCT = dscr("YCT", [4, 128, S], BF16)
    YAT, r_YAT = dscr("YAT", [4, 128, S], BF16)
    YBT, r_YBT = dscr("YBT", [4, 128, S], BF16)

    def mm(out, lhsT, rhs, start, stop, reads, writes, inc=None):
        P.op("pe", lambda e: e.matmul(out, lhsT=lhsT, rhs=rhs, start=start, stop=stop),
             reads, writes, inc=(stop if inc is None else inc))

    def tr(out, in_, ident, reads, writes, inc=True):
        P.op("pe", lambda e: e.transpose(out=out, in_=in_, identity=ident), reads, writes, inc=inc)

    def act(out, in_, func, reads, writes, bias=None, scale=None, accum=None):
        kw = {}
        if bias is not None:
            kw["bias"] = bias
        if scale is not None:
            kw["scale"] = scale
        if accum is not None:
            kw["accum_out"] = accum
        P.op("act", lambda e: e.activation(out=out, in_=in_, func=func, **kw), reads, writes)

    def tt(eng, out, a, b, op, reads, writes):
        P.op(eng, lambda e: e.tensor_tensor(out=out, in0=a, in1=b, op=op), reads, writes)

    def ts(eng, out, a, s1, op0, reads, writes, s2=None, op1=None, accum=None):
        kw = {}
        if op1 is not None:
            kw["op1"] = op1
        if accum is not None:
            kw["accum_out"] = accum
        P.op(eng, lambda e: e.tensor_scalar(out=out, in0=a, scalar1=s1, scalar2=s2, op0=op0, **kw), reads, writes)

    def stt(out, in0, scalar, in1, op0, op1, reads, writes, accum=None):
        kw = {}
        if accum is not None:
            kw["accum_out"] = accum
        P.op("dve", lambda e: e.scalar_tensor_tensor(out=out, in0=in0, scalar=scalar, in1=in1, op0=op0, op1=op1, **kw),
             reads, writes)

    def cp(eng, out, in_, reads, writes):
        if eng == "act":
            P.op("act", lambda e: e.copy(out=out, in_=in_), reads, writes)
        else:
            P.op(eng, lambda e: e.tensor_copy(out=out, in_=in_), reads, writes)

    def red(out, in_, op, reads, writes):
        P.op("dve", lambda e: e.tensor_reduce(out=out, in_=in_, axis=AX.X, op=op), reads, writes)

    def recip(out, in_, reads, writes):
        P.op("dve", lambda e: e.reciprocal(out=out, in_=in_), reads, writes)

    def memset(eng, out, val, writes):
        P.op(eng, lambda e: e.memset(out, val), (), writes)

    def dma(q, out, in_, reads, writes, **kw):
        reads = [r for r in reads if not r.dram]
        writes = [r for r in writes if not r.dram]
        P.dma(q, lambda e: e.dma_start(out=out, in_=in_, **kw), reads, writes)

    def wload(dst, k_chunks, src2d, c0, c1, rows0=0):
        dst.rl = [Res("%s_k%d" % (dst.r.name, k)) for k in range(k_chunks)]
        for k in range(k_chunks):
            dma("pool", dst.t[:, k, 0:c1 - c0], src2d[rows0 + k * 128: rows0 + (k + 1) * 128, c0:c1],
                [r_const] + ([dst.rl[k - 1]] if k > 0 else []), [dst.rl[k]], max_dma_last_dim=8192)

    top = ExitStack()

    uid = [0]

    def alloc(stack, name, shape, dt):
        uid[0] += 1
        name = "%s_%d" % (name, uid[0])
        return Tl(stack.enter_context(nc.sbuf_tensor(name, list(shape), dt)), name)

    PS = [Tl(top.enter_context(nc.psum_tensor("ps%d" % i, [128, 512], F32)), "ps%d" % i, True) for i in range(7)]
    PB = Tl(top.enter_context(nc.psum_tensor("pb", [128, 1024], BF16)), "pb", True)

    identf = alloc(top, "identf", [128, 128], F32)
    ident = alloc(top, "ident", [128, 128], BF16)
    onesM = alloc(top, "onesM", [128, 128], F32)
    onesB = alloc(top, "onesB", [128, 128], BF16)
    caus = alloc(top, "caus", [128, 128], F32)
    strict = alloc(top, "strict", [128, 128], F32)
    pow2 = alloc(top, "pow2", [128, NIT], F32)
    neglo = alloc(top, "neglo", [128, 1], F32)
    epsT = alloc(top, "epsT", [128, 1], F32)
    oneT = alloc(top, "oneT", [128, 1], F32)
    memset("pool", identf.t[:], 0.0, [identf.r])
    P.op("pool", lambda e: e.affine_select(out=identf.t[:], in_=identf.t[:], pattern=[[-1, 128]],
                                           compare_op=ALU.not_equal, fill=1.0, base=0, channel_multiplier=1),
         [identf.r], [identf.r])
    cp("dve", ident.t[:], identf.t[:], [identf.r], [ident.r])
    memset("pool", onesM.t[:], 1.0 / D, [onesM.r])
    memset("pool", onesB.t[:], 1.0, [onesB.r])
    memset("pool", caus.t[:], 0.0, [caus.r])
    P.op("pool", lambda e: e.affine_select(out=caus.t[:], in_=caus.t[:], pattern=[[-1, 128]],
                                           compare_op=ALU.is_ge, fill=NEG, base=0, channel_multiplier=1),
         [caus.r], [caus.r])
    memset("pool", strict.t[:], 1.0, [strict.r])
    P.op("pool", lambda e: e.affine_select(out=strict.t[:], in_=strict.t[:], pattern=[[-1, 128]],
                                           compare_op=ALU.is_gt, fill=0.0, base=0, channel_multiplier=1),
         [strict.r], [strict.r])
    for j in range(NIT):
        memset("pool", pow2.t[:, j:j + 1], 0.5 ** (j + 1), [pow2.r])
    memset("pool", neglo.t[:], -1.0e29, [neglo.r])
    memset("pool", epsT.t[:], EPS, [epsT.r])
    memset("pool", oneT.t[:], 1.0, [oneT.r])

    def ln_part1(Z, SQ, G, pa):
        for c in range(8):
            mm(pa.t[:, 0:G], onesM.t[:], Z.t[:, c, :], c == 0, c == 7, [onesM.r, Z.r], [pa.r])
        for c in range(8):
            tt("dve", Z.t[:, c, :], Z.t[:, c, :], pa.t[:, 0:G], ALU.subtract, [Z.r, pa.r], [Z.r])
        act(SQ.t[:], Z.t[:], AF.Square, [Z.r], [SQ.r])

    def ln_part2(Z, SQ, RS, G, gT, bT, dst, r_dst, g0, pb_):
        for c in range(8):
            mm(pb_.t[:, 0:G], onesM.t[:], SQ.t[:, c, :], c == 0, c == 7, [onesM.r, SQ.r], [pb_.r])
        act(RS.t[:], pb_.t[:, 0:G], AF.Sqrt, [pb_.r, epsT.r], [RS.r], bias=epsT.t[:, 0:1])
        recip(RS.t[:], RS.t[:], [RS.r], [RS.r])
        for c in range(8):
            tt("pool", Z.t[:, c, :], Z.t[:, c, :], RS.t[:], ALU.mult, [Z.r, RS.r], [Z.r])
        for c in range(8):
            act(SQ.t[:, c, :], Z.t[:, c, :], AF.Identity, [Z.r, gT.r, bT.r], [SQ.r],
                scale=gT.t[:, c:c + 1], bias=bT.t[:, c:c + 1])
        dma("sp", dst[:, :, g0:g0 + G].rearrange("c p t -> p c t"), SQ.t[:], [SQ.r], [r_dst])

    def layer_norm(Z, SQ, RS, G, gT, bT, dst, r_dst, g0, pa, pb_):
        ln_part1(Z, SQ, G, pa)
        ln_part2(Z, SQ, RS, G, gT, bT, dst, r_dst, g0, pb_)

    def ffn_phase(l, which, ln_i, src, r_src, dst, r_dst):
        G = 256
        NG = S // G
        with ExitStack() as st:
            Win = alloc(st, "Win", [128, 8, 2 * DFF], BF16)
            Wout = alloc(st, "Wout", [128, 22, D], BF16)
            gT = alloc(st, "gT", [128, 8], F32); bT = alloc(st, "bT", [128, 8], F32)
            X32 = [alloc(st, "X32%d" % b, [128, 8, G], F32) for b in range(2)]
            Xb = [alloc(st, "Xb%d" % b, [128, 8, G], BF16) for b in range(2)]
            H = alloc(st, "H", [128, 22, G], BF16)
            SA = [alloc(st, "SA%d" % i, [128, G], F32) for i in range(2)]
            Z = [alloc(st, "Z%d" % b, [128, 8, G], F32) for b in range(2)]
            SQ = alloc(st, "SQ", [128, 8, G], F32)
            RS = alloc(st, "RS", [128, G], F32)
            dma("sp", gT.t[:], lng[l, ln_i], [r_const], [gT.r])
            dma("sp", bT.t[:], lnb[l, ln_i], [r_const], [bT.r])
            wload(Win, 8, w_in[l, which], 0, 2 * DFF)
            wload(Wout, 22, w_out[l, which], 0, D)

            def load(g):
                b = g % 2
                g0 = g * G
                dma("sp", X32[b].t[:], src[:, :, g0:g0 + G].rearrange("c p t -> p c t"), [r_src], [X32[b].r])
                cp("pool", Xb[b].t[:], X32[b].t[:], [X32[b].r], [Xb[b].r])

            def inproj(g):
                xb = Xb[g % 2]
                for j in range(22):
                    pa = PS[(2 * j) % 4]; pb_ = PS[(2 * j + 1) % 4]
                    for k in range(8):
                        mm(pa.t[:, 0:G], Win.t[:, k, j * 128:(j + 1) * 128], xb.t[:, k, :], k == 0, k == 7,
                           Win.rl + [xb.r], [pa.r])
                    for k in range(8):
                        mm(pb_.t[:, 0:G], Win.t[:, k, DFF + j * 128:DFF + (j + 1) * 128], xb.t[:, k, :], k == 0, k == 7,
                           Win.rl + [xb.r], [pb_.r])
                    sa = SA[j % 2]
                    act(sa.t[:], pa.t[:, 0:G], AF.Silu, [pa.r], [sa.r])
                    stt(H.t[:, j, :], sa.t[:], 0.5, pb_.t[:, 0:G], ALU.mult, ALU.mult, [sa.r, pb_.r], [H.r])

            def outproj(g):
                x32, z = X32[g % 2], Z[g % 2]
                for c in range(8):
                    po = PS[4 + (c % 2)]
                    for j in range(22):
                        mm(po.t[:, 0:G], Wout.t[:, j, c * 128:(c + 1) * 128], H.t[:, j, :], j == 0, j == 21,
                           Wout.rl + [H.r], [po.r])
                    stt(z.t[:, c, :], x32.t[:, c, :], ALPHA, po.t[:, 0:G], ALU.mult, ALU.add, [x32.r, po.r], [z.r])

            load(0)
            if NG > 1:
                load(1)
            inproj(0)
            outproj(0)
            for g in range(NG):
                if g + 2 < NG:
                    load(g + 2)
                if g + 1 < NG:
                    inproj(g + 1)
                ln_part1(Z[g % 2], SQ, G, PS[6])
                if g + 1 < NG:
                    outproj(g + 1)
                ln_part2(Z[g % 2], SQ, RS, G, gT, bT, dst, r_dst, g * G, PS[6])
            P.barrier()

    C_QA, C_CKV, C_IQ, C_IK, C_IW, C_QS, C_KS, C_VS, C_CB, C_CC, C_CH, C_G = \
        0, 512, 640, 1152, 1216, 1224, 1736, 2248, 2760, 3272, 3784, 4296

    def proj_phase(l, src, r_src):
        G = 512
        NW = C_G
        with ExitStack() as st:
            Wm = alloc(st, "Wm", [128, 8, NW], BF16)
            WUK = alloc(st, "WUK", [128, 4, 128], BF16)
            CW = alloc(st, "CW", [128, 4, 3], F32)
            KVG = alloc(st, "KVG", [128, 128], F32)
            X32 = alloc(st, "X32", [128, 8, G], F32)
            Xb = alloc(st, "Xb", [128, 8, G], BF16)
            QA = alloc(st, "QA", [128, 4, G], BF16)
            OB = [alloc(st, "OB%d" % i, [128, G], BF16) for i in range(3)]
            CCs = alloc(st, "CCs", [128, G], F32)
            ZC = [alloc(st, "ZC%d" % q, [128, G + 2], F32) for q in range(4)]
            YC = alloc(st, "YC", [128, G], F32)
            JK = alloc(st, "JK", [128, 128], F32)
            SS = alloc(st, "SS", [128, 2], F32)
            CKb = alloc(st, "CKb", [128, 128], BF16)
            CKTb = alloc(st, "CKTb", [128, 128], BF16)
            IWs = alloc(st, "IWs", [128, 8], F32)
            VSb = alloc(st, "VSb", [128, 512], BF16)
            wload(Wm, 8, w_mix[l], 0, NW)
            dma("pool", WUK.t[:], wuk[l], [r_const], [WUK.r])
            dma("sp", CW.t[:], convw[l], [r_const], [CW.r])
            dma("sp", KVG.t[:], kvg[l].partition_broadcast(128), [r_const], [KVG.r])
            for q in range(4):
                memset("pool", ZC[q].t[:, 0:2], 0.0, [ZC[q].r])
            rot = [0]

            def nextps():
                rot[0] = (rot[0] + 1) % 5
                return PS[rot[0]]
            obr = [0]

            def nextob():
                obr[0] = (obr[0] + 1) % 3
                return OB[obr[0]]

            def fm_chunk(col, M=128):
                ps = nextps()
                for k in range(8):
                    mm(ps.t[0:M, 0:G], Wm.t[:, k, col:col + M], Xb.t[:, k, :], k == 0, k == 7, Wm.rl + [Xb.r], [ps.r])
                return ps

            for g in range(S // G):
                g0 = g * G
                dma("sp", X32.t[:], src[:, :, g0:g0 + G].rearrange("c p t -> p c t"), [r_src], [X32.r])
                cp("pool", Xb.t[:], X32.t[:], [X32.r], [Xb.r])
                for q in range(4):
                    ps = fm_chunk(C_QA + q * 128)
                    cp("act", QA.t[:, q, :], ps.t[:, 0:G], [ps.r], [QA.r])
                for h in range(8):
                    p0 = (h % 2) * 64
                    ps = nextps()
                    mm(ps.t[:, 0:G], WUK.t[p0:p0 + 64, h // 2, :], QA.t[p0:p0 + 64, h // 2, :], True, True,
                       [WUK.r, QA.r], [ps.r])
                    ob = nextob()
                    act(ob.t[:], ps.t[:, 0:G], AF.Copy, [ps.r], [ob.r], scale=0.125)
                    dma("sp", QL[h, :, g0:g0 + G], ob.t[:], [ob.r], [r_QL])
                for (col, dstT, r_d, sc) in ((C_IQ, IQT, r_IQT, 1.0), (C_QS, QST, r_QST, 0.125), (C_KS, KST, r_KST, 1.0)):
                    for q in range(4):
                        ps = fm_chunk(col + q * 128)
                        ob = nextob()
                        act(ob.t[:], ps.t[:, 0:G], AF.Copy, [ps.r], [ob.r], scale=sc)
                        dma("sp", dstT[q, :, g0:g0 + G], ob.t[:], [ob.r], [r_d])
                ps = fm_chunk(C_IK, 64)
                ob = nextob()
                cp("act", ob.t[0:64, :], ps.t[0:64, 0:G], [ps.r], [ob.r])
                dma("sp", IKT[:, g0:g0 + G], ob.t[0:64, :], [ob.r], [r_IKT])
                for q in range(4):
                    ps = fm_chunk(C_CC + q * 128)
                    cp("act", CCs.t[:], ps.t[:, 0:G], [ps.r], [CCs.r])
                    ps = fm_chunk(C_CH + q * 128)
                    zc = ZC[q]
                    tt("dve", zc.t[:, 2:G + 2], CCs.t[:], ps.t[:, 0:G], ALU.mult, [CCs.r, ps.r], [zc.r])
                    ts("dve", YC.t[:], zc.t[:, 2:G + 2], CW.t[:, q, 2:3], ALU.mult, [zc.r, CW.r], [YC.r])
                    stt(YC.t[:], zc.t[:, 1:G + 1], CW.t[:, q, 1:2], YC.t[:], ALU.mult, ALU.add, [zc.r, CW.r, YC.r], [YC.r])
                    stt(YC.t[:], zc.t[:, 0:G], CW.t[:, q, 0:1], YC.t[:], ALU.mult, ALU.add, [zc.r, CW.r, YC.r], [YC.r])
                    ps = fm_chunk(C_CB + q * 128)
                    ob = nextob()
                    tt("dve", ob.t[:], YC.t[:], ps.t[:, 0:G], ALU.mult, [YC.r, ps.r], [ob.r])
                    dma("sp", YCT[q, :, g0:g0 + G], ob.t[:], [ob.r], [r_YCT])
                    cp("pool", zc.t[:, 0:2], zc.t[:, G:G + 2], [zc.r], [zc.r])
                for t4 in range(G // 128):
                    t0 = g0 + t4 * 128
                    xs = slice(t4 * 128, (t4 + 1) * 128)
                    ps = nextps()
                    for k in range(8):
                        mm(ps.t[:, 0:128], Xb.t[:, k, xs], Wm.t[:, k, C_CKV:C_CKV + 128], k == 0, k == 7,
                           Wm.rl + [Xb.r], [ps.r])
                    act(JK.t[:], ps.t[:, 0:128], AF.Square, [ps.r], [JK.r, SS.r], accum=SS.t[:, 0:1])
                    act(SS.t[:, 1:2], SS.t[:, 0:1], AF.Sqrt, [SS.r, epsT.r], [SS.r], scale=1.0 / 128, bias=epsT.t[:, 0:1])
                    recip(SS.t[:, 1:2], SS.t[:, 1:2], [SS.r], [SS.r])
                    stt(CKb.t[:], ps.t[:, 0:128], SS.t[:, 1:2], KVG.t[:], ALU.mult, ALU.mult, [ps.r, SS.r, KVG.r], [CKb.r])
                    dma("sp", CKV[t0:t0 + 128, :], CKb.t[:], [CKb.r], [r_CKV])
                    tr(PB.t[:, 0:128], CKb.t[:], ident.t[:], [CKb.r, ident.r], [PB.r])
                    cp("act", CKTb.t[:], PB.t[:, 0:128], [PB.r], [CKTb.r])
                    dma("sp", CKVT[:, t0:t0 + 128], CKTb.t[:], [CKTb.r], [r_CKVT])
                    ps = nextps()
                    for k in range(8):
                        mm(ps.t[:, 0:8], Xb.t[:, k, xs], Wm.t[:, k, C_IW:C_IW + 8], k == 0, k == 7, Wm.rl + [Xb.r], [ps.r])
                    cp("act", IWs.t[:], ps.t[:, 0:8], [ps.r], [IWs.r])
                    dma("sp", IW[t0:t0 + 128, :], IWs.t[:], [IWs.r], [r_IW])
                    ps = nextps()
                    for k in range(8):
                        mm(ps.t[:, 0:512], Xb.t[:, k, xs], Wm.t[:, k, C_VS:C_VS + 512], k == 0, k == 7, Wm.rl + [Xb.r], [ps.r])
                    cp("act", VSb.t[:], ps.t[:, 0:512], [ps.r], [VSb.r])
                    dma("sp", VS[t0:t0 + 128, :], VSb.t[:], [VSb.r], [r_VS])
            P.barrier()

    def chunks(n):
        return [(c0, min(512, n - c0)) for c0 in range(0, n, 512)]

    def dsa_phase(l):
        with ExitStack() as st:
            CKVX = alloc(st, "CKVX", [128, NT, 128], BF16)
            CKT = alloc(st, "CKT", [128, S], BF16)
            IK2 = alloc(st, "IK2", [128, S], BF16)
            BI = alloc(st, "BI", [128, 8, 256], F32)
            RB = alloc(st, "RB", [128, 8], F32)
            WUVP = alloc(st, "WUVP", [128, 8, 128], BF16)
            QLg = [alloc(st, "QLg%d" % b, [128, 8, 512], BF16) for b in range(2)]
            IQg = [alloc(st, "IQg%d" % b, [128, 4, 512], BF16) for b in range(2)]
            IWg = [alloc(st, "IWg%d" % b, [128, 4, 8], F32) for b in range(2)]
            WA = [alloc(st, "WA%d" % b, [128, 8], F32) for b in range(2)]
            SG = [alloc(st, "SG%d" % b, [128, 8], F32) for b in range(2)]
            SC = [alloc(st, "SC%d" % b, [128, S], F32) for b in range(2)]
            Mk = [alloc(st, "Mk%d" % b, [128, S], BF16) for b in range(2)]
            TH = [alloc(st, "TH%d" % b, [128, 8], F32) for b in range(2)]
            WALL = [alloc(st, "WALL%d" % b, [128, NIT], F32) for b in range(2)]
            PM = [alloc(st, "PM%d" % b, [128, S], BF16) for b in range(2)]
            PT = [alloc(st, "PT%d" % b, [128, NT, 128], BF16) for b in range(2)]
            TMP = [alloc(st, "TMP%d" % i, [128, 512], F32) for i in range(4)]
            JKB = alloc(st, "JKB", [128, S], BF16)
            RSM = alloc(st, "RSM", [128, 8, 8], F32)
            RSUM = alloc(st, "RSUM", [128, 8], F32)
            OLN = alloc(st, "OLN", [128, 8, 128], BF16)
            OLT = alloc(st, "OLT", [128, 8, 128], BF16)
            YAg = alloc(st, "YAg", [128, 4, 512], BF16)
            dma("sp", CKVX.t[:], CKV.rearrange("(j p) c -> p j c", p=128), [r_CKV], [CKVX.r])
            dma("sp", CKT.t[:], CKVT, [r_CKVT], [CKT.r])
            dma("sp", IK2.t[0:64, :], IKT, [r_IKT], [IK2.r])
            dma("sp", IK2.t[64:128, :], IKT, [r_IKT], [IK2.r])
            dma("sp", BI.t[:], biasT, [r_const], [BI.r])
            dma("sp", RB.t[:], rb31.partition_broadcast(128), [r_const], [RB.r])
            memset("pool", WUVP.t[:], 0.0, [WUVP.r])
            for h in range(8):
                p0 = (h % 2) * 64
                dma("pool", WUVP.t[:, h, p0:p0 + 64], wuv[l, h], [r_const], [WUVP.r])
            tmr = [0]

            def nexttmp():
                tmr[0] = (tmr[0] + 1) % 4
                return TMP[tmr[0]]
            psr = [0]

            def nextps():
                psr[0] = (psr[0] + 1) % 3
                return PS[psr[0]]

            def load_group(g):
                b = g % 2
                g0 = g * 512
                dma("sp", QLg[b].t[:], QL[:, :, g0:g0 + 512].rearrange("h c t -> c h t"), [r_QL], [QLg[b].r])
                dma("sp", IQg[b].t[:], IQT[:, :, g0:g0 + 512].rearrange("q p t -> p q t"), [r_IQT], [IQg[b].r])
                dma("sp", IWg[b].t[:], IW[g0:g0 + 512, :].rearrange("(a p) h -> p a h", p=128), [r_IW], [IWg[b].r])

            def index_scores(i):
                g, t4 = divmod(i, 4)
                gb = g % 2
                b = i % 2
                n = (i + 1) * 128
                tsl = slice(t4 * 128, (t4 + 1) * 128)
                sc, th, wall, wa, sg, mk = SC[b], TH[b], WALL[b], WA[b], SG[b], Mk[b]
                act(wa.t[:], IWg[gb].t[:, t4, :], AF.Abs, [IWg[gb].r], [wa.r])
                P.op("act", lambda e: e.sign(out=sg.t[:], in_=IWg[gb].t[:, t4, :]), [IWg[gb].r], [sg.r])
                for (c0, w) in chunks(n):
                    for h in range(8):
                        p0 = (h % 2) * 64
                        ps = nextps()
                        mm(ps.t[:, 0:w], IQg[gb].t[p0:p0 + 64, h // 2, tsl], IK2.t[p0:p0 + 64, c0:c0 + w], True, True,
                           [IQg[gb].r, IK2.r], [ps.r])
                        R = nexttmp()
                        act(R.t[:, 0:w], ps.t[:, 0:w], AF.Relu, [ps.r, wa.r], [R.r], scale=wa.t[:, h:h + 1])
                        if h == 0:
                            ts("dve", sc.t[:, c0:c0 + w], R.t[:, 0:w], sg.t[:, 0:1], ALU.mult, [R.r, sg.r], [sc.r])
                        else:
                            stt(sc.t[:, c0:c0 + w], R.t[:, 0:w], sg.t[:, h:h + 1], sc.t[:, c0:c0 + w], ALU.mult, ALU.add,
                                [R.r, sg.r, sc.r], [sc.r])
                tt("dve", sc.t[:, n - 128:n], sc.t[:, n - 128:n], caus.t[:], ALU.add, [sc.r, caus.r], [sc.r])
                steps = []
                if i >= 2:
                    def init():
                        red(th.t[:, 0:1], sc.t[:, 0:n - 128], ALU.min, [sc.r], [th.r])
                        red(th.t[:, 1:2], sc.t[:, 0:n], ALU.max, [sc.r], [th.r])
                        tt("dve", th.t[:, 2:3], th.t[:, 1:2], th.t[:, 0:1], ALU.subtract, [th.r], [th.r])
                        ts("dve", wall.t[:], pow2.t[:], th.t[:, 2:3], ALU.mult, [pow2.r, th.r], [wall.r])
                        tt("dve", th.t[:, 3:4], th.t[:, 0:1], wall.t[:, 0:1], ALU.add, [th.r, wall.r], [th.r])
                    steps.append(init)

                    def mkstep(j):
                        def step():
                            ts("dve", JKB.t[:, 0:n], sc.t[:, 0:n], th.t[:, 3:4], ALU.is_ge, [sc.r, th.r], [th.r],
                               op1=ALU.add, accum=th.t[:, 4:5])
                            ts("dve", th.t[:, 5:6], th.t[:, 4:5], KTOP - 0.5, ALU.is_ge, [th.r, wall.r], [th.r],
                               s2=wall.t[:, j:j + 1], op1=ALU.mult)
                            if j + 1 < NIT:
                                stt(th.t[:, 3:4], th.t[:, 5:6], wall.t[:, j + 1:j + 2], th.t[:, 3:4], ALU.subtract, ALU.add,
                                    [th.r, wall.r], [th.r])
                            else:
                                stt(th.t[:, 0:1], th.t[:, 5:6], wall.t[:, j:j + 1], th.t[:, 3:4], ALU.subtract, ALU.add,
                                    [th.r, wall.r], [th.r])
                        return step
                    for j in range(NIT):
                        steps.append(mkstep(j))
                    steps.append(lambda: ts("dve", mk.t[:, 0:n], sc.t[:, 0:n], th.t[:, 0:1], ALU.is_ge, [sc.r, th.r], [mk.r]))
                else:
                    steps.append(lambda: ts("dve", mk.t[:, 0:n], sc.t[:, 0:n], neglo.t[:, 0:1], ALU.is_ge, [sc.r, neglo.r], [mk.r]))
                return steps

            def heads(i, pending):
                g, t4 = divmod(i, 4)
                gb = g % 2
                n = (i + 1) * 128
                tsl = slice(t4 * 128, (t4 + 1) * 128)
                mk = Mk[i % 2]
                nb0 = max(0, n - 256)
                per_head = (len(pending) + 7) // 8
                for h in range(8):
                    pm, pt = PM[h % 2], PT[h % 2]
                    ch = chunks(n)
                    for ci, (c0, w) in enumerate(ch):
                        ps = nextps()
                        mm(ps.t[:, 0:w], QLg[gb].t[:, h, tsl], CKT.t[:, c0:c0 + w], True, True, [QLg[gb].r, CKT.r], [ps.r])
                        Pe = nexttmp()
                        fa, fb = c0, min(c0 + w, nb0)
                        na, nb_ = max(c0, nb0), c0 + w
                        if fb > fa:
                            act(Pe.t[:, fa - c0:fb - c0], ps.t[:, fa - c0:fb - c0], AF.Exp, [ps.r, RB.r], [Pe.r],
                                bias=RB.t[:, h:h + 1])
                        if nb_ > na:
                            bo = 256 - (n - na)
                            tt("dve", Pe.t[:, na - c0:nb_ - c0], ps.t[:, na - c0:nb_ - c0], BI.t[:, h, bo:bo + (nb_ - na)],
                               ALU.add, [ps.r, BI.r], [Pe.r])
                            act(Pe.t[:, na - c0:nb_ - c0], Pe.t[:, na - c0:nb_ - c0], AF.Exp, [Pe.r], [Pe.r])
                        stt(pm.t[:, c0:c0 + w], Pe.t[:, 0:w], 1.0, mk.t[:, c0:c0 + w], ALU.mult, ALU.mult,
                            [Pe.r, mk.r], [pm.r, RSM.r], accum=RSM.t[:, h, ci:ci + 1])
                    red(RSUM.t[:, h:h + 1], RSM.t[:, h, 0:len(ch)], ALU.add, [RSM.r], [RSUM.r])
                    recip(RSUM.t[:, h:h + 1], RSUM.t[:, h:h + 1], [RSUM.r], [RSUM.r])
                    for _ in range(per_head):
                        if pending:
                            pending.pop(0)()
                    for j0 in range(0, i + 1, 8):
                        nbk = min(8, i + 1 - j0)
                        for jj in range(nbk):
                            j = j0 + jj
                            tr(PB.t[:, jj * 128:(jj + 1) * 128], pm.t[:, j * 128:(j + 1) * 128], ident.t[:],
                               [pm.r, ident.r], [PB.r], inc=(jj == nbk - 1))
                        cp("act", pt.t[:, j0:j0 + nbk, :], PB.t[:, 0:nbk * 128].rearrange("p (j t) -> p j t", t=128),
                           [PB.r], [pt.r])
                    ol = PS[3 + (h % 2)]
                    for j in range(i + 1):
                        mm(ol.t[:, 0:128], pt.t[:, j, :], CKVX.t[:, j, :], j == 0, j == i, [pt.r, CKVX.r], [ol.r])
                    act(OLN.t[:, h, :], ol.t[:, 0:128], AF.Copy, [ol.r, RSUM.r], [OLN.r], scale=RSUM.t[:, h:h + 1])
                while pending:
                    pending.pop(0)()
                for h in range(8):
                    tr(PB.t[:, h * 128:(h + 1) * 128], OLN.t[:, h, :], ident.t[:], [OLN.r, ident.r], [PB.r], inc=(h == 7))
                cp("act", OLT.t[:], PB.t[:].rearrange("p (j t) -> p j t", t=128), [PB.r], [OLT.r])
                for q in range(4):
                    ps = PS[5]
                    mm(ps.t[:, 0:128], WUVP.t[:, 2 * q, :], OLT.t[:, 2 * q, :], True, False, [WUVP.r, OLT.r], [ps.r])
                    mm(ps.t[:, 0:128], WUVP.t[:, 2 * q + 1, :], OLT.t[:, 2 * q + 1, :], False, True, [WUVP.r, OLT.r], [ps.r])
                    cp("act", YAg.t[:, q, tsl], ps.t[:, 0:128], [ps.r], [YAg.r])
                if t4 == 3:
                    g0 = g * 512
                    dma("sp", YAT[:, :, g0:g0 + 512].rearrange("q p t -> p q t"), YAg.t[:], [YAg.r], [r_YAT])

            load_group(0)
            for s_ in index_scores(0):
                s_()
            for i in range(NT):
                pending = []
                if i + 1 < NT:
                    if (i + 1) % 4 == 0:
                        load_group((i + 1) // 4)
                    pending = index_scores(i + 1)
                heads(i, pending)
            P.barrier()

    def sb_phase(l):
        with ExitStack() as st:
            KS_ = alloc(st, "KS_", [128, 4, S], BF16)
            VSX = alloc(st, "VSX", [128, NT, 512], BF16)
            QSg = [alloc(st, "QSg%d" % b, [128, 4, 512], BF16) for b in range(2)]
            LB = [alloc(st, "LB%d" % b, [128, S + 1], F32) for b in range(2)]
            NLX = [alloc(st, "NLX", [128, S], F32)] * 2
            ZS = [alloc(st, "ZS%d" % b, [128, S], F32) for b in range(2)]
            AB = [alloc(st, "AB%d" % b, [128, S], BF16) for b in range(2)]
            PT = [alloc(st, "PT2%d" % b, [128, NT, 128], BF16) for b in range(2)]
            TMP = [alloc(st, "TMQ%d" % i, [128, 512], F32) for i in range(4)]
            TSM = [alloc(st, "TSM%d" % b, [128, 16], F32) for b in range(2)]
            TOT = [alloc(st, "TOT%d" % b, [128, 2], F32) for b in range(2)]
            YBs = alloc(st, "YBs", [128, 512], BF16)
            YBg = alloc(st, "YBg", [128, 4, 512], BF16)
            dma("sp", KS_.t[:], KST.rearrange("q p t -> p q t"), [r_KST], [KS_.r])
            dma("sp", VSX.t[:], VS.rearrange("(j p) c -> p j c", p=128), [r_VS], [VSX.r])
            for b in range(2):
                memset("pool", LB[b].t[:, 0:1], 0.0, [LB[b].r])
            tmr = [0]

            def nexttmp():
                tmr[0] = (tmr[0] + 1) % 4
                return TMP[tmr[0]]
            psr = [0]

            def nextps():
                psr[0] = (psr[0] + 1) % 4
                return PS[psr[0]]
            yb = PS[4]

            def pass1(i, h):
                g, t4 = divmod(i, 4)
                qs = QSg[g % 2]
                n = (i + 1) * 128
                tsl = slice(t4 * 128, (t4 + 1) * 128)
                p0 = (h % 2) * 64
                lb, tsm, tot, zs = LB[h % 2], TSM[h % 2], TOT[h % 2], ZS[h % 2]
                memset("dve", tsm.t[:], 0.0, [tsm.r])
                for ci, (c0, w) in enumerate(chunks(n)):
                    ps = nextps()
                    mm(ps.t[:, 0:w], qs.t[p0:p0 + 64, h // 2, tsl], KS_.t[p0:p0 + 64, h // 2, c0:c0 + w], True, True,
                       [qs.r, KS_.r], [ps.r])
                    E1 = nexttmp()
                    act(E1.t[:, 0:w], ps.t[:, 0:w], AF.Exp, [ps.r], [E1.r])
                    cp("dve", zs.t[:, c0:c0 + w], ps.t[:, 0:w], [ps.r], [zs.r])
                    last = (c0 + w == n)
                    wf = w - 128 if last else w
                    if wf > 0:
                        act(lb.t[:, 1 + c0:1 + c0 + wf], E1.t[:, 0:wf], AF.Ln, [E1.r, oneT.r], [lb.r, tsm.r],
                            bias=oneT.t[:, 0:1], accum=tsm.t[:, ci:ci + 1])
                    if last:
                        act(lb.t[:, 1 + n - 128:1 + n], E1.t[:, w - 128:w], AF.Ln, [E1.r, oneT.r], [lb.r], bias=oneT.t[:, 0:1])
                        tt("dve", lb.t[:, 1 + n - 128:1 + n], lb.t[:, 1 + n - 128:1 + n], strict.t[:], ALU.mult,
                           [lb.r, strict.r], [lb.r])
                        red(tsm.t[:, 15:16], lb.t[:, 1 + n - 128:1 + n], ALU.add, [lb.r], [tsm.r])
                red(tot.t[:, 0:1], tsm.t[:], ALU.add, [tsm.r], [tot.r])
                ts("dve", tot.t[:, 1:2], tot.t[:, 0:1], -1.0, ALU.mult, [tot.r], [tot.r])

            def scan_part(i, h):
                n = (i + 1) * 128
                lb, tot, nlx = LB[h % 2], TOT[h % 2], NLX[h % 2]
                P.op("dve", lambda e: e.tensor_tensor_scan(out=nlx.t[:, 0:n], data0=lb.t[:, 0:n], data1=lb.t[:, 0:n],
                                                           initial=tot.t[:, 1:2], op0=ALU.add, op1=ALU.min),
                     [lb.r, tot.r], [nlx.r])

            pbr = [0]
            PB2 = [(PB.t[:], PB.r), (PS[5].t[:].bitcast(BF16), PS[5].r)]

            def pass2(i, h):
                g, t4 = divmod(i, 4)
                qs = QSg[g % 2]
                n = (i + 1) * 128
                tsl = slice(t4 * 128, (t4 + 1) * 128)
                p0 = (h % 2) * 64
                lb, tot, nlx, ab, pt = LB[h % 2], TOT[h % 2], NLX[h % 2], AB[h % 2], PT[h % 2]
                zs = ZS[h % 2]
                for ci, (c0, w) in enumerate(chunks(n)):
                    EB = nexttmp()
                    tt("dve", EB.t[:, 0:w], zs.t[:, c0:c0 + w], nlx.t[:, c0:c0 + w], ALU.add, [zs.r, nlx.r], [EB.r])
                    act(ab.t[:, c0:c0 + w], EB.t[:, 0:w], AF.Exp, [EB.r], [ab.r])
                tt("dve", ab.t[:, n - 128:n], ab.t[:, n - 128:n], strict.t[:], ALU.mult, [ab.r, strict.r], [ab.r])
                for j0 in range(0, i + 1, 8):
                    nbk = min(8, i + 1 - j0)
                    pbr[0] ^= 1
                    pbt, pbres = PB2[pbr[0]]
                    for jj in range(nbk):
                        j = j0 + jj
                        tr(pbt[:, jj * 128:(jj + 1) * 128], ab.t[:, j * 128:(j + 1) * 128], ident.t[:],
                           [ab.r, ident.r], [pbres], inc=(jj == nbk - 1))
                    cp("act", pt.t[:, j0:j0 + nbk, :], pbt[:, 0:nbk * 128].rearrange("p (j t) -> p j t", t=128),
                       [pbres], [pt.r])
                for j in range(i + 1):
                    mm(yb.t[:, h * 64:(h + 1) * 64], pt.t[:, j, :], VSX.t[:, j, h * 64:(h + 1) * 64], j == 0, j == i,
                       [pt.r, VSX.r], [yb.r])

            def load_group(g):
                g0 = g * 512
                dma("sp", QSg[g % 2].t[:], QST[:, :, g0:g0 + 512].rearrange("q p t -> p q t"), [r_QST], [QSg[g % 2].r])

            load_group(0)
            pass1(0, 0)
            for i in range(NT):
                g, t4 = divmod(i, 4)
                tsl = slice(t4 * 128, (t4 + 1) * 128)
                for h in range(8):
                    scan_part(i, h)
                    if h < 7:
                        pass1(i, h + 1)
                    elif i + 1 < NT:
                        if (i + 1) % 4 == 0:
                            load_group((i + 1) // 4)
                        pass1(i + 1, 0)
                    pass2(i, h)
                cp("act", YBs.t[:], yb.t[:, 0:512], [yb.r], [YBs.r])
                for q in range(4):
                    tr(PB.t[:, q * 128:(q + 1) * 128], YBs.t[:, q * 128:(q + 1) * 128], ident.t[:], [YBs.r, ident.r], [PB.r], inc=(q == 3))
                cp("act", YBg.t[:, :, tsl], PB.t[:, 0:512].rearrange("p (q t) -> p q t", t=128), [PB.r], [YBg.r])
                if t4 == 3:
                    g0 = g * 512
                    dma("sp", YBT[:, :, g0:g0 + 512].rearrange("q p t -> p q t"), YBg.t[:], [YBg.r], [r_YBT])
            P.barrier()

    def merge_phase(l, src, r_src, dst, r_dst):
        G = 256
        NG = S // G
        with ExitStack() as st:
            WG = alloc(st, "WG", [128, 8, 3 * D], BF16)
            WBR = alloc(st, "WBR", [128, 12, D], BF16)
            WO = alloc(st, "WO", [128, 8, D], BF16)
            BG = alloc(st, "BG", [128, 3, 8], F32)
            gT = alloc(st, "gT", [128, 8], F32); bT = alloc(st, "bT", [128, 8], F32)
            X32 = [alloc(st, "X32%d" % b, [128, 8, G], F32) for b in range(2)]
            Xb = [alloc(st, "Xb%d" % b, [128, 8, G], BF16) for b in range(2)]
            Y = [[alloc(st, "Y%d_%d" % (i, b), [128, 4, G], BF16) for i in range(3)] for b in range(2)]
            SGt = [alloc(st, "SGt%d" % i, [128, G], F32) for i in range(2)]
            TM = [alloc(st, "TM%d" % i, [128, G], F32) for i in range(2)]
            MGf = alloc(st, "MGf", [128, 8, G], F32)
            MGb = alloc(st, "MGb", [128, 8, G], BF16)
            Z = [alloc(st, "Z%d" % b, [128, 8, G], F32) for b in range(2)]
            SQ = alloc(st, "SQ", [128, 8, G], F32)
            RS = alloc(st, "RS", [128, G], F32)
            dma("sp", gT.t[:], lng[l, 1], [r_const], [gT.r])
            dma("sp", bT.t[:], lnb[l, 1], [r_const], [bT.r])
            dma("sp", BG.t[:], bgate[l].rearrange("i p c -> p i c"), [r_const], [BG.r])
            wload(WG, 8, w_mix[l], C_G, PMIX)
            for i in range(3):
                for k in range(4):
                    dma("pool", WBR.t[:, i * 4 + k, :], wbr[l, i, k * 128:(k + 1) * 128, :], [r_const], [WBR.r], max_dma_last_dim=8192)
            wload(WO, 8, wmo[l], 0, D)
            srcs = ((YAT, r_YAT), (YBT, r_YBT), (YCT, r_YCT))

            def load(g):
                b = g % 2
                g0 = g * G
                dma("sp", X32[b].t[:], src[:, :, g0:g0 + G].rearrange("c p t -> p c t"), [r_src], [X32[b].r])
                cp("pool", Xb[b].t[:], X32[b].t[:], [X32[b].r], [Xb[b].r])
                for i in range(3):
                    dma("sp", Y[b][i].t[:], srcs[i][0][:, :, g0:g0 + G].rearrange("q p t -> p q t"), [srcs[i][1]], [Y[b][i].r])

            def stage1(g):
                xb, y = Xb[g % 2], Y[g % 2]
                n_ = 0
                for c in range(8):
                    for i in range(3):
                        pg = PS[n_ % 2]; pb_ = PS[2 + n_ % 2]; sg = SGt[n_ % 2]; tm = TM[n_ % 2]
                        n_ += 1
                        col = i * D + c * 128
                        for k in range(8):
                            mm(pg.t[:, 0:G], WG.t[:, k, col:col + 128], xb.t[:, k, :], k == 0, k == 7, WG.rl + [xb.r], [pg.r])
                        act(sg.t[:], pg.t[:, 0:G], AF.Sigmoid, [pg.r, BG.r], [sg.r], bias=BG.t[:, i, c:c + 1])
                        for k in range(4):
                            mm(pb_.t[:, 0:G], WBR.t[:, i * 4 + k, c * 128:(c + 1) * 128], y[i].t[:, k, :], k == 0, k == 3,
                               [WBR.r, y[i].r], [pb_.r])
                        if i == 0:
                            tt("dve", MGf.t[:, c, :], sg.t[:], pb_.t[:, 0:G], ALU.mult, [sg.r, pb_.r], [MGf.r])
                        else:
                            tt("dve", tm.t[:], sg.t[:], pb_.t[:, 0:G], ALU.mult, [sg.r, pb_.r], [tm.r])
                            tt("pool", MGf.t[:, c, :], MGf.t[:, c, :], tm.t[:], ALU.add, [MGf.r, tm.r], [MGf.r])
                cp("pool", MGb.t[:], MGf.t[:], [MGf.r], [MGb.r])

            def stage2(g):
                x32, z = X32[g % 2], Z[g % 2]
                for c in range(8):
                    po = PS[4 + (c % 2)]
                    for k in range(8):
                        mm(po.t[:, 0:G], WO.t[:, k, c * 128:(c + 1) * 128], MGb.t[:, k, :], k == 0, k == 7, WO.rl + [MGb.r], [po.r])
                    stt(z.t[:, c, :], x32.t[:, c, :], ALPHA, po.t[:, 0:G], ALU.mult, ALU.add, [x32.r, po.r], [z.r])

            load(0)
            if NG > 1:
                load(1)
            stage1(0)
            stage2(0)
            for g in range(NG):
                if g + 2 < NG:
                    load(g + 2)
                if g + 1 < NG:
                    stage1(g + 1)
                ln_part1(Z[g % 2], SQ, G, PS[6])
                if g + 1 < NG:
                    stage2(g + 1)
                ln_part2(Z[g % 2], SQ, RS, G, gT, bT, dst, r_dst, g * G, PS[6])
            P.barrier()

    def xattn_phase(l, src, r_src, dst, r_dst):
        G = 256
        with ExitStack() as st:
            WQ = alloc(st, "WQ", [128, 8, 512], BF16)
            WKV = alloc(st, "WKV", [128, 8, D], BF16)
            WO = alloc(st, "WOx", [128, 4, D], BF16)
            gT = alloc(st, "gT", [128, 8], F32); bT = alloc(st, "bT", [128, 8], F32)
            MT = alloc(st, "MT", [128, 8, 256], BF16)
            KT = alloc(st, "KT", [128, 4, 256], BF16)
            VX = alloc(st, "VX", [128, 2, 512], BF16)
            X32 = [alloc(st, "X32%d" % b_, [128, 8, G], F32) for b_ in range(2)]
            Xb = [alloc(st, "Xb%d" % b_, [128, 8, G], BF16) for b_ in range(2)]
            QT = alloc(st, "QT", [128, 4, G], BF16)
            PTm = [alloc(st, "PTm%d" % i, [128, G], BF16) for i in range(2)]
            RD = alloc(st, "RD", [128, G], F32)
            OT = alloc(st, "OT", [128, 4, G], BF16)
            Z = [alloc(st, "Z%d" % b_, [128, 8, G], F32) for b_ in range(2)]
            SQ = alloc(st, "SQ", [128, 8, G], F32)
            RS = alloc(st, "RS", [128, G], F32)
            dma("sp", gT.t[:], lng[l, 2], [r_const], [gT.r])
            dma("sp", bT.t[:], lnb[l, 2], [r_const], [bT.r])
            wload(WQ, 8, xwq[l], 0, 512)
            wload(WKV, 8, xwkv[l], 0, D)
            wload(WO, 4, xwo[l], 0, D)
            dma("pool", MT.t[:], memT.rearrange("c p m -> p c m"), [r_const], [MT.r])
            for h in range(4):
                ps = PS[h % 2]
                for k in range(8):
                    mm(ps.t[:, 0:256], WKV.t[:, k, h * 128:(h + 1) * 128], MT.t[:, k, :], k == 0, k == 7, WKV.rl + [MT.r], [ps.r])
                cp("act", KT.t[:, h, :], ps.t[:, 0:256], [ps.r], [KT.r])
            for mt in range(2):
                ps = PS[2 + mt]
                for k in range(8):
                    mm(ps.t[:, 0:512], MT.t[:, k, mt * 128:(mt + 1) * 128], WKV.t[:, k, 512:1024], k == 0, k == 7, WKV.rl + [MT.r], [ps.r])
                cp("act", VX.t[:, mt, :], ps.t[:, 0:512], [ps.r], [VX.r])
            def load(g):
                b_ = g % 2
                g0 = g * G
                dma("sp", X32[b_].t[:], src[:, :, g0:g0 + G].rearrange("c p t -> p c t"), [r_src], [X32[b_].r])
                cp("pool", Xb[b_].t[:], X32[b_].t[:], [X32[b_].r], [Xb[b_].r])

            def attn(g):
                xb = Xb[g % 2]
                for h in range(4):
                    ps = PS[h % 2]
                    for k in range(8):
                        mm(ps.t[:, 0:G], WQ.t[:, k, h * 128:(h + 1) * 128], xb.t[:, k, :], k == 0, k == 7, WQ.rl + [xb.r], [ps.r])
                    act(QT.t[:, h, :], ps.t[:, 0:G], AF.Copy, [ps.r], [QT.r], scale=128.0 ** -0.5)
                for h in range(4):
                    for mt in range(2):
                        ps = PS[mt]
                        mm(ps.t[:, 0:G], KT.t[:, h, mt * 128:(mt + 1) * 128], QT.t[:, h, :], True, True, [KT.r, QT.r], [ps.r])
                        act(PTm[mt].t[:], ps.t[:, 0:G], AF.Exp, [ps.r], [PTm[mt].r])
                    po = PS[2]; pd = PS[3]
                    for mt in range(2):
                        mm(po.t[:, 0:G], VX.t[:, mt, h * 128:(h + 1) * 128], PTm[mt].t[:], mt == 0, mt == 1, [VX.r, PTm[mt].r], [po.r])
                    for mt in range(2):
                        mm(pd.t[:, 0:G], onesB.t[:], PTm[mt].t[:], mt == 0, mt == 1, [onesB.r, PTm[mt].r], [pd.r])
                    recip(RD.t[:], pd.t[:, 0:G], [pd.r], [RD.r])
                    tt("dve", OT.t[:, h, :], po.t[:, 0:G], RD.t[:], ALU.mult, [po.r, RD.r], [OT.r])

            def outp(g):
                x32, z = X32[g % 2], Z[g % 2]
                for c in range(8):
                    po = PS[4 + (c % 2)]
                    for k in range(4):
                        mm(po.t[:, 0:G], WO.t[:, k, c * 128:(c + 1) * 128], OT.t[:, k, :], k == 0, k == 3, WO.rl + [OT.r], [po.r])
                    stt(z.t[:, c, :], x32.t[:, c, :], ALPHA, po.t[:, 0:G], ALU.mult, ALU.add, [x32.r, po.r], [z.r])

            NG = S // G
            load(0)
            if NG > 1:
                load(1)
            attn(0)
            outp(0)
            for g in range(NG):
                if g + 2 < NG:
                    load(g + 2)
                if g + 1 < NG:
                    attn(g + 1)
                ln_part1(Z[g % 2], SQ, G, PS[6])
                if g + 1 < NG:
                    outp(g + 1)
                ln_part2(Z[g % 2], SQ, RS, G, gT, bT, dst, r_dst, g * G, PS[6])
            P.barrier()

    P.barrier()
    cur, r_cur = xT, r_xT
    nph = [0]

    def run(f, *a):
        if nph[0] < stop_after:
            f(*a)
        nph[0] += 1
    for l in range(L):
        last = (l == L - 1)
        run(ffn_phase, l, 0, 0, cur, r_cur, XB, r_XB)
        run(proj_phase, l, XB, r_XB)
        run(dsa_phase, l)
        run(sb_phase, l)
        run(merge_phase, l, XB, r_XB, XA, r_XA)
        run(xattn_phase, l, XA, r_XA, XB, r_XB)
        if last:
            run(ffn_phase, l, 1, 3, XB, r_XB, outT, r_outT)
        else:
            run(ffn_phase, l, 1, 3, XB, r_XB, XA, r_XA)
        cur, r_cur = XA, r_XA
    P.emit()
    top.close()
    return nc, P


def _bucket_table():
    import math
    n = np.arange(256)
    nf = np.maximum(n, 1).astype(np.float32)
    large = 16 + (np.log(nf / np.float32(16)) / np.float32(math.log(8.0)) * np.float32(16)).astype(np.int32)
    large = np.minimum(large, 31)
    return np.where(n < 16, n, large)


def prep_inputs(x, mem, ln_g, ln_b, ffn_w_in, ffn_w_out, w_mix_in, b_gate, kv_norm_g, w_uk, w_uv,
                conv_w, w_branch, w_mix_out, xa_wq, xa_wkv, xa_wo, rel_bias):
    f = lambda a: np.ascontiguousarray(np.asarray(a, dtype=np.float32))
    B, S, _ = x.shape
    L = ln_g.shape[0]
    bt = _bucket_table()
    tl = np.arange(128)[:, None]
    col = np.arange(256)[None, :]
    nrel = np.where(col < 128, tl - col + 128, tl - (col - 128))
    idx = bt[np.clip(nrel, 0, 255)]
    rel_bias = f(rel_bias)
    biasT = f(np.transpose(rel_bias[idx], (0, 2, 1)))
    shared = {
        "lng": f(np.transpose(f(ln_g).reshape(L, 4, 8, 128), (0, 1, 3, 2))),
        "lnb": f(np.transpose(f(ln_b).reshape(L, 4, 8, 128), (0, 1, 3, 2))),
        "ffn_w_in": f(ffn_w_in), "ffn_w_out": f(ffn_w_out), "w_mix_in": f(w_mix_in),
        "bgate": f(np.transpose(f(b_gate).reshape(L, 3, 8, 128), (0, 1, 3, 2))),
        "kvg": f(kv_norm_g),
        "wuk": f(np.transpose(f(w_uk).reshape(L, 4, 2, 64, 128), (0, 2, 3, 1, 4)).reshape(L, 128, 4, 128)),
        "wuv": f(w_uv),
        "convw": f(np.transpose(f(conv_w).reshape(L, 3, 4, 128), (0, 3, 2, 1))),
        "w_branch": f(w_branch), "w_mix_out": f(w_mix_out),
        "xa_wq": f(xa_wq), "xa_wkv": f(xa_wkv), "xa_wo": f(xa_wo),
        "biasT": biasT, "rb31": f(rel_bias[31]),
    }
    maps = []
    for b in range(B):
        m = dict(shared)
        m["xT"] = f(f(x[b]).T.reshape(8, 128, S))
        m["memT"] = f(f(mem[b]).T.reshape(8, 128, 256))
        maps.append(m)
    return maps


_NC_CACHE = {}


def kernel(**inputs):
    x = np.asarray(inputs["x"])
    B, S, _ = x.shape
    L = np.asarray(inputs["ln_g"]).shape[0]
    key = (S, L)
    if key not in _NC_CACHE:
        _NC_CACHE[key] = build(S, L)[0]
    nc = _NC_CACHE[key]
    maps = prep_inputs(**inputs)
    res = run_bass_kernel_spmd(nc, maps, core_ids=list(range(B)))
    out = np.empty((B, S, D), dtype=np.float32)
    for b in range(B):
        out[b] = np.asarray(res.results[b]["outT"]).reshape(D, S).T
    return out
```

```python
import numpy as np
from contextlib import ExitStack
import concourse.bass as bass
import concourse.mybir as mybir
from concourse.bass_utils import run_bass_kernel_spmd

F32 = mybir.dt.float32
BF16 = mybir.dt.bfloat16
AF = mybir.ActivationFunctionType
ALU = mybir.AluOpType
AX = mybir.AxisListType


class Res:
    __slots__ = ("name", "w", "r", "excl", "dram")

    def __init__(self, name, excl=False):
        self.name = name
        self.w = None
        self.r = {}
        self.excl = excl
        self.dram = False


class Prog:
    ENG = ("pe", "act", "dve", "pool", "sp")

    def __init__(self, nc, n_slots=12):
        self.nc = nc
        self.ops = {e: [] for e in self.ENG}
        self.cnt = {e: 0 for e in self.ENG}
        self.seen = {e: {} for e in self.ENG}
        self.n_slots = n_slots
        self.slot_uses = {q: [0] * n_slots for q in ("sp", "pool", "act")}
        self.slot_rr = {q: 0 for q in ("sp", "pool", "act")}
        self.pe_pending = False
        self.nops = 0

    def _deps(self, eng, reads, writes):
        deps = {}

        def add(kv):
            if kv is None:
                return
            k, v = kv
            if deps.get(k, 0) < v:
                deps[k] = v
        for r in reads:
            add(r.w)
            if r.excl:
                for k, v in r.r.items():
                    add((k, v))
        for w in writes:
            add(w.w)
            for k, v in w.r.items():
                add((k, v))
        out = []
        seen = self.seen[eng]
        for k, v in deps.items():
            if k == "pe" and eng == "pe":
                continue
            if seen.get(k, 0) >= v:
                continue
            seen[k] = v
            out.append((k, v))
        return out

    def _register(self, key, val, reads, writes):
        for r in reads:
            if r.excl:
                r.w = (key, val)
                r.r = {}
            else:
                if r.r.get(key, 0) < val:
                    r.r[key] = val
        for w in writes:
            w.w = (key, val)
            w.r = {}

    def op(self, eng, emit, reads=(), writes=(), inc=True):
        waits = self._deps(eng, reads, writes)
        if inc:
            self.cnt[eng] += 1
            val = self.cnt[eng]
            if eng == "pe":
                self.pe_pending = False
        else:
            assert eng == "pe"
            val = self.cnt[eng] + 1
            self.pe_pending = True
        self._register(eng, val, reads, writes)
        self.ops[eng].append((waits, emit, (eng, 1) if inc else None))
        self.nops += 1

    def dma(self, q, emit, reads=(), writes=()):
        j = self.slot_rr[q]
        self.slot_rr[q] = (j + 1) % self.n_slots
        n = self.slot_uses[q][j]
        key = ("dma", q, j)
        waits = self._deps(q, reads, writes)
        if n > 0 and self.seen[q].get(key, 0) < 16 * n:
            self.seen[q][key] = 16 * n
            waits.append((key, 16 * n))
        self.slot_uses[q][j] = n + 1
        val = 16 * (n + 1)
        self._register(key, val, reads, writes)
        self.ops[q].append((waits, emit, (key, 16)))
        self.nops += 1

    def barrier(self):
        assert not self.pe_pending
        targets = [(e, self.cnt[e]) for e in ("pe", "act", "dve", "pool") if self.cnt[e] > 0]
        for q in self.slot_uses:
            for j, n in enumerate(self.slot_uses[q]):
                if n > 0:
                    targets.append((("dma", q, j), 16 * n))
        for e in self.ENG:
            waits = []
            for k, v in targets:
                if k == "pe" and e == "pe":
                    continue
                if self.seen[e].get(k, 0) < v:
                    self.seen[e][k] = v
                    waits.append((k, v))
            self.ops[e].append((waits, None, None))

    def wait_all(self, eng, resources):
        waits = self._deps(eng, resources, ())
        self.ops[eng].append((waits, None, None))

    def emit(self):
        nc = self.nc
        keys = set(self.ENG)
        for q in self.slot_uses:
            for j in range(self.n_slots):
                if self.slot_uses[q][j] > 0:
                    keys.add(("dma", q, j))
        with ExitStack() as es:
            sems = {}
            for k in sorted(keys, key=str):
                nm = k if isinstance(k, str) else "d_%s_%d" % (k[1], k[2])
                sems[k] = es.enter_context(nc.semaphore("s_" + nm))
            block = es.enter_context(nc.Block())

            def run(e, name):
                for waits, emit, inc in self.ops[name]:
                    for k, v in waits:
                        e.wait_ge(sems[k], v)
                    if emit is None:
                        continue
                    ins = emit(e)
                    if inc is not None:
                        ins.then_inc(sems[inc[0]], inc[1])

            @block.tensor
            def _(e):
                run(e, "pe")

            @block.scalar
            def _(e):
                run(e, "act")

            @block.vector
            def _(e):
                run(e, "dve")

            @block.gpsimd
            def _(e):
                run(e, "pool")

            @block.sync
            def _(e):
                run(e, "sp")

D = 1024
DFF = 2816
PMIX = 7368
ALPHA = 4.0 ** 0.25
EPS = 1e-5
KTOP = 256
NIT = 14
NEG = -1.0e30


class Tl:
    def __init__(self, t, name, excl=False):
        self.t = t
        self.r = Res(name, excl)
        self.rl = [self.r]

    def __getitem__(self, k):
        return self.t[k]


def build(S=4096, L=2, dbg=False, stop_after=99):
    NT = S // 128
    nc = bass.Bass("TRN2", target_bir_lowering=False)
    P = Prog(nc)

    def din(name, shape, dt=F32):
        return nc.dram_tensor(name, list(shape), dt, kind="ExternalInput").ap()

    def dres(name):
        r = Res(name)
        r.dram = True
        return r

    def dscr(name, shape, dt):
        return nc.dram_tensor(name, list(shape), dt, kind=("ExternalOutput" if dbg else "Internal")).ap(), dres(name)

    xT = din("xT", [8, 128, S]); r_xT = dres("xT")
    memT = din("memT", [8, 128, 256])
    lng = din("lng", [L, 4, 128, 8]); lnb = din("lnb", [L, 4, 128, 8])
    w_in = din("ffn_w_in", [L, 2, D, 2 * DFF]); w_out = din("ffn_w_out", [L, 2, DFF, D])
    w_mix = din("w_mix_in", [L, D, PMIX])
    bgate = din("bgate", [L, 3, 128, 8])
    kvg = din("kvg", [L, 128])
    wuk = din("wuk", [L, 128, 4, 128])
    wuv = din("wuv", [L, 8, 128, 64])
    convw = din("convw", [L, 128, 4, 3])
    wbr = din("w_branch", [L, 3, 512, D])
    wmo = din("w_mix_out", [L, D, D])
    xwq = din("xa_wq", [L, D, 512]); xwkv = din("xa_wkv", [L, D, D]); xwo = din("xa_wo", [L, 512, D])
    biasT = din("biasT", [128, 8, 256])
    rb31 = din("rb31", [8])
    outT = nc.dram_tensor("outT", [8, 128, S], F32, kind="ExternalOutput").ap(); r_outT = dres("outT")
    r_const = dres("const_in")

    XA, r_XA = dscr("XA", [8, 128, S], F32)
    XB, r_XB = dscr("XB", [8, 128, S], F32)
    QL, r_QL = dscr("QL", [8, 128, S], BF16)
    IQT, r_IQT = dscr("IQT", [4, 128, S], BF16)
    IKT, r_IKT = dscr("IKT", [64, S], BF16)
    QST, r_QST = dscr("QST", [4, 128, S], BF16)
    KST, r_KST = dscr("KST", [4, 128, S], BF16)
    VS, r_VS = dscr("VS", [S, 512], BF16)
    CKV, r_CKV = dscr("CKV", [S, 128], BF16)
    CKVT, r_CKVT = dscr("CKVT", [128, S], BF16)
    IW, r_IW = dscr("IW", [S, 8], F32)
    YCT, r_YCT = dscr("YCT", [4, 128, S], BF16)
    YAT, r_YAT = dscr("YAT", [4, 128, S], BF16)
    YBT, r_YBT = dscr("YBT", [4, 128, S], BF16)

    def mm(out, lhsT, rhs, start, stop, reads, writes, inc=None):
        P.op("pe", lambda e: e.matmul(out, lhsT=lhsT, rhs=rhs, start=start, stop=stop),
             reads, writes, inc=(stop if inc is None else inc))

    def tr(out, in_, ident, reads, writes, inc=True):
        P.op("pe", lambda e: e.transpose(out=out, in_=in_, identity=ident), reads, writes, inc=inc)

    def act(out, in_, func, reads, writes, bias=None, scale=None, accum=None):
        kw = {}
        if bias is not None:
            kw["bias"] = bias
        if scale is not None:
            kw["scale"] = scale
        if accum is not None:
            kw["accum_out"] = accum
        P.op("act", lambda e: e.activation(out=out, in_=in_, func=func, **kw), reads, writes)

    def tt(eng, out, a, b, op, reads, writes):
        P.op(eng, lambda e: e.tensor_tensor(out=out, in0=a, in1=b, op=op), reads, writes)

    def ts(eng, out, a, s1, op0, reads, writes, s2=None, op1=None, accum=None):
        kw = {}
        if op1 is not None:
            kw["op1"] = op1
        if accum is not None:
            kw["accum_out"] = accum
        P.op(eng, lambda e: e.tensor_scalar(out=out, in0=a, scalar1=s1, scalar2=s2, op0=op0, **kw), reads, writes)

    def stt(out, in0, scalar, in1, op0, op1, reads, writes, accum=None):
        kw = {}
        if accum is not None:
            kw["accum_out"] = accum
        P.op("dve", lambda e: e.scalar_tensor_tensor(out=out, in0=in0, scalar=scalar, in1=in1, op0=op0, op1=op1, **kw),
             reads, writes)

    def cp(eng, out, in_, reads, writes):
        if eng == "act":
            P.op("act", lambda e: e.copy(out=out, in_=in_), reads, writes)
        else:
            P.op(eng, lambda e: e.tensor_copy(out=out, in_=in_), reads, writes)

    def red(out, in_, op, reads, writes):
        P.op("dve", lambda e: e.tensor_reduce(out=out, in_=in_, axis=AX.X, op=op), reads, writes)

    def recip(out, in_, reads, writes):
        P.op("dve", lambda e: e.reciprocal(out=out, in_=in_), reads, writes)

    def memset(eng, out, val, writes):
        P.op(eng, lambda e: e.memset(out, val), (), writes)

    def dma(q, out, in_, reads, writes, **kw):
        reads = [r for r in reads if not r.dram]
        writes = [r for r in writes if not r.dram]
        P.dma(q, lambda e: e.dma_start(out=out, in_=in_, **kw), reads, writes)

    def wload(dst, k_chunks, src2d, c0, c1, rows0=0):
        dst.rl = [Res("%s_k%d" % (dst.r.name, k)) for k in range(k_chunks)]
        for k in range(k_chunks):
            dma("pool", dst.t[:, k, 0:c1 - c0], src2d[rows0 + k * 128: rows0 + (k + 1) * 128, c0:c1],
                [r_const] + ([dst.rl[k - 1]] if k > 0 else []), [dst.rl[k]], max_dma_last_dim=8192)

    top = ExitStack()

    uid = [0]

    def alloc(stack, name, shape, dt):
        uid[0] += 1
        name = "%s_%d" % (name, uid[0])
        return Tl(stack.enter_context(nc.sbuf_tensor(name, list(shape), dt)), name)

    PS = [Tl(top.enter_context(nc.psum_tensor("ps%d" % i, [128, 512], F32)), "ps%d" % i, True) for i in range(7)]
    PB = Tl(top.enter_context(nc.psum_tensor("pb", [128, 1024], BF16)), "pb", True)

    identf = alloc(top, "identf", [128, 128], F32)
    ident = alloc(top, "ident", [128, 128], BF16)
    onesM = alloc(top, "onesM", [128, 128], F32)
    onesB = alloc(top, "onesB", [128, 128], BF16)
    caus = alloc(top, "caus", [128, 128], F32)
    strict = alloc(top, "strict", [128, 128], F32)
    pow2 = alloc(top, "pow2", [128, NIT], F32)
    neglo = alloc(top, "neglo", [128, 1], F32)
    epsT = alloc(top, "epsT", [128, 1], F32)
    oneT = alloc(top, "oneT", [128, 1], F32)
    memset("pool", identf.t[:], 0.0, [identf.r])
    P.op("pool", lambda e: e.affine_select(out=identf.t[:], in_=identf.t[:], pattern=[[-1, 128]],
                                           compare_op=ALU.not_equal, fill=1.0, base=0, channel_multiplier=1),
         [identf.r], [identf.r])
    cp("dve", ident.t[:], identf.t[:], [identf.r], [ident.r])
    memset("pool", onesM.t[:], 1.0 / D, [onesM.r])
    memset("pool", onesB.t[:], 1.0, [onesB.r])
    memset("pool", caus.t[:], 0.0, [caus.r])
    P.op("pool", lambda e: e.affine_select(out=caus.t[:], in_=caus.t[:], pattern=[[-1, 128]],
                                           compare_op=ALU.is_ge, fill=NEG, base=0, channel_multiplier=1),
         [caus.r], [caus.r])
    memset("pool", strict.t[:], 1.0, [strict.r])
    P.op("pool", lambda e: e.affine_select(out=strict.t[:], in_=strict.t[:], pattern=[[-1, 128]],
                                           compare_op=ALU.is_gt, fill=0.0, base=0, channel_multiplier=1),
         [strict.r], [strict.r])
    for j in range(NIT):
        memset("pool", pow2.t[:, j:j + 1], 0.5 ** (j + 1), [pow2.r])
    memset("pool", neglo.t[:], -1.0e29, [neglo.r])
    memset("pool", epsT.t[:], EPS, [epsT.r])
    memset("pool", oneT.t[:], 1.0, [oneT.r])

    def ln_part1(Z, SQ, G, pa):
        for c in range(8):
            mm(pa.t[:, 0:G], onesM.t[:], Z.t[:, c, :], c == 0, c == 7, [onesM.r, Z.r], [pa.r])
        for c in range(8):
            tt("dve", Z.t[:, c, :], Z.t[:, c, :], pa.t[:, 0:G], ALU.subtract, [Z.r, pa.r], [Z.r])
        act(SQ.t[:], Z.t[:], AF.Square, [Z.r], [SQ.r])

    def ln_part2(Z, SQ, RS, G, gT, bT, dst, r_dst, g0, pb_):
        for c in range(8):
            mm(pb_.t[:, 0:G], onesM.t[:], SQ.t[:, c, :], c == 0, c == 7, [onesM.r, SQ.r], [pb_.r])
        act(RS.t[:], pb_.t[:, 0:G], AF.Sqrt, [pb_.r, epsT.r], [RS.r], bias=epsT.t[:, 0:1])
        recip(RS.t[:], RS.t[:], [RS.r], [RS.r])
        for c in range(8):
            tt("pool", Z.t[:, c, :], Z.t[:, c, :], RS.t[:], ALU.mult, [Z.r, RS.r], [Z.r])
        for c in range(8):
            act(SQ.t[:, c, :], Z.t[:, c, :], AF.Identity, [Z.r, gT.r, bT.r], [SQ.r],
                scale=gT.t[:, c:c + 1], bias=bT.t[:, c:c + 1])
        dma("sp", dst[:, :, g0:g0 + G].rearrange("c p t -> p c t"), SQ.t[:], [SQ.r], [r_dst])

    def layer_norm(Z, SQ, RS, G, gT, bT, dst, r_dst, g0, pa, pb_):
        ln_part1(Z, SQ, G, pa)
        ln_part2(Z, SQ, RS, G, gT, bT, dst, r_dst, g0, pb_)

    def ffn_phase(l, which, ln_i, src, r_src, dst, r_dst):
        G = 256
        NG = S // G
        with ExitStack() as st:
            Win = alloc(st, "Win", [128, 8, 2 * DFF], BF16)
            Wout = alloc(st, "Wout", [128, 22, D], BF16)
            gT = alloc(st, "gT", [128, 8], F32); bT = alloc(st, "bT", [128, 8], F32)
            X32 = [alloc(st, "X32%d" % b, [128, 8, G], F32) for b in range(2)]
            Xb = [alloc(st, "Xb%d" % b, [128, 8, G], BF16) for b in range(2)]
            H = alloc(st, "H", [128, 22, G], BF16)
            SA = [alloc(st, "SA%d" % i, [128, G], F32) for i in range(2)]
            Z = [alloc(st, "Z%d" % b, [128, 8, G], F32) for b in range(2)]
            SQ = alloc(st, "SQ", [128, 8, G], F32)
            RS = alloc(st, "RS", [128, G], F32)
            dma("sp", gT.t[:], lng[l, ln_i], [r_const], [gT.r])
            dma("sp", bT.t[:], lnb[l, ln_i], [r_const], [bT.r])
            wload(Win, 8, w_in[l, which], 0, 2 * DFF)
            wload(Wout, 22, w_out[l, which], 0, D)

            def load(g):
                b = g % 2
                g0 = g * G
                dma("sp", X32[b].t[:], src[:, :, g0:g0 + G].rearrange("c p t -> p c t"), [r_src], [X32[b].r])
                cp("pool", Xb[b].t[:], X32[b].t[:], [X32[b].r], [Xb[b].r])

            def inproj(g):
                xb = Xb[g % 2]
                for j in range(22):
                    pa = PS[(2 * j) % 4]; pb_ = PS[(2 * j + 1) % 4]
                    for k in range(8):
                        mm(pa.t[:, 0:G], Win.t[:, k, j * 128:(j + 1) * 128], xb.t[:, k, :], k == 0, k == 7,
                           Win.rl + [xb.r], [pa.r])
                    for k in range(8):
                        mm(pb_.t[:, 0:G], Win.t[:, k, DFF + j * 128:DFF + (j + 1) * 128], xb.t[:, k, :], k == 0, k == 7,
                           Win.rl + [xb.r], [pb_.r])
                    sa = SA[j % 2]
                    act(sa.t[:], pa.t[:, 0:G], AF.Silu, [pa.r], [sa.r])
                    stt(H.t[:, j, :], sa.t[:], 0.5, pb_.t[:, 0:G], ALU.mult, ALU.mult, [sa.r, pb_.r], [H.r])

            def outproj(g):
                x32, z = X32[g % 2], Z[g % 2]
                for c in range(8):
                    po = PS[4 + (c % 2)]
                    for j in range(22):
                        mm(po.t[:, 0:G], Wout.t[:, j, c * 128:(c + 1) * 128], H.t[:, j, :], j == 0, j == 21,
                           Wout.rl + [H.r], [po.r])
                    stt(z.t[:, c, :], x32.t[:, c, :], ALPHA, po.t[:, 0:G], ALU.mult, ALU.add, [x32.r, po.r], [z.r])

            load(0)
            if NG > 1:
                load(1)
            inproj(0)
            outproj(0)
            for g in range(NG):
                if g + 2 < NG:
                    load(g + 2)
                if g + 1 < NG:
                    inproj(g + 1)
                ln_part1(Z[g % 2], SQ, G, PS[6])
                if g + 1 < NG:
                    outproj(g + 1)
                ln_part2(Z[g % 2], SQ, RS, G, gT, bT, dst, r_dst, g * G, PS[6])
            P.barrier()

    C_QA, C_CKV, C_IQ, C_IK, C_IW, C_QS, C_KS, C_VS, C_CB, C_CC, C_CH, C_G = \
        0, 512, 640, 1152, 1216, 1224, 1736, 2248, 2760, 3272, 3784, 4296

    def proj_phase(l, src, r_src):
        G = 512
        NW = C_G
        with ExitStack() as st:
            Wm = alloc(st, "Wm", [128, 8, NW], BF16)
            WUK = alloc(st, "WUK", [128, 4, 128], BF16)
            CW = alloc(st, "CW", [128, 4, 3], F32)
            KVG = alloc(st, "KVG", [128, 128], F32)
            X32 = alloc(st, "X32", [128, 8, G], F32)
            Xb = alloc(st, "Xb", [128, 8, G], BF16)
            QA = alloc(st, "QA", [128, 4, G], BF16)
            OB = [alloc(st, "OB%d" % i, [128, G], BF16) for i in range(3)]
            CCs = alloc(st, "CCs", [128, G], F32)
            ZC = [alloc(st, "ZC%d" % q, [128, G + 2], F32) for q in range(4)]
            YC = alloc(st, "YC", [128, G], F32)
            JK = alloc(st, "JK", [128, 128], F32)
            SS = alloc(st, "SS", [128, 2], F32)
            CKb = alloc(st, "CKb", [128, 128], BF16)
            CKTb = alloc(st, "CKTb", [128, 128], BF16)
            IWs = alloc(st, "IWs", [128, 8], F32)
            VSb = alloc(st, "VSb", [128, 512], BF16)
            wload(Wm, 8, w_mix[l], 0, NW)
            dma("pool", WUK.t[:], wuk[l], [r_const], [WUK.r])
            dma("sp", CW.t[:], convw[l], [r_const], [CW.r])
            dma("sp", KVG.t[:], kvg[l].partition_broadcast(128), [r_const], [KVG.r])
            for q in range(4):
                memset("pool", ZC[q].t[:, 0:2], 0.0, [ZC[q].r])
            rot = [0]

            def nextps():
                rot[0] = (rot[0] + 1) % 5
                return PS[rot[0]]
            obr = [0]

            def nextob():
                obr[0] = (obr[0] + 1) % 3
                return OB[obr[0]]

            def fm_chunk(col, M=128):
                ps = nextps()
                for k in range(8):
                    mm(ps.t[0:M, 0:G], Wm.t[:, k, col:col + M], Xb.t[:, k, :], k == 0, k == 7, Wm.rl + [Xb.r], [ps.r])
                return ps

            for g in range(S // G):
                g0 = g * G
                dma("sp", X32.t[:], src[:, :, g0:g0 + G].rearrange("c p t -> p c t"), [r_src], [X32.r])
                cp("pool", Xb.t[:], X32.t[:], [X32.r], [Xb.r])
                for q in range(4):
                    ps = fm_chunk(C_QA + q * 128)
                    cp("act", QA.t[:, q, :], ps.t[:, 0:G], [ps.r], [QA.r])
                for h in range(8):
                    p0 = (h % 2) * 64
                    ps = nextps()
                    mm(ps.t[:, 0:G], WUK.t[p0:p0 + 64, h // 2, :], QA.t[p0:p0 + 64, h // 2, :], True, True,
                       [WUK.r, QA.r], [ps.r])
                    ob = nextob()
                    act(ob.t[:], ps.t[:, 0:G], AF.Copy, [ps.r], [ob.r], scale=0.125)
                    dma("sp", QL[h, :, g0:g0 + G], ob.t[:], [ob.r], [r_QL])
                for (col, dstT, r_d, sc) in ((C_IQ, IQT, r_IQT, 1.0), (C_QS, QST, r_QST, 0.125), (C_KS, KST, r_KST, 1.0)):
                    for q in range(4):
                        ps = fm_chunk(col + q * 128)
                        ob = nextob()
                        act(ob.t[:], ps.t[:, 0:G], AF.Copy, [ps.r], [ob.r], scale=sc)
                        dma("sp", dstT[q, :, g0:g0 + G], ob.t[:], [ob.r], [r_d])
                ps = fm_chunk(C_IK, 64)
                ob = nextob()
                cp("act", ob.t[0:64, :], ps.t[0:64, 0:G], [ps.r], [ob.r])
                dma("sp", IKT[:, g0:g0 + G], ob.t[0:64, :], [ob.r], [r_IKT])
                for q in range(4):
                    ps = fm_chunk(C_CC + q * 128)
                    cp("act", CCs.t[:], ps.t[:, 0:G], [ps.r], [CCs.r])
                    ps = fm_chunk(C_CH + q * 128)
                    zc = ZC[q]
                    tt("dve", zc.t[:, 2:G + 2], CCs.t[:], ps.t[:, 0:G], ALU.mult, [CCs.r, ps.r], [zc.r])
                    ts("dve", YC.t[:], zc.t[:, 2:G + 2], CW.t[:, q, 2:3], ALU.mult, [zc.r, CW.r], [YC.r])
                    stt(YC.t[:], zc.t[:, 1:G + 1], CW.t[:, q, 1:2], YC.t[:], ALU.mult, ALU.add, [zc.r, CW.r, YC.r], [YC.r])
                    stt(YC.t[:], zc.t[:, 0:G], CW.t[:, q, 0:1], YC.t[:], ALU.mult, ALU.add, [zc.r, CW.r, YC.r], [YC.r])
                    ps = fm_chunk(C_CB + q * 128)
                    ob = nextob()
                    tt("dve", ob.t[:], YC.t[:], ps.t[:, 0:G], ALU.mult, [YC.r, ps.r], [ob.r])
                    dma("sp", YCT[q, :, g0:g0 + G], ob.t[:], [ob.r], [r_YCT])
                    cp("pool", zc.t[:, 0:2], zc.t[:, G:G + 2], [zc.r], [zc.r])
                for t4 in range(G // 128):
                    t0 = g0 + t4 * 128
                    xs = slice(t4 * 128, (t4 + 1) * 128)
                    ps = nextps()
                    for k in range(8):
                        mm(ps.t[:, 0:128], Xb.t[:, k, xs], Wm.t[:, k, C_CKV:C_CKV + 128], k == 0, k == 7,
                           Wm.rl + [Xb.r], [ps.r])
                    act(JK.t[:], ps.t[:, 0:128], AF.Square, [ps.r], [JK.r, SS.r], accum=SS.t[:, 0:1])
                    act(SS.t[:, 1:2], SS.t[:, 0:1], AF.Sqrt, [SS.r, epsT.r], [SS.r], scale=1.0 / 128, bias=epsT.t[:, 0:1])
                    recip(SS.t[:, 1:2], SS.t[:, 1:2], [SS.r], [SS.r])
                    stt(CKb.t[:], ps.t[:, 0:128], SS.t[:, 1:2], KVG.t[:], ALU.mult, ALU.mult, [ps.r, SS.r, KVG.r], [CKb.r])
                    dma("sp", CKV[t0:t0 + 128, :], CKb.t[:], [CKb.r], [r_CKV])
                    tr(PB.t[:, 0:128], CKb.t[:], ident.t[:], [CKb.r, ident.r], [PB.r])
                    cp("act", CKTb.t[:], PB.t[:, 0:128], [PB.r], [CKTb.r])
                    dma("sp", CKVT[:, t0:t0 + 128], CKTb.t[:], [CKTb.r], [r_CKVT])
                    ps = nextps()
                    for k in range(8):
                        mm(ps.t[:, 0:8], Xb.t[:, k, xs], Wm.t[:, k, C_IW:C_IW + 8], k == 0, k == 7, Wm.rl + [Xb.r], [ps.r])
                    cp("act", IWs.t[:], ps.t[:, 0:8], [ps.r], [IWs.r])
                    dma("sp", IW[t0:t0 + 128, :], IWs.t[:], [IWs.r], [r_IW])
                    ps = nextps()
                    for k in range(8):
                        mm(ps.t[:, 0:512], Xb.t[:, k, xs], Wm.t[:, k, C_VS:C_VS + 512], k == 0, k == 7, Wm.rl + [Xb.r], [ps.r])
                    cp("act", VSb.t[:], ps.t[:, 0:512], [ps.r], [VSb.r])
                    dma("sp", VS[t0:t0 + 128, :], VSb.t[:], [VSb.r], [r_VS])
            P.barrier()

    def chunks(n):
        return [(c0, min(512, n - c0)) for c0 in range(0, n, 512)]

    def dsa_phase(l):
        with ExitStack() as st:
            CKVX = alloc(st, "CKVX", [128, NT, 128], BF16)
            CKT = alloc(st, "CKT", [128, S], BF16)
            IK2 = alloc(st, "IK2", [128, S], BF16)
            BI = alloc(st, "BI", [128, 8, 256], F32)
            RB = alloc(st, "RB", [128, 8], F32)
            WUVP = alloc(st, "WUVP", [128, 8, 128], BF16)
            QLg = [alloc(st, "QLg%d" % b, [128, 8, 512], BF16) for b in range(2)]
            IQg = [alloc(st, "IQg%d" % b, [128, 4, 512], BF16) for b in range(2)]
            IWg = [alloc(st, "IWg%d" % b, [128, 4, 8], F32) for b in range(2)]
            WA = [alloc(st, "WA%d" % b, [128, 8], F32) for b in range(2)]
            SG = [alloc(st, "SG%d" % b, [128, 8], F32) for b in range(2)]
            SC = [alloc(st, "SC%d" % b, [128, S], F32) for b in range(2)]
            Mk = [alloc(st, "Mk%d" % b, [128, S], BF16) for b in range(2)]
            TH = [alloc(st, "TH%d" % b, [128, 8], F32) for b in range(2)]
            WALL = [alloc(st, "WALL%d" % b, [128, NIT], F32) for b in range(2)]
            PM = [alloc(st, "PM%d" % b, [128, S], BF16) for b in range(2)]
            PT = [alloc(st, "PT%d" % b, [128, NT, 128], BF16) for b in range(2)]
            TMP = [alloc(st, "TMP%d" % i, [128, 512], F32) for i in range(4)]
            JKB = alloc(st, "JKB", [128, S], BF16)
            RSM = alloc(st, "RSM", [128, 8, 8], F32)
            RSUM = alloc(st, "RSUM", [128, 8], F32)
            OLN = alloc(st, "OLN", [128, 8, 128], BF16)
            OLT = alloc(st, "OLT", [128, 8, 128], BF16)
            YAg = alloc(st, "YAg", [128, 4, 512], BF16)
            dma("sp", CKVX.t[:], CKV.rearrange("(j p) c -> p j c", p=128), [r_CKV], [CKVX.r])
            dma("sp", CKT.t[:], CKVT, [r_CKVT], [CKT.r])
            dma("sp", IK2.t[0:64, :], IKT, [r_IKT], [IK2.r])
            dma("sp", IK2.t[64:128, :], IKT, [r_IKT], [IK2.r])
            dma("sp", BI.t[:], biasT, [r_const], [BI.r])
            dma("sp", RB.t[:], rb31.partition_broadcast(128), [r_const], [RB.r])
            memset("pool", WUVP.t[:], 0.0, [WUVP.r])
            for h in range(8):
                p0 = (h % 2) * 64
                dma("pool", WUVP.t[:, h, p0:p0 + 64], wuv[l, h], [r_const], [WUVP.r])
            tmr = [0]

            def nexttmp():
                tmr[0] = (tmr[0] + 1) % 4
                return TMP[tmr[0]]
            psr = [0]

            def nextps():
                psr[0] = (psr[0] + 1) % 3
                return PS[psr[0]]

            def load_group(g):
                b = g % 2
                g0 = g * 512
                dma("sp", QLg[b].t[:], QL[:, :, g0:g0 + 512].rearrange("h c t -> c h t"), [r_QL], [QLg[b].r])
                dma("sp", IQg[b].t[:], IQT[:, :, g0:g0 + 512].rearrange("q p t -> p q t"), [r_IQT], [IQg[b].r])
                dma("sp", IWg[b].t[:], IW[g0:g0 + 512, :].rearrange("(a p) h -> p a h", p=128), [r_IW], [IWg[b].r])

            def index_scores(i):
                g, t4 = divmod(i, 4)
                gb = g % 2
                b = i % 2
                n = (i + 1) * 128
                tsl = slice(t4 * 128, (t4 + 1) * 128)
                sc, th, wall, wa, sg, mk = SC[b], TH[b], WALL[b], WA[b], SG[b], Mk[b]
                act(wa.t[:], IWg[gb].t[:, t4, :], AF.Abs, [IWg[gb].r], [wa.r])
                P.op("act", lambda e: e.sign(out=sg.t[:], in_=IWg[gb].t[:, t4, :]), [IWg[gb].r], [sg.r])
                for (c0, w) in chunks(n):
                    for h in range(8):
                        p0 = (h % 2) * 64
                        ps = nextps()
                        mm(ps.t[:, 0:w], IQg[gb].t[p0:p0 + 64, h // 2, tsl], IK2.t[p0:p0 + 64, c0:c0 + w], True, True,
                           [IQg[gb].r, IK2.r], [ps.r])
                        R = nexttmp()
                        act(R.t[:, 0:w], ps.t[:, 0:w], AF.Relu, [ps.r, wa.r], [R.r], scale=wa.t[:, h:h + 1])
                        if h == 0:
                            ts("dve", sc.t[:, c0:c0 + w], R.t[:, 0:w], sg.t[:, 0:1], ALU.mult, [R.r, sg.r], [sc.r])
                        else:
                            stt(sc.t[:, c0:c0 + w], R.t[:, 0:w], sg.t[:, h:h + 1], sc.t[:, c0:c0 + w], ALU.mult, ALU.add,
                                [R.r, sg.r, sc.r], [sc.r])
                tt("dve", sc.t[:, n - 128:n], sc.t[:, n - 128:n], caus.t[:], ALU.add, [sc.r, caus.r], [sc.r])
                steps = []
                if i >= 2:
                    def init():
                        red(th.t[:, 0:1], sc.t[:, 0:n - 128], ALU.min, [sc.r], [th.r])
                        red(th.t[:, 1:2], sc.t[:, 0:n], ALU.max, [sc.r], [th.r])
                        tt("dve", th.t[:, 2:3], th.t[:, 1:2], th.t[:, 0:1], ALU.subtract, [th.r], [th.r])
                        ts("dve", wall.t[:], pow2.t[:], th.t[:, 2:3], ALU.mult, [pow2.r, th.r], [wall.r])
                        tt("dve", th.t[:, 3:4], th.t[:, 0:1], wall.t[:, 0:1], ALU.add, [th.r, wall.r], [th.r])
                    steps.append(init)

                    def mkstep(j):
                        def step():
                            ts("dve", JKB.t[:, 0:n], sc.t[:, 0:n], th.t[:, 3:4], ALU.is_ge, [sc.r, th.r], [th.r],
                               op1=ALU.add, accum=th.t[:, 4:5])
                            ts("dve", th.t[:, 5:6], th.t[:, 4:5], KTOP - 0.5, ALU.is_ge, [th.r, wall.r], [th.r],
                               s2=wall.t[:, j:j + 1], op1=ALU.mult)
                            if j + 1 < NIT:
                                stt(th.t[:, 3:4], th.t[:, 5:6], wall.t[:, j + 1:j + 2], th.t[:, 3:4], ALU.subtract, ALU.add,
                                    [th.r, wall.r], [th.r])
                            else:
                                stt(th.t[:, 0:1], th.t[:, 5:6], wall.t[:, j:j + 1], th.t[:, 3:4], ALU.subtract, ALU.add,
                                    [th.r, wall.r], [th.r])
                        return step
                    for j in range(NIT):
                        steps.append(mkstep(j))
                    steps.append(lambda: ts("dve", mk.t[:, 0:n], sc.t[:, 0:n], th.t[:, 0:1], ALU.is_ge, [sc.r, th.r], [mk.r]))
                else:
                    steps.append(lambda: ts("dve", mk.t[:, 0:n], sc.t[:, 0:n], neglo.t[:, 0:1], ALU.is_ge, [sc.r, neglo.r], [mk.r]))
                return steps

            pbr = [0]
            PB2 = [(PB.t[:], PB.r), (PS[6].t[:].bitcast(BF16), PS[6].r)]

            def heads(i, pending):
                g, t4 = divmod(i, 4)
                gb = g % 2
                n = (i + 1) * 128
                tsl = slice(t4 * 128, (t4 + 1) * 128)
                mk = Mk[i % 2]
                nb0 = max(0, n - 256)
                per_head = (len(pending) + 7) // 8
                ch = chunks(n)

                def chunk_phase(h):
                    pm = PM[h % 2]
                    for ci, (c0, w) in enumerate(ch):
                        ps = nextps()
                        mm(ps.t[:, 0:w], QLg[gb].t[:, h, tsl], CKT.t[:, c0:c0 + w], True, True, [QLg[gb].r, CKT.r], [ps.r])
                        Pe = nexttmp()
                        fa, fb = c0, min(c0 + w, nb0)
                        na, nb_ = max(c0, nb0), c0 + w
                        if fb > fa:
                            act(Pe.t[:, fa - c0:fb - c0], ps.t[:, fa - c0:fb - c0], AF.Exp, [ps.r, RB.r], [Pe.r],
                                bias=RB.t[:, h:h + 1])
                        if nb_ > na:
                            bo = 256 - (n - na)
                            tt("dve", Pe.t[:, na - c0:nb_ - c0], ps.t[:, na - c0:nb_ - c0], BI.t[:, h, bo:bo + (nb_ - na)],
                               ALU.add, [ps.r, BI.r], [Pe.r])
                            act(Pe.t[:, na - c0:nb_ - c0], Pe.t[:, na - c0:nb_ - c0], AF.Exp, [Pe.r], [Pe.r])
                        stt(pm.t[:, c0:c0 + w], Pe.t[:, 0:w], 1.0, mk.t[:, c0:c0 + w], ALU.mult, ALU.mult,
                            [Pe.r, mk.r], [pm.r, RSM.r], accum=RSM.t[:, h, ci:ci + 1])
                    red(RSUM.t[:, h:h + 1], RSM.t[:, h, 0:len(ch)], ALU.add, [RSM.r], [RSUM.r])
                    recip(RSUM.t[:, h:h + 1], RSUM.t[:, h:h + 1], [RSUM.r], [RSUM.r])

                def tail_phase(h):
                    pm, pt = PM[h % 2], PT[h % 2]
                    for j0 in range(0, i + 1, 8):
                        nbk = min(8, i + 1 - j0)
                        pbr[0] ^= 1
                        pbt, pbres = PB2[pbr[0]]
                        for jj in range(nbk):
                            j = j0 + jj
                            tr(pbt[:, jj * 128:(jj + 1) * 128], pm.t[:, j * 128:(j + 1) * 128], ident.t[:],
                               [pm.r, ident.r], [pbres], inc=(jj == nbk - 1))
                        cp("act", pt.t[:, j0:j0 + nbk, :], pbt[:, 0:nbk * 128].rearrange("p (j t) -> p j t", t=128),
                           [pbres], [pt.r])
                    ol = PS[3 + (h % 2)]
                    for j in range(i + 1):
                        mm(ol.t[:, 0:128], pt.t[:, j, :], CKVX.t[:, j, :], j == 0, j == i, [pt.r, CKVX.r], [ol.r])
                    act(OLN.t[:, h, :], ol.t[:, 0:128], AF.Copy, [ol.r, RSUM.r], [OLN.r], scale=RSUM.t[:, h:h + 1])

                chunk_phase(0)
                for h in range(8):
                    if h < 7:
                        chunk_phase(h + 1)
                    for _ in range(per_head):
                        if pending:
                            pending.pop(0)()
                    tail_phase(h)
                while pending:
                    pending.pop(0)()
                for h in range(8):
                    tr(PB.t[:, h * 128:(h + 1) * 128], OLN.t[:, h, :], ident.t[:], [OLN.r, ident.r], [PB.r], inc=(h == 7))
                cp("act", OLT.t[:], PB.t[:].rearrange("p (j t) -> p j t", t=128), [PB.r], [OLT.r])
                for q in range(4):
                    ps = PS[5]
                    mm(ps.t[:, 0:128], WUVP.t[:, 2 * q, :], OLT.t[:, 2 * q, :], True, False, [WUVP.r, OLT.r], [ps.r])
                    mm(ps.t[:, 0:128], WUVP.t[:, 2 * q + 1, :], OLT.t[:, 2 * q + 1, :], False, True, [WUVP.r, OLT.r], [ps.r])
                    cp("act", YAg.t[:, q, tsl], ps.t[:, 0:128], [ps.r], [YAg.r])
                if t4 == 3:
                    g0 = g * 512
                    dma("sp", YAT[:, :, g0:g0 + 512].rearrange("q p t -> p q t"), YAg.t[:], [YAg.r], [r_YAT])

            load_group(0)
            for s_ in index_scores(0):
                s_()
            for i in range(NT):
                pending = []
                if i + 1 < NT:
                    if (i + 1) % 4 == 0:
                        load_group((i + 1) // 4)
                    pending = index_scores(i + 1)
                heads(i, pending)
            P.barrier()

    def sb_phase(l):
        with ExitStack() as st:
            KS_ = alloc(st, "KS_", [128, 4, S], BF16)
            VSX = alloc(st, "VSX", [128, NT, 512], BF16)
            QSg = [alloc(st, "QSg%d" % b, [128, 4, 512], BF16) for b in range(2)]
            LB = [alloc(st, "LB%d" % b, [128, S + 1], F32) for b in range(2)]
            NLX = [alloc(st, "NLX", [128, S], F32)] * 2
            ZS = [alloc(st, "ZS%d" % b, [128, S], F32) for b in range(2)]
            AB = [alloc(st, "AB%d" % b, [128, S], BF16) for b in range(2)]
            PT = [alloc(st, "PT2%d" % b, [128, NT, 128], BF16) for b in range(2)]
            TMP = [alloc(st, "TMQ%d" % i, [128, 512], F32) for i in range(4)]
            TSM = [alloc(st, "TSM%d" % b, [128, 16], F32) for b in range(2)]
            TOT = [alloc(st, "TOT%d" % b, [128, 2], F32) for b in range(2)]
            YBs = alloc(st, "YBs", [128, 512], BF16)
            YBg = alloc(st, "YBg", [128, 4, 512], BF16)
            dma("sp", KS_.t[:], KST.rearrange("q p t -> p q t"), [r_KST], [KS_.r])
            dma("sp", VSX.t[:], VS.rearrange("(j p) c -> p j c", p=128), [r_VS], [VSX.r])
            for b in range(2):
                memset("pool", LB[b].t[:, 0:1], 0.0, [LB[b].r])
            tmr = [0]

            def nexttmp():
                tmr[0] = (tmr[0] + 1) % 4
                return TMP[tmr[0]]
            psr = [0]

            def nextps():
                psr[0] = (psr[0] + 1) % 4
                return PS[psr[0]]
            yb = PS[4]

            def pass1(i, h):
                g, t4 = divmod(i, 4)
                qs = QSg[g % 2]
                n = (i + 1) * 128
                tsl = slice(t4 * 128, (t4 + 1) * 128)
                p0 = (h % 2) * 64
                lb, tsm, tot, zs = LB[h % 2], TSM[h % 2], TOT[h % 2], ZS[h % 2]
                memset("dve", tsm.t[:], 0.0, [tsm.r])
                for ci, (c0, w) in enumerate(chunks(n)):
                    ps = nextps()
                    mm(ps.t[:, 0:w], qs.t[p0:p0 + 64, h // 2, tsl], KS_.t[p0:p0 + 64, h // 2, c0:c0 + w], True, True,
                       [qs.r, KS_.r], [ps.r])
                    E1 = nexttmp()
                    act(E1.t[:, 0:w], ps.t[:, 0:w], AF.Exp, [ps.r], [E1.r])
                    cp("dve", zs.t[:, c0:c0 + w], ps.t[:, 0:w], [ps.r], [zs.r])
                    last = (c0 + w == n)
                    wf = w - 128 if last else w
                    if wf > 0:
                        act(lb.t[:, 1 + c0:1 + c0 + wf], E1.t[:, 0:wf], AF.Ln, [E1.r, oneT.r], [lb.r, tsm.r],
                            bias=oneT.t[:, 0:1], accum=tsm.t[:, ci:ci + 1])
                    if last:
                        act(lb.t[:, 1 + n - 128:1 + n], E1.t[:, w - 128:w], AF.Ln, [E1.r, oneT.r], [lb.r], bias=oneT.t[:, 0:1])
                        tt("dve", lb.t[:, 1 + n - 128:1 + n], lb.t[:, 1 + n - 128:1 + n], strict.t[:], ALU.mult,
                           [lb.r, strict.r], [lb.r])
                        red(tsm.t[:, 15:16], lb.t[:, 1 + n - 128:1 + n], ALU.add, [lb.r], [tsm.r])
                red(tot.t[:, 0:1], tsm.t[:], ALU.add, [tsm.r], [tot.r])
                ts("dve", tot.t[:, 1:2], tot.t[:, 0:1], -1.0, ALU.mult, [tot.r], [tot.r])

            def scan_part(i, h):
                n = (i + 1) * 128
                lb, tot, nlx = LB[h % 2], TOT[h % 2], NLX[h % 2]
                P.op("dve", lambda e: e.tensor_tensor_scan(out=nlx.t[:, 0:n], data0=lb.t[:, 0:n], data1=lb.t[:, 0:n],
                                                           initial=tot.t[:, 1:2], op0=ALU.add, op1=ALU.min),
                     [lb.r, tot.r], [nlx.r])

            pbr = [0]
            PB2 = [(PB.t[:], PB.r), (PS[5].t[:].bitcast(BF16), PS[5].r)]

            def pass2(i, h):
                g, t4 = divmod(i, 4)
                qs = QSg[g % 2]
                n = (i + 1) * 128
                tsl = slice(t4 * 128, (t4 + 1) * 128)
                p0 = (h % 2) * 64
                lb, tot, nlx, ab, pt = LB[h % 2], TOT[h % 2], NLX[h % 2], AB[h % 2], PT[h % 2]
                zs = ZS[h % 2]
                for ci, (c0, w) in enumerate(chunks(n)):
                    EB = nexttmp()
                    tt("dve", EB.t[:, 0:w], zs.t[:, c0:c0 + w], nlx.t[:, c0:c0 + w], ALU.add, [zs.r, nlx.r], [EB.r])
                    act(ab.t[:, c0:c0 + w], EB.t[:, 0:w], AF.Exp, [EB.r], [ab.r])
                tt("dve", ab.t[:, n - 128:n], ab.t[:, n - 128:n], strict.t[:], ALU.mult, [ab.r, strict.r], [ab.r])
                for j0 in range(0, i + 1, 8):
                    nbk = min(8, i + 1 - j0)
                    pbr[0] ^= 1
                    pbt, pbres = PB2[pbr[0]]
                    for jj in range(nbk):
                        j = j0 + jj
                        tr(pbt[:, jj * 128:(jj + 1) * 128], ab.t[:, j * 128:(j + 1) * 128], ident.t[:],
                           [ab.r, ident.r], [pbres], inc=(jj == nbk - 1))
                    cp("act", pt.t[:, j0:j0 + nbk, :], pbt[:, 0:nbk * 128].rearrange("p (j t) -> p j t", t=128),
                       [pbres], [pt.r])
                for j in range(i + 1):
                    mm(yb.t[:, h * 64:(h + 1) * 64], pt.t[:, j, :], VSX.t[:, j, h * 64:(h + 1) * 64], j == 0, j == i,
                       [pt.r, VSX.r], [yb.r])

            def load_group(g):
                g0 = g * 512
                dma("sp", QSg[g % 2].t[:], QST[:, :, g0:g0 + 512].rearrange("q p t -> p q t"), [r_QST], [QSg[g % 2].r])

            load_group(0)
            pass1(0, 0)
            for i in range(NT):
                g, t4 = divmod(i, 4)
                tsl = slice(t4 * 128, (t4 + 1) * 128)
                for h in range(8):
                    scan_part(i, h)
                    if h < 7:
                        pass1(i, h + 1)
                    elif i + 1 < NT:
                        if (i + 1) % 4 == 0:
                            load_group((i + 1) // 4)
                        pass1(i + 1, 0)
                    pass2(i, h)
                cp("act", YBs.t[:], yb.t[:, 0:512], [yb.r], [YBs.r])
                for q in range(4):
                    tr(PB.t[:, q * 128:(q + 1) * 128], YBs.t[:, q * 128:(q + 1) * 128], ident.t[:], [YBs.r, ident.r], [PB.r], inc=(q == 3))
                cp("act", YBg.t[:, :, tsl], PB.t[:, 0:512].rearrange("p (q t) -> p q t", t=128), [PB.r], [YBg.r])
                if t4 == 3:
                    g0 = g * 512
                    dma("sp", YBT[:, :, g0:g0 + 512].rearrange("q p t -> p q t"), YBg.t[:], [YBg.r], [r_YBT])
            P.barrier()

    def merge_phase(l, src, r_src, dst, r_dst):
        G = 256
        NG = S // G
        with ExitStack() as st:
            WG = alloc(st, "WG", [128, 8, 3 * D], BF16)
            WBR = alloc(st, "WBR", [128, 12, D], BF16)
            WO = alloc(st, "WO", [128, 8, D], BF16)
            BG = alloc(st, "BG", [128, 3, 8], F32)
            gT = alloc(st, "gT", [128, 8], F32); bT = alloc(st, "bT", [128, 8], F32)
            X32 = [alloc(st, "X32%d" % b, [128, 8, G], F32) for b in range(2)]
            Xb = [alloc(st, "Xb%d" % b, [128, 8, G], BF16) for b in range(2)]
            Y = [[alloc(st, "Y%d_%d" % (i, b), [128, 4, G], BF16) for i in range(3)] for b in range(2)]
            SGt = [alloc(st, "SGt%d" % i, [128, G], F32) for i in range(2)]
            TM = [alloc(st, "TM%d" % i, [128, G], F32) for i in range(2)]
            MGf = alloc(st, "MGf", [128, 8, G], F32)
            MGb = alloc(st, "MGb", [128, 8, G], BF16)
            Z = [alloc(st, "Z%d" % b, [128, 8, G], F32) for b in range(2)]
            SQ = alloc(st, "SQ", [128, 8, G], F32)
            RS = alloc(st, "RS", [128, G], F32)
            dma("sp", gT.t[:], lng[l, 1], [r_const], [gT.r])
            dma("sp", bT.t[:], lnb[l, 1], [r_const], [bT.r])
            dma("sp", BG.t[:], bgate[l].rearrange("i p c -> p i c"), [r_const], [BG.r])
            wload(WG, 8, w_mix[l], C_G, PMIX)
            for i in range(3):
                for k in range(4):
                    dma("pool", WBR.t[:, i * 4 + k, :], wbr[l, i, k * 128:(k + 1) * 128, :], [r_const], [WBR.r], max_dma_last_dim=8192)
            wload(WO, 8, wmo[l], 0, D)
            srcs = ((YAT, r_YAT), (YBT, r_YBT), (YCT, r_YCT))

            def load(g):
                b = g % 2
                g0 = g * G
                dma("sp", X32[b].t[:], src[:, :, g0:g0 + G].rearrange("c p t -> p c t"), [r_src], [X32[b].r])
                cp("pool", Xb[b].t[:], X32[b].t[:], [X32[b].r], [Xb[b].r])
                for i in range(3):
                    dma("sp", Y[b][i].t[:], srcs[i][0][:, :, g0:g0 + G].rearrange("q p t -> p q t"), [srcs[i][1]], [Y[b][i].r])

            def stage1(g):
                xb, y = Xb[g % 2], Y[g % 2]
                n_ = 0
                for c in range(8):
                    for i in range(3):
                        pg = PS[n_ % 2]; pb_ = PS[2 + n_ % 2]; sg = SGt[n_ % 2]; tm = TM[n_ % 2]
                        n_ += 1
                        col = i * D + c * 128
                        for k in range(8):
                            mm(pg.t[:, 0:G], WG.t[:, k, col:col + 128], xb.t[:, k, :], k == 0, k == 7, WG.rl + [xb.r], [pg.r])
                        act(sg.t[:], pg.t[:, 0:G], AF.Sigmoid, [pg.r, BG.r], [sg.r], bias=BG.t[:, i, c:c + 1])
                        for k in range(4):
                            mm(pb_.t[:, 0:G], WBR.t[:, i * 4 + k, c * 128:(c + 1) * 128], y[i].t[:, k, :], k == 0, k == 3,
                               [WBR.r, y[i].r], [pb_.r])
                        if i == 0:
                            tt("dve", MGf.t[:, c, :], sg.t[:], pb_.t[:, 0:G], ALU.mult, [sg.r, pb_.r], [MGf.r])
                        else:
                            tt("dve", tm.t[:], sg.t[:], pb_.t[:, 0:G], ALU.mult, [sg.r, pb_.r], [tm.r])
                            tt("pool", MGf.t[:, c, :], MGf.t[:, c, :], tm.t[:], ALU.add, [MGf.r, tm.r], [MGf.r])
                cp("pool", MGb.t[:], MGf.t[:], [MGf.r], [MGb.r])

            def stage2(g):
                x32, z = X32[g % 2], Z[g % 2]
                for c in range(8):
                    po = PS[4 + (c % 2)]
                    for k in range(8):
                        mm(po.t[:, 0:G], WO.t[:, k, c * 128:(c + 1) * 128], MGb.t[:, k, :], k == 0, k == 7, WO.rl + [MGb.r], [po.r])
                    stt(z.t[:, c, :], x32.t[:, c, :], ALPHA, po.t[:, 0:G], ALU.mult, ALU.add, [x32.r, po.r], [z.r])

            load(0)
            if NG > 1:
                load(1)
            stage1(0)
            stage2(0)
            for g in range(NG):
                if g + 2 < NG:
                    load(g + 2)
                if g + 1 < NG:
                    stage1(g + 1)
                ln_part1(Z[g % 2], SQ, G, PS[6])
                if g + 1 < NG:
                    stage2(g + 1)
                ln_part2(Z[g % 2], SQ, RS, G, gT, bT, dst, r_dst, g * G, PS[6])
            P.barrier()

    def xattn_phase(l, src, r_src, dst, r_dst):
        G = 256
        with ExitStack() as st:
            WQ = alloc(st, "WQ", [128, 8, 512], BF16)
            WKV = alloc(st, "WKV", [128, 8, D], BF16)
            WO = alloc(st, "WOx", [128, 4, D], BF16)
            gT = alloc(st, "gT", [128, 8], F32); bT = alloc(st, "bT", [128, 8], F32)
            MT = alloc(st, "MT", [128, 8, 256], BF16)
            KT = alloc(st, "KT", [128, 4, 256], BF16)
            VX = alloc(st, "VX", [128, 2, 512], BF16)
            X32 = [alloc(st, "X32%d" % b_, [128, 8, G], F32) for b_ in range(2)]
            Xb = [alloc(st, "Xb%d" % b_, [128, 8, G], BF16) for b_ in range(2)]
            QT = alloc(st, "QT", [128, 4, G], BF16)
            PTm = [alloc(st, "PTm%d" % i, [128, G], BF16) for i in range(2)]
            RD = alloc(st, "RD", [128, G], F32)
            OT = alloc(st, "OT", [128, 4, G], BF16)
            Z = [alloc(st, "Z%d" % b_, [128, 8, G], F32) for b_ in range(2)]
            SQ = alloc(st, "SQ", [128, 8, G], F32)
            RS = alloc(st, "RS", [128, G], F32)
            dma("sp", gT.t[:], lng[l, 2], [r_const], [gT.r])
            dma("sp", bT.t[:], lnb[l, 2], [r_const], [bT.r])
            wload(WQ, 8, xwq[l], 0, 512)
            wload(WKV, 8, xwkv[l], 0, D)
            wload(WO, 4, xwo[l], 0, D)
            dma("pool", MT.t[:], memT.rearrange("c p m -> p c m"), [r_const], [MT.r])
            for h in range(4):
                ps = PS[h % 2]
                for k in range(8):
                    mm(ps.t[:, 0:256], WKV.t[:, k, h * 128:(h + 1) * 128], MT.t[:, k, :], k == 0, k == 7, WKV.rl + [MT.r], [ps.r])
                cp("act", KT.t[:, h, :], ps.t[:, 0:256], [ps.r], [KT.r])
            for mt in range(2):
                ps = PS[2 + mt]
                for k in range(8):
                    mm(ps.t[:, 0:512], MT.t[:, k, mt * 128:(mt + 1) * 128], WKV.t[:, k, 512:1024], k == 0, k == 7, WKV.rl + [MT.r], [ps.r])
                cp("act", VX.t[:, mt, :], ps.t[:, 0:512], [ps.r], [VX.r])
            def load(g):
                b_ = g % 2
                g0 = g * G
                dma("sp", X32[b_].t[:], src[:, :, g0:g0 + G].rearrange("c p t -> p c t"), [r_src], [X32[b_].r])
                cp("pool", Xb[b_].t[:], X32[b_].t[:], [X32[b_].r], [Xb[b_].r])

            def attn(g):
                xb = Xb[g % 2]
                for h in range(4):
                    ps = PS[h % 2]
                    for k in range(8):
                        mm(ps.t[:, 0:G], WQ.t[:, k, h * 128:(h + 1) * 128], xb.t[:, k, :], k == 0, k == 7, WQ.rl + [xb.r], [ps.r])
                    act(QT.t[:, h, :], ps.t[:, 0:G], AF.Copy, [ps.r], [QT.r], scale=128.0 ** -0.5)
                for h in range(4):
                    for mt in range(2):
                        ps = PS[mt]
                        mm(ps.t[:, 0:G], KT.t[:, h, mt * 128:(mt + 1) * 128], QT.t[:, h, :], True, True, [KT.r, QT.r], [ps.r])
                        act(PTm[mt].t[:], ps.t[:, 0:G], AF.Exp, [ps.r], [PTm[mt].r])
                    po = PS[2]; pd = PS[3]
                    for mt in range(2):
                        mm(po.t[:, 0:G], VX.t[:, mt, h * 128:(h + 1) * 128], PTm[mt].t[:], mt == 0, mt == 1, [VX.r, PTm[mt].r], [po.r])
                    for mt in range(2):
                        mm(pd.t[:, 0:G], onesB.t[:], PTm[mt].t[:], mt == 0, mt == 1, [onesB.r, PTm[mt].r], [pd.r])
                    recip(RD.t[:], pd.t[:, 0:G], [pd.r], [RD.r])
                    tt("dve", OT.t[:, h, :], po.t[:, 0:G], RD.t[:], ALU.mult, [po.r, RD.r], [OT.r])

            def outp(g):
                x32, z = X32[g % 2], Z[g % 2]
                for c in range(8):
                    po = PS[4 + (c % 2)]
                    for k in range(4):
                        mm(po.t[:, 0:G], WO.t[:, k, c * 128:(c + 1) * 128], OT.t[:, k, :], k == 0, k == 3, WO.rl + [OT.r], [po.r])
                    stt(z.t[:, c, :], x32.t[:, c, :], ALPHA, po.t[:, 0:G], ALU.mult, ALU.add, [x32.r, po.r], [z.r])

            NG = S // G
            load(0)
            if NG > 1:
                load(1)
            attn(0)
            outp(0)
            for g in range(NG):
                if g + 2 < NG:
                    load(g + 2)
                if g + 1 < NG:
                    attn(g + 1)
                ln_part1(Z[g % 2], SQ, G, PS[6])
                if g + 1 < NG:
                    outp(g + 1)
                ln_part2(Z[g % 2], SQ, RS, G, gT, bT, dst, r_dst, g * G, PS[6])
            P.barrier()

    P.barrier()
    cur, r_cur = xT, r_xT
    nph = [0]

    def run(f, *a):
        if nph[0] < stop_after:
            f(*a)
        nph[0] += 1
    for l in range(L):
        last = (l == L - 1)
        run(ffn_phase, l, 0, 0, cur, r_cur, XB, r_XB)
        run(proj_phase, l, XB, r_XB)
        run(dsa_phase, l)
        run(sb_phase, l)
        run(merge_phase, l, XB, r_XB, XA, r_XA)
        run(xattn_phase, l, XA, r_XA, XB, r_XB)
        if last:
            run(ffn_phase, l, 1, 3, XB, r_XB, outT, r_outT)
        else:
            run(ffn_phase, l, 1, 3, XB, r_XB, XA, r_XA)
        cur, r_cur = XA, r_XA
    P.emit()
    top.close()
    return nc, P


def _bucket_table():
    import math
    n = np.arange(256)
    nf = np.maximum(n, 1).astype(np.float32)
    large = 16 + (np.log(nf / np.float32(16)) / np.float32(math.log(8.0)) * np.float32(16)).astype(np.int32)
    large = np.minimum(large, 31)
    return np.where(n < 16, n, large)


def prep_inputs(x, mem, ln_g, ln_b, ffn_w_in, ffn_w_out, w_mix_in, b_gate, kv_norm_g, w_uk, w_uv,
                conv_w, w_branch, w_mix_out, xa_wq, xa_wkv, xa_wo, rel_bias):
    f = lambda a: np.ascontiguousarray(np.asarray(a, dtype=np.float32))
    B, S, _ = x.shape
    L = ln_g.shape[0]
    bt = _bucket_table()
    tl = np.arange(128)[:, None]
    col = np.arange(256)[None, :]
    nrel = np.where(col < 128, tl - col + 128, tl - (col - 128))
    idx = bt[np.clip(nrel, 0, 255)]
    rel_bias = f(rel_bias)
    biasT = f(np.transpose(rel_bias[idx], (0, 2, 1)))
    shared = {
        "lng": f(np.transpose(f(ln_g).reshape(L, 4, 8, 128), (0, 1, 3, 2))),
        "lnb": f(np.transpose(f(ln_b).reshape(L, 4, 8, 128), (0, 1, 3, 2))),
        "ffn_w_in": f(ffn_w_in), "ffn_w_out": f(ffn_w_out), "w_mix_in": f(w_mix_in),
        "bgate": f(np.transpose(f(b_gate).reshape(L, 3, 8, 128), (0, 1, 3, 2))),
        "kvg": f(kv_norm_g),
        "wuk": f(np.transpose(f(w_uk).reshape(L, 4, 2, 64, 128), (0, 2, 3, 1, 4)).reshape(L, 128, 4, 128)),
        "wuv": f(w_uv),
        "convw": f(np.transpose(f(conv_w).reshape(L, 3, 4, 128), (0, 3, 2, 1))),
        "w_branch": f(w_branch), "w_mix_out": f(w_mix_out),
        "xa_wq": f(xa_wq), "xa_wkv": f(xa_wkv), "xa_wo": f(xa_wo),
        "biasT": biasT, "rb31": f(rel_bias[31]),
    }
    maps = []
    for b in range(B):
        m = dict(shared)
        m["xT"] = f(f(x[b]).T.reshape(8, 128, S))
        m["memT"] = f(f(mem[b]).T.reshape(8, 128, 256))
        maps.append(m)
    return maps


_NC_CACHE = {}


def kernel(**inputs):
    x = np.asarray(inputs["x"])
    B, S, _ = x.shape
    L = np.asarray(inputs["ln_g"]).shape[0]
    key = (S, L)
    if key not in _NC_CACHE:
        _NC_CACHE[key] = build(S, L)[0]
    nc = _NC_CACHE[key]
    maps = prep_inputs(**inputs)
    res = run_bass_kernel_spmd(nc, maps, core_ids=list(range(B)))
    out = np.empty((B, S, D), dtype=np.float32)
    for b in range(B):
        out[b] = np.asarray(res.results[b]["outT"]).reshape(D, S).T
    return out
```
